# Optimizing a Trainium2 kernel written in Bass

```python
import math
import jax
import jax.numpy as jnp
from jax import lax
import numpy as np

D_MODEL = 1024
BATCH = 2
SEQ = 8192
DEPTH = 4

D_MIX = 1024
FOX_HEADS = 6
FOX_HEAD_DIM = 64
FOX_WIDTH = FOX_HEADS * FOX_HEAD_DIM
Q_BLOCK = 128
GLA_HEADS = 4
GLA_DV = 96
GLA_DK = 48
GLA_KWIDTH = GLA_HEADS * GLA_DK
GLA_VWIDTH = GLA_HEADS * GLA_DV
GLA_GATE_RANK = 16
GLA_GATE_TAU = 16.0
GLA_CHUNK = 64
POOL_WINDOWS = (2, 4, 8, 16)
POOL_GROUP = 64
POOL_WIDTH = len(POOL_WINDOWS) * POOL_GROUP
IN_SPLITS = (FOX_WIDTH, FOX_WIDTH, FOX_WIDTH, FOX_HEADS,
             GLA_KWIDTH, GLA_KWIDTH, GLA_VWIDTH, GLA_VWIDTH, GLA_GATE_RANK,
             POOL_WIDTH)
IN_OFFSETS = tuple(int(o) for o in np.cumsum(IN_SPLITS)[:-1])
N_IN = sum(IN_SPLITS)
FORGET_OFFSET = 3 * FOX_WIDTH
N_EXPERTS = 32
TOP_K = 4
D_EXPERT = 1024
SWIGLU_ALPHA = 1.702
SWIGLU_LIMIT = 7.0
MOE_BLOCK = 256
N_MOD = 6
LN_EPS = 1e-5
RMS_EPS = 1e-6
DEEPNORM_ALPHA = (2 * DEPTH) ** 0.25
DEEPNORM_BETA = (8 * DEPTH) ** -0.25

kernel_name = 'hybrid_fox_gla_pool_moe_deepnorm'


def layer_norm(x, g, b):
    xf = x.astype(jnp.float32)
    mu = jnp.mean(xf, axis=-1, keepdims=True)
    var = jnp.mean(jnp.square(xf - mu), axis=-1, keepdims=True)
    return ((xf - mu) * lax.rsqrt(var + LN_EPS)).astype(x.dtype) * g + b


def forgetting_attention(q, k, v, f_logit):
    B, S, H, Dh = q.shape
    nb = S // Q_BLOCK
    scale = Dh ** -0.5
    F = jnp.cumsum(jax.nn.log_sigmoid(f_logit.astype(jnp.float32)), axis=1).transpose(0, 2, 1)
    k_t = k.transpose(0, 2, 1, 3)
    v_t = v.transpose(0, 2, 1, 3)
    q_blocks = q.reshape(B, nb, Q_BLOCK, H, Dh).transpose(1, 0, 3, 2, 4)
    F_blocks = F.reshape(B, H, nb, Q_BLOCK).transpose(2, 0, 1, 3)
    k_pos = jnp.arange(S)

    def block(args):
        qb, Fq, i = args
        q_pos = i * Q_BLOCK + jnp.arange(Q_BLOCK)
        s = jnp.einsum('bhqd,bhkd->bhqk', qb, k_t, preferred_element_type=jnp.float32) * scale
        s = s + Fq[..., None] - F[:, :, None, :]
        s = jnp.where(k_pos[None, :] <= q_pos[:, None], s, -jnp.inf)
        p = jax.nn.softmax(s, axis=-1)
        return jnp.einsum('bhqk,bhkd->bhqd', p.astype(v.dtype), v_t)

    o = lax.map(block, (q_blocks, F_blocks, jnp.arange(nb)))
    return o.transpose(1, 0, 3, 2, 4).reshape(B, S, H * Dh)


def gla_chunked(q, k, v, log_a):
    B, S, H, Dk = q.shape
    Dv = v.shape[-1]
    C = GLA_CHUNK
    n = S // C

    def chunk(t):
        return t.astype(jnp.float32).reshape(B, n, C, H, -1).transpose(0, 3, 1, 2, 4)

    qc, kc, vc, ac = chunk(q) * (Dk ** -0.5), chunk(k), chunk(v), chunk(log_a)
    b = jnp.cumsum(ac, axis=3)
    b_last = b[:, :, :, -1:, :]
    q_in = qc * jnp.exp(b)
    k_in = kc * jnp.exp(-b)
    k_out = kc * jnp.exp(b_last - b)
    causal = jnp.tril(jnp.ones((C, C), dtype=bool))
    attn = jnp.where(causal, jnp.einsum('bhnid,bhnjd->bhnij', q_in, k_in), 0.0)
    o_intra = jnp.einsum('bhnij,bhnjv->bhniv', attn, vc)
    kv = jnp.einsum('bhncd,bhncv->bhndv', k_out, vc)
    decay = jnp.exp(b_last[:, :, :, 0, :])

    def step(state, inp):
        kv_n, dec_n = inp
        return state * dec_n[..., None] + kv_n, state

    init = jnp.zeros((B, H, Dk, Dv), jnp.float32)
    _, states = lax.scan(step, init, (kv.transpose(2, 0, 1, 3, 4), decay.transpose(2, 0, 1, 3)))
    o_inter = jnp.einsum('bhncd,nbhdv->bhncv', q_in, states)
    o = o_intra + o_inter
    return o.transpose(0, 2, 3, 1, 4).reshape(B, S, H, Dv)


def multiscale_pool(u, w_pool, pool_scale):
    B, S, W = u.shape
    uf = u.astype(jnp.float32)
    cs = jnp.concatenate([jnp.zeros((B, 1, W), jnp.float32), jnp.cumsum(uf, axis=1)], axis=1)
    win = jnp.repeat(jnp.array(POOL_WINDOWS, jnp.int32), POOL_GROUP)
    pos = jnp.arange(S, dtype=jnp.int32)
    lo_idx = jnp.maximum(pos[:, None] + 1 - win[None, :], 0)
    lo = jnp.take_along_axis(cs, jnp.broadcast_to(lo_idx[None], (B, S, W)), axis=1)
    count = jnp.minimum(pos[:, None] + 1, win[None, :]).astype(jnp.float32)
    pooled = (cs[:, 1:] - lo) / count[None] - uf
    pooled = pooled.reshape(B, S, len(POOL_WINDOWS), POOL_GROUP)
    mixed = jnp.einsum('bsgc,gcd->bsgd', pooled, w_pool.astype(jnp.float32)).reshape(B, S, W)
    return (mixed * pool_scale.astype(jnp.float32)).astype(u.dtype)


def hybrid_mixer(h, w_in, b_in, gla_w_a2, gla_b_a, gla_norm_g, pool_w, pool_scale, w_out):
    B, S, _ = h.shape
    z = h @ w_in + b_in
    fq, fk, fv, ff, gq, gk, gv, gr, ga1, pu = jnp.split(z, IN_OFFSETS, axis=-1)
    hd = (B, S, FOX_HEADS, FOX_HEAD_DIM)
    o_fox = forgetting_attention(fq.reshape(hd), fk.reshape(hd), fv.reshape(hd), ff)
    log_a = jax.nn.log_sigmoid((ga1 @ gla_w_a2 + gla_b_a).astype(jnp.float32)) / GLA_GATE_TAU
    kd = (B, S, GLA_HEADS, GLA_DK)
    o = gla_chunked(gq.reshape(kd), gk.reshape(kd), gv.reshape(B, S, GLA_HEADS, GLA_DV), log_a.reshape(kd))
    o = o * lax.rsqrt(jnp.mean(jnp.square(o), axis=-1, keepdims=True) + RMS_EPS)
    o_gla = (o.reshape(B, S, GLA_VWIDTH).astype(h.dtype) * gla_norm_g) * jax.nn.silu(gr)
    o_pool = multiscale_pool(pu, pool_w, pool_scale)
    return jnp.concatenate([o_fox, o_gla, o_pool], axis=-1) @ w_out


def clamped_swiglu(gu):
    glu, lin = jnp.split(gu, 2, axis=-1)
    glu = jnp.minimum(glu, SWIGLU_LIMIT)
    lin = jnp.clip(lin, -SWIGLU_LIMIT, SWIGLU_LIMIT)
    return glu * jax.nn.sigmoid(SWIGLU_ALPHA * glu) * (lin + 1.0)


def moe_ffn(h, w_router, b_router, w_gate_up, b_gate_up, w_down, b_down):
    B, S, D = h.shape
    T = B * S
    hf = h.reshape(T, D)
    logits = (hf @ w_router + b_router).astype(jnp.float32)
    top_logit, top_e = lax.top_k(logits, TOP_K)
    gate = jax.nn.softmax(top_logit, axis=-1)
    M = T * TOP_K
    flat_e = top_e.reshape(M)
    order = jnp.argsort(flat_e)
    sorted_e = flat_e[order]
    token_of = (order // TOP_K).astype(jnp.int32)
    counts = jnp.bincount(flat_e, length=N_EXPERTS)
    padded = (counts + MOE_BLOCK - 1) // MOE_BLOCK * MOE_BLOCK
    group_start = jnp.cumsum(counts) - counts
    padded_end = jnp.cumsum(padded)
    padded_start = padded_end - padded
    dest = padded_start[sorted_e] + jnp.arange(M) - group_start[sorted_e]
    n_blocks = -(-M // MOE_BLOCK) + N_EXPERTS
    rows = n_blocks * MOE_BLOCK
    row_token = jnp.full((rows,), T, jnp.int32).at[dest].set(token_of)
    h_pad = jnp.concatenate([hf, jnp.zeros((1, D), hf.dtype)], axis=0)
    x_rows = h_pad[row_token].reshape(n_blocks, MOE_BLOCK, D)
    block_expert = jnp.minimum(
        jnp.searchsorted(padded_end, jnp.arange(n_blocks) * MOE_BLOCK, side='right'), N_EXPERTS - 1)

    def expert_block(args):
        xb, e = args
        gu = xb @ w_gate_up[e] + b_gate_up[e]
        return clamped_swiglu(gu) @ w_down[e] + b_down[e]

    y_rows = lax.map(expert_block, (x_rows, block_expert)).reshape(rows, D)
    y = y_rows[dest] * gate.reshape(M)[order][:, None].astype(h.dtype)
    out = jnp.zeros((T, D), h.dtype).at[token_of].add(y)
    return out.reshape(B, S, D)


def setup_inputs(seed: int = 0) -> dict:
    key = jax.random.key(seed)
    ks = jax.random.split(key, 24)
    nrm = jax.random.normal
    f32 = jnp.float32
    L, D = DEPTH, D_MODEL
    x = nrm(ks[0], (BATCH, SEQ, D), f32)
    c = nrm(ks[1], (BATCH, D), f32)
    w_ada = nrm(ks[2], (L, D, N_MOD * D), f32) * (0.1 * D ** -0.5)
    b_ada = nrm(ks[3], (L, N_MOD * D), f32) * 0.02
    w_in = nrm(ks[4], (L, D, N_IN), f32) * D ** -0.5
    b_in = nrm(ks[5], (L, N_IN), f32) * 0.02
    b_in = b_in.at[:, FORGET_OFFSET:FORGET_OFFSET + FOX_HEADS].set(
        2.0 + 0.5 * nrm(ks[6], (L, FOX_HEADS), f32))
    gla_w_a2 = nrm(ks[7], (L, GLA_GATE_RANK, GLA_KWIDTH), f32) * GLA_GATE_RANK ** -0.5
    gla_b_a = nrm(ks[8], (L, GLA_KWIDTH), f32) * 0.1
    gla_norm_g = 1.0 + 0.02 * nrm(ks[9], (L, GLA_VWIDTH), f32)
    pool_w = nrm(ks[10], (L, len(POOL_WINDOWS), POOL_GROUP, POOL_GROUP), f32) * POOL_GROUP ** -0.5
    pool_scale = 1.0 + 0.02 * nrm(ks[11], (L, POOL_WIDTH), f32)
    w_out = nrm(ks[12], (L, D_MIX, D), f32) * (D_MIX ** -0.5 * DEEPNORM_BETA)
    ln1_g = 1.0 + 0.02 * nrm(ks[13], (L, D), f32)
    ln1_b = 0.02 * nrm(ks[14], (L, D), f32)
    w_router = nrm(ks[15], (L, D, N_EXPERTS), f32) * D ** -0.5
    b_router = 0.01 * nrm(ks[16], (L, N_EXPERTS), f32)
    w_gate_up = nrm(ks[17], (L, N_EXPERTS, D, 2 * D_EXPERT), f32) * D ** -0.5
    b_gate_up = 0.02 * nrm(ks[18], (L, N_EXPERTS, 2 * D_EXPERT), f32)
    w_down = nrm(ks[19], (L, N_EXPERTS, D_EXPERT, D), f32) * (D_EXPERT ** -0.5 * DEEPNORM_BETA)
    b_down = 0.02 * nrm(ks[20], (L, N_EXPERTS, D), f32)
    ln2_g = 1.0 + 0.02 * nrm(ks[21], (L, D), f32)
    ln2_b = 0.02 * nrm(ks[22], (L, D), f32)
    return {'x': x, 'c': c, 'w_ada': w_ada, 'b_ada': b_ada, 'w_in': w_in, 'b_in': b_in,
            'gla_w_a2': gla_w_a2, 'gla_b_a': gla_b_a, 'gla_norm_g': gla_norm_g,
            'pool_w': pool_w, 'pool_scale': pool_scale, 'w_out': w_out,
            'ln1_g': ln1_g, 'ln1_b': ln1_b, 'w_router': w_router, 'b_router': b_router,
            'w_gate_up': w_gate_up, 'b_gate_up': b_gate_up, 'w_down': w_down, 'b_down': b_down,
            'ln2_g': ln2_g, 'ln2_b': ln2_b}


def reference(x, c, w_ada, b_ada, w_in, b_in, gla_w_a2, gla_b_a, gla_norm_g, pool_w, pool_scale,
              w_out, ln1_g, ln1_b, w_router, b_router, w_gate_up, b_gate_up, w_down, b_down,
              ln2_g, ln2_b):
    cond = jax.nn.silu(c)
    for l in range(DEPTH):
        mod = cond @ w_ada[l] + b_ada[l]
        shift1, scale1, gate1, shift2, scale2, gate2 = [m[:, None, :] for m in jnp.split(mod, N_MOD, axis=-1)]
        h = x * (1.0 + scale1) + shift1
        y = hybrid_mixer(h, w_in[l], b_in[l], gla_w_a2[l], gla_b_a[l], gla_norm_g[l],
                         pool_w[l], pool_scale[l], w_out[l])
        x = layer_norm(DEEPNORM_ALPHA * x + (1.0 + gate1) * y, ln1_g[l], ln1_b[l])
        h = x * (1.0 + scale2) + shift2
        y = moe_ffn(h, w_router[l], b_router[l], w_gate_up[l], b_gate_up[l], w_down[l], b_down[l])
        x = layer_norm(DEEPNORM_ALPHA * x + (1.0 + gate2) * y, ln2_g[l], ln2_b[l])
    return x
```

```python
import numpy as np
import concourse.bass as bass
import concourse.mybir as mybir
from concourse.bass_utils import run_bass_kernel_spmd

F32 = mybir.dt.float32
F32R = mybir.dt.float32r
I32 = mybir.dt.int32
AF = mybir.ActivationFunctionType
ALU = mybir.AluOpType
AX = mybir.AxisListType


class KB:
    NDMA = 24

    def __init__(self):
        nc = bass.Bass("TRN2", target_bir_lowering=False)
        self.nc = nc
        self.eng = {"pe": nc.tensor, "act": nc.scalar, "dve": nc.vector, "pool": nc.gpsimd, "sp": nc.sync}
        self.psem = {}
        self.pcnt = {}
        for e in ("pe", "act", "dve", "pool"):
            self.psem[e] = [nc.alloc_semaphore(f"prog_{e}")]
            self.pcnt[e] = 0
        self.dsem = [nc.alloc_semaphore(f"dma_{i}") for i in range(self.NDMA)]
        self.dcnt = [0] * self.NDMA
        self.dnext = 0
        self.seen = {e: {} for e in self.eng}
        self.buf = {}
        self.out_toks = []
        self._ctx = []

    def dram(self, name, shape, dt=F32, kind="ExternalInput"):
        return self.nc.dram_tensor(name, list(shape), dt, kind=kind).ap()

    def sb(self, name, shape, dt=F32):
        g = self.nc.sbuf_tensor("sb_" + name, list(shape), dt)
        t = g.__enter__()
        self._ctx.append(g)
        return t

    def ps(self, name, shape, dt=F32):
        g = self.nc.psum_tensor("pp_" + name, list(shape), dt)
        t = g.__enter__()
        self._ctx.append(g)
        return t

    def _wait(self, e, toks, raw_keys_same_engine=True):
        eng = self.eng[e]
        best = {}
        for t in toks:
            if t is None:
                continue
            sem, val, prod = t
            k = id(sem)
            if self.seen[e].get(k, 0) >= val:
                continue
            if k not in best or best[k][1] < val:
                best[k] = (sem, val)
        for k, (sem, val) in best.items():
            eng.wait_ge(sem, val)
            self.seen[e][k] = val

    def _deps(self, e, r, w):
        toks = []
        for k in r:
            b = self.buf.get(k)
            if b and b["w"] is not None:
                toks.append(b["w"])
        for k in w:
            b = self.buf.get(k)
            if b:
                if b["w"] is not None and (e == "dma" or b["w"][2] != e):
                    toks.append(b["w"])
                for pe_, t in b["r"].items():
                    if e == "dma" or isinstance(pe_, tuple) or pe_ != e:
                        toks.append(t)
        return toks

    def _record(self, tok, r, w):
        e = tok[2]
        for k in r:
            b = self.buf.setdefault(k, {"w": None, "r": {}})
            b["r"][e if e != "dma" else ("dma", id(tok[0]))] = tok
        for k in w:
            self.buf[k] = {"w": tok, "r": {}}

    def op(self, e, fn, r=(), w=()):
        self._wait(e, self._deps(e, r, w))
        ins = fn()
        self.pcnt[e] += 1
        sem = self.psem[e][-1]
        ins.then_inc(sem, 1)
        tok = (sem, self.pcnt[e], e)
        self._record(tok, r, w)
        return tok

    def dma(self, e, out, in_, r=(), w=(), is_output=False, **kw):
        slot = self.dnext
        self.dnext = (self.dnext + 1) % self.NDMA
        sem = self.dsem[slot]
        toks = self._deps("dma", r, w)
        if self.dcnt[slot] > 0:
            toks.append((sem, self.dcnt[slot], "dma"))
        self._wait(e, toks)
        ins = self.eng[e].dma_start(out=out, in_=in_, **kw)
        self.dcnt[slot] += 16
        ins.then_inc(sem, 16)
        tok = (sem, self.dcnt[slot], "dma")
        self._record(tok, r, w)
        if is_output:
            self.out_toks.append(tok)
        return tok

    def finish(self):
        toks = list(self.out_toks)
        for i, s in enumerate(self.dsem):
            if self.dcnt[i] > 0:
                toks.append((s, self.dcnt[i], "dma"))
        self._wait("sp", toks)
        toks = []
        for e in ("pe", "act", "dve", "pool"):
            if self.pcnt[e] > 0:
                toks.append((self.psem[e][-1], self.pcnt[e], e))
        self._wait("sp", toks)
        for g in reversed(self._ctx):
            g.__exit__(None, None, None)
        self._ctx = []
        return self.nc


def run(nc, in_maps, trace=False):
    res = run_bass_kernel_spmd(nc, in_maps, core_ids=list(range(len(in_maps))), trace=trace)
    return res


D = 1024
NIN = 2582
ALPHA = 8 ** 0.25


def build_M():
    kb = KB()
    nc = kb.nc
    cT = kb.dram("cT", [128, 8, 2])
    Wd = kb.dram("W", [128, 8, 3072])
    bd = kb.dram("bias", [128, 24])
    od = kb.dram("modT", [128, 24, 2], kind="ExternalOutput")
    ct = kb.sb("ct", [128, 8, 2])
    cs = kb.sb("cs", [128, 8, 2])
    Wt = kb.sb("Wt", [128, 8, 3072])
    bt = kb.sb("bt", [128, 24])
    ot = kb.sb("ot", [128, 24, 2])
    ps = kb.ps("ps", [128, 512])
    kb.dma("sp", ct[:], cT[:, :, :], w=["ct"])
    kb.dma("sp", bt[:], bd[:, :], w=["bt"])
    for k in range(8):
        kb.dma("sp", Wt[:, k, :], Wd[:, k, :], w=[f"W{k}"])
    kb.op("act", lambda: nc.scalar.activation(out=cs[:], in_=ct[:], func=AF.Silu), r=["ct"], w=["cs"])
    for j in range(24):
        def mm(j=j):
            for k in range(8):
                ins = nc.tensor.matmul(ps[:, 2 * j:2 * j + 2], Wt[:, k, j * 128:(j + 1) * 128], cs[:, k, :],
                                       start=(k == 0), stop=(k == 7))
            return ins
        kb.op("pe", mm, r=["cs"] + [f"W{k}" for k in range(8)], w=["ps"])
    for b in range(2):
        kb.op("dve", lambda b=b: nc.vector.tensor_tensor(out=ot[:, :, b], in0=ps[:, b:48:2], in1=bt[:], op=ALU.add),
              r=["ps", "bt"], w=[f"ot{b}"])
    kb.dma("sp", od[:, :, :], ot[:], r=["ot0", "ot1"], is_output=True)
    return kb.finish()


def host_M(I):
    Wall = np.concatenate([I['w_ada'][l] for l in range(4)], axis=1)
    ball = np.concatenate([I['b_ada'][l] for l in range(4)], axis=0)
    cT = np.ascontiguousarray(I['c'].T.reshape(8, 128, 2).transpose(1, 0, 2))
    maps = []
    for r in range(8):
        W = np.ascontiguousarray(Wall[:, r * 3072:(r + 1) * 3072].reshape(8, 128, 3072).transpose(1, 0, 2))
        bb = np.ascontiguousarray(ball[r * 3072:(r + 1) * 3072].reshape(24, 128).T)
        maps.append({"cT": cT, "W": W, "bias": bb})
    return maps


def post_M(res):
    cols = []
    for r in range(8):
        m = res.results[r]["modT"]
        cols.append(m.transpose(2, 1, 0).reshape(2, 3072))
    allm = np.concatenate(cols, axis=1)
    return allm.reshape(2, 4, 6144).transpose(1, 0, 2)


GROUPS = [(0, 512), (512, 512), (1024, 512), (1536, 512), (2048, 512), (2560, 22)]


def build_A():
    kb = KB()
    nc = kb.nc
    xT = kb.dram("xT", [128, 8, 2048])
    scd = kb.dram("sc", [128, 8])
    shd = kb.dram("sh", [128, 8])
    wd = kb.dram("w", [128, 8, NIN])
    bd = kb.dram("bias", [128, NIN])
    zd = kb.dram("z", [2048, NIN], kind="ExternalOutput")
    sc = kb.sb("sc", [128, 8]); sc1 = kb.sb("sc1", [128, 8]); sh = kb.sb("sh", [128, 8])
    wt = kb.sb("wt", [128, 8, NIN], F32R)
    bt = kb.sb("bt", [128, NIN])
    xb = [kb.sb(f"xb{i}", [128, 8, 512]) for i in range(2)]
    hT = [kb.sb(f"hT{i}", [128, 8, 512], F32R) for i in range(2)]
    zt = [kb.sb(f"zt{i}", [128, NIN]) for i in range(2)]
    pss = [kb.ps(f"ps{i}", [128, 512]) for i in range(4)]
    kb.dma("sp", sc[:], scd[:, :], w=["sc"])
    kb.dma("sp", sh[:], shd[:, :], w=["sh"])
    kb.dma("sp", bt[:], bd[:, :], w=["bt"])
    for k in range(8):
        for (c0, n) in ((0, 1291), (1291, 1291)):
            kb.dma("pool", wt[:, k, c0:c0 + n], wd[:, k, c0:c0 + n], w=[f"w{k}_{c0}"])
    wkeys = [f"w{k}_{c0}" for k in range(8) for c0 in (0, 1291)]
    kb.op("dve", lambda: nc.vector.tensor_scalar_add(out=sc1[:], in0=sc[:], scalar1=1.0), r=["sc"], w=["sc1"])
    pcnt = 0
    for tb in range(4):
        x_ = xb[tb % 2]; h_ = hT[tb % 2]
        kb.dma("sp", x_[:], xT[:, :, tb * 512:(tb + 1) * 512], w=[f"xb{tb%2}"])
        for k in range(8):
            kb.op("act", lambda k=k: nc.scalar.activation(out=h_[:, k, :], in_=x_[:, k, :], func=AF.Identity,
                                                          scale=sc1[:, k:k + 1], bias=sh[:, k:k + 1]),
                  r=[f"xb{tb%2}", "sc1", "sh"], w=[f"hT{tb%2}_{k}"])
        for ti in range(4):
            tile = tb * 4 + ti
            z_ = zt[tile % 2]
            for gi, (c0, n) in enumerate(GROUPS):
                p_ = pss[pcnt % 4]; pk = f"ps{pcnt%4}"; pcnt += 1
                def mm(p_=p_, c0=c0, n=n, ti=ti):
                    for k in range(8):
                        ins = nc.tensor.matmul(p_[:, 0:n], h_[:, k, ti * 128:(ti + 1) * 128], wt[:, k, c0:c0 + n],
                                               start=(k == 0), stop=(k == 7))
                    return ins
                kb.op("pe", mm, r=[f"hT{tb%2}_{k}" for k in range(8)] + wkeys, w=[pk])
                kb.op("dve", lambda p_=p_, c0=c0, n=n: nc.vector.tensor_tensor(out=z_[:, c0:c0 + n], in0=p_[:, 0:n],
                                                                               in1=bt[:, c0:c0 + n], op=ALU.add),
                      r=[pk, "bt"], w=[f"zt{tile%2}_{gi}"])
            kb.dma("sp", zd[tile * 128:(tile + 1) * 128, :], z_[:], r=[f"zt{tile%2}_{gi}" for gi in range(6)],
                   is_output=True)
    return kb.finish()


def cols128(v):
    return np.ascontiguousarray(v.reshape(8, 128).T)


def host_A(I, l, x, mods):
    maps = []
    w = np.ascontiguousarray(I['w_in'][l].reshape(8, 128, NIN).transpose(1, 0, 2))
    bias = np.ascontiguousarray(np.broadcast_to(I['b_in'][l][None, :], (128, NIN)))
    for r in range(8):
        b, j = r // 4, r % 4
        xs = x[b, j * 2048:(j + 1) * 2048, :]
        xT = np.ascontiguousarray(xs.T.reshape(8, 128, 2048).transpose(1, 0, 2))
        maps.append({"xT": xT, "sc": cols128(mods[l, b, 1024:2048]), "sh": cols128(mods[l, b, 0:1024]),
                     "w": w, "bias": bias})
    return maps


def post_A(res):
    z = np.stack([np.concatenate([res.results[b * 4 + j]["z"] for j in range(4)], axis=0) for b in range(2)])
    return z


def fox_consts():
    s = np.arange(128)[:, None]; m = np.arange(128)[None, :]
    tri = (s <= m).astype(np.float32)
    ones = np.ones((128, 128), np.float32)
    ident = np.eye(128, dtype=np.float32)
    t = np.arange(512)[None, None, :]; d = np.arange(4)[None, :, None]; ss = np.arange(128)[:, None, None]
    maskadd = np.where(128 * d + ss <= t, 0.0, -30000.0).astype(np.float32)
    return {"tri": tri, "ones": ones, "ident": ident, "maskadd": np.ascontiguousarray(maskadd)}


def build_B():
    kb = KB()
    nc = kb.nc
    S = 8192
    qd = kb.dram("qT", [64, S]); kd = kb.dram("kT", [64, S]); vd = kb.dram("v", [128, 64, 64]); fd = kb.dram("ff", [128, 64])
    trid = kb.dram("tri", [128, 128]); onesd = kb.dram("ones", [128, 128]); identd = kb.dram("ident", [128, 128])
    maskd = kb.dram("maskadd", [128, 4, 512])
    od = kb.dram("oT", [64, S], kind="ExternalOutput")
    qa = kb.sb("qa", [66, S], F32R); ka = kb.sb("ka", [66, S], F32R); va = kb.sb("va", [128, 64, 65], F32R)
    stg = kb.sb("stg", [128, 4224])
    tri = kb.sb("tri", [128, 128]); ones = kb.sb("ones", [128, 128]); ident = kb.sb("ident", [128, 128])
    identR = kb.sb("identR", [128, 128], F32R)
    maskadd = kb.sb("maskadd", [128, 4, 512])
    ff = kb.sb("ff", [128, 64]); sg = kb.sb("sg", [128, 64]); ls = kb.sb("ls", [128, 64])
    tot = kb.sb("tot", [128, 64]); incl = kb.sb("incl", [128, 64]); tmpc = kb.sb("tmpc", [128, 64])
    Fc = kb.sb("Fc", [128, 64]); negF = kb.sb("negF", [128, 64]); F8 = kb.sb("F8", [128, 64]); lo8 = kb.sb("lo8", [128, 64])
    X = kb.sb("X", [128, 64, 66], F32R)
    P = [kb.sb(f"P{i}", [128, 512], F32R) for i in range(4)]
    tmpd = [kb.sb(f"tmpd{i}", [128, 512]) for i in range(2)]
    Osb = [kb.sb(f"Osb{i}", [65, 512]) for i in range(2)]
    rec = [kb.sb(f"rec{i}", [65, 512]) for i in range(2)]
    ot = [kb.sb(f"ot{i}", [65, 512]) for i in range(2)]
    psS = [kb.ps(f"psS{i}", [128, 512]) for i in range(4)]
    psO = [kb.ps(f"psO{i}", [128, 512]) for i in range(2)]
    psM = [kb.ps(f"psM{i}", [128, 512]) for i in range(2)]

    for (t_, d_, k_) in ((tri, trid, "tri"), (ones, onesd, "ones"), (ident, identd, "ident")):
        kb.dma("sp", t_[:], d_[:, :], w=[k_])
    kb.dma("sp", maskadd[:], maskd[:, :, :], w=["maskadd"])
    kb.dma("sp", ff[:], fd[:, :], w=["ff"])
    kb.op("dve", lambda: nc.vector.tensor_copy(out=identR[:], in_=ident[:]), r=["ident"], w=["identR"])
    kb.op("pool", lambda: nc.gpsimd.memset(stg[:], 1.0), w=["stg"])
    for hf in range(2):
        kb.op("dve", lambda hf=hf: nc.vector.tensor_copy(out=ka[64:66, hf * 4096:(hf + 1) * 4096], in_=stg[64:66, 0:4096]), r=["stg"], w=["ka_hi"])
    kb.op("dve", lambda: nc.vector.tensor_copy(out=va[:, :, 0], in_=stg[:, 0:64]), r=["stg"], w=["va_1"])
    kb.op("pool", lambda: nc.gpsimd.memset(stg[:], 0.0), r=[], w=["stg"])
    kb.op("dve", lambda: nc.vector.tensor_copy(out=X[:].rearrange("p t c -> p (t c)"), in_=stg[:, 0:4224]), r=["stg"], w=["X"])
    for nm, src, dst in (("q", qd, qa), ("k", kd, ka)):
        for hf in range(2):
            kb.dma("sp", stg[0:64, 0:4096], src[:, hf * 4096:(hf + 1) * 4096], w=["stg"])
            kb.op("dve", lambda dst=dst, hf=hf: nc.vector.tensor_copy(out=dst[0:64, hf * 4096:(hf + 1) * 4096], in_=stg[0:64, 0:4096]),
                  r=["stg"], w=[f"{nm}a_lo"])
    kb.dma("sp", stg[:, 0:4096], vd.rearrange("p t d -> p (t d)"), w=["stg"])
    kb.op("dve", lambda: nc.vector.tensor_copy(out=va[:, :, 1:65], in_=stg[:, 0:4096].rearrange("p (t d) -> p t d", d=64)),
          r=["stg"], w=["va_v"])
    kb.op("act", lambda: nc.scalar.activation(out=sg[:], in_=ff[:], func=AF.Sigmoid), r=["ff"], w=["sg"])
    kb.op("act", lambda: nc.scalar.activation(out=ls[:], in_=sg[:], func=AF.Ln), r=["sg"], w=["ls"])
    def mmc():
        nc.tensor.matmul(psM[0][:, 0:64], tri[:], ls[:], start=True, stop=True)
        return nc.tensor.matmul(psM[0][:, 64:128], ones[:], ls[:], start=True, stop=True)
    kb.op("pe", mmc, r=["tri", "ones", "ls"], w=["psM0"])
    kb.op("dve", lambda: nc.vector.tensor_copy(out=tot[:], in_=psM[0][:, 64:128]), r=["psM0"], w=["tot"])
    kb.op("dve", lambda: nc.vector.tensor_tensor_scan(out=incl[:], data0=ones[:, 0:64], data1=tot[:], initial=0.0,
                                                      op0=ALU.mult, op1=ALU.add), r=["tot", "ones"], w=["incl"])
    kb.op("dve", lambda: nc.vector.tensor_tensor(out=tmpc[:], in0=incl[:], in1=tot[:], op=ALU.subtract), r=["incl", "tot"], w=["tmpc"])
    kb.op("dve", lambda: nc.vector.tensor_tensor(out=Fc[:], in0=psM[0][:, 0:64], in1=tmpc[:], op=ALU.add), r=["psM0", "tmpc"], w=["Fc"])
    kb.op("dve", lambda: nc.vector.tensor_scalar_mul(out=negF[:], in0=Fc[:], scalar1=-1.0), r=["Fc"], w=["negF"])
    kb.op("dve", lambda: nc.vector.tensor_scalar_mul(out=F8[:], in0=Fc[:], scalar1=8.0), r=["Fc"], w=["F8"])
    kb.op("dve", lambda: nc.vector.tensor_copy(out=X[:, :, 64], in_=F8[:]), r=["F8", "X"], w=["Xhi"])
    kb.op("dve", lambda: nc.vector.tensor_tensor(out=lo8[:], in0=F8[:], in1=X[:, :, 64].bitcast(F32), op=ALU.subtract),
          r=["F8", "Xhi"], w=["lo8"])
    kb.op("dve", lambda: nc.vector.tensor_copy(out=X[:, :, 65], in_=lo8[:]), r=["lo8", "X"], w=["Xlo"])
    for blk in range(16):
        pm = psM[1]
        def mmx(blk=blk):
            for i in range(4):
                t = blk * 4 + i
                ins = nc.tensor.matmul(pm[0:66, i * 128:(i + 1) * 128], X[:, t, :], identR[:], start=True, stop=True)
            return ins
        kb.op("pe", mmx, r=["X", "Xhi", "Xlo", "identR"], w=["psM1"])
        kb.op("act", lambda blk=blk: nc.scalar.activation(out=qa[64:66, blk * 512:(blk + 1) * 512], in_=pm[64:66, :], func=AF.Copy),
              r=["psM1"], w=[f"qa_hi{blk}"])
    sc = 0; pc = 0; dc = 0
    for qb in range(16):
        nk = 4 * qb + 4
        pO = psO[qb % 2]; pOk = f"psO{qb%2}"
        for kt in range(nk):
            pS = psS[sc % 4]; pSk = f"psS{sc%4}"; sc += 1
            P_ = P[pc % 4]; Pk = f"P{pc%4}"; pc += 1
            kb.op("pe", lambda pS=pS, kt=kt, qb=qb: nc.tensor.matmul(pS[:], ka[:, kt * 128:(kt + 1) * 128], qa[:, qb * 512:(qb + 1) * 512],
                                                                    start=True, stop=True),
                  r=["qa_lo", "ka_lo", "ka_hi", f"qa_hi{qb}"], w=[pSk])
            d = kt - 4 * qb
            if d >= 0:
                td = tmpd[dc % 2]; tdk = f"tmpd{dc%2}"; dc += 1
                kb.op("dve", lambda pS=pS, td=td, d=d: nc.vector.scalar_tensor_tensor(out=td[:], in0=pS[:], scalar=0.125, in1=maskadd[:, d, :],
                                                                                  op0=ALU.mult, op1=ALU.add),
                      r=[pSk, "maskadd"], w=[tdk])
                kb.op("act", lambda td=td, P_=P_, kt=kt: nc.scalar.activation(out=P_[:], in_=td[:], func=AF.Exp, bias=negF[:, kt:kt + 1], scale=1.0),
                      r=[tdk, "negF"], w=[Pk])
            else:
                kb.op("act", lambda pS=pS, P_=P_, kt=kt: nc.scalar.activation(out=P_[:], in_=pS[:], func=AF.Exp, bias=negF[:, kt:kt + 1], scale=0.125),
                      r=[pSk, "negF"], w=[Pk])
            kb.op("pe", lambda pO=pO, P_=P_, kt=kt, nk=nk: nc.tensor.matmul(pO[0:65, :], va[:, kt, :], P_[:], start=(kt == 0), stop=(kt == nk - 1)),
                  r=[Pk, "va_v", "va_1"], w=[pOk])
        O_ = Osb[qb % 2]; Ok = f"Osb{qb%2}"
        kb.op("dve", lambda pO=pO, O_=O_: nc.vector.tensor_copy(out=O_[:], in_=pO[0:65, :]), r=[pOk], w=[Ok])
        kb.op("pe", lambda O_=O_: nc.tensor.matmul(psM[0][0:65, :], ones[0:1, 0:65], O_[0:1, :], start=True, stop=True),
              r=[Ok, "ones"], w=["psM0"])
        r_ = rec[qb % 2]; rk = f"rec{qb%2}"
        kb.op("dve", lambda r_=r_: nc.vector.reciprocal(out=r_[:], in_=psM[0][0:65, :]), r=["psM0"], w=[rk])
        o_ = ot[qb % 2]; ok = f"ot{qb%2}"
        kb.op("pool", lambda o_=o_, O_=O_, r_=r_: nc.gpsimd.tensor_tensor(out=o_[:], in0=O_[:], in1=r_[:], op=ALU.mult), r=[Ok, rk], w=[ok])
        kb.dma("sp", od[:, qb * 512:(qb + 1) * 512], o_[1:65, :], r=[ok], is_output=True)
    return kb.finish()


def host_B(z, pairs):
    C = fox_consts()
    maps = []
    for (b, h) in pairs:
        q = z[b, :, h * 64:(h + 1) * 64]; k = z[b, :, 384 + h * 64:384 + (h + 1) * 64]; v = z[b, :, 768 + h * 64:768 + (h + 1) * 64]
        ff = z[b, :, 1152 + h]
        m = {"qT": np.ascontiguousarray(q.T), "kT": np.ascontiguousarray(k.T),
             "v": np.ascontiguousarray(v.reshape(64, 128, 64).transpose(1, 0, 2)),
             "ff": np.ascontiguousarray(ff.reshape(64, 128).T)}
        m.update(C)
        maps.append(m)
    return maps


def gla_consts(g):
    w = (2, 4, 8, 16)[g]
    s = np.arange(128)[:, None]; t = np.arange(128)[None, :]
    tri = (s <= t).astype(np.float32)
    triC = (s > t).astype(np.float32)
    eye = np.eye(128, dtype=np.float32)
    bandCur = np.where((s <= t) & (s >= t - w + 1), 1.0 / w, 0.0).astype(np.float32) - eye
    bandPrev = np.where(s >= 128 + t - w + 1, 1.0 / w, 0.0).astype(np.float32)
    cnt = np.minimum(t + 1, w).astype(np.float32)
    bandCur0 = np.where((s <= t) & (s >= t - w + 1), 1.0 / cnt, 0.0).astype(np.float32) - eye
    return {"tri": tri, "triC": triC, "bandCur": bandCur, "bandPrev": bandPrev, "bandCur0": bandCur0}


def build_G():
    kb = KB()
    nc = kb.nc
    S = 8192
    d_ga = kb.dram("ga1T", [17, S]); d_wa = kb.dram("wa2", [17, 48])
    d_qT = kb.dram("gqT", [48, S]); d_kT = kb.dram("gkT", [48, S]); d_k = kb.dram("gk", [128, 64, 48])
    d_v = kb.dram("gv", [128, 64, 96]); d_gr = kb.dram("gr", [128, 64, 96]); d_ng = kb.dram("normg", [128, 96])
    d_tri = kb.dram("tri", [128, 128]); d_triC = kb.dram("triC", [128, 128])
    d_u = kb.dram("pu", [128, 64, 64]); d_bc = kb.dram("bandCur", [128, 128]); d_bp = kb.dram("bandPrev", [128, 128])
    d_bc0 = kb.dram("bandCur0", [128, 128]); d_pw = kb.dram("poolw", [64, 64]); d_psc = kb.dram("pscale", [64, 1])
    d_og = kb.dram("og", [128, 64, 96], kind="ExternalOutput"); d_op = kb.dram("opT", [64, S], kind="ExternalOutput")

    gaB = [kb.sb(f"ga{i}", [17, 2048]) for i in range(2)]; wa = kb.sb("wa", [17, 48])
    qTB = [kb.sb(f"qT{i}", [48, 2048]) for i in range(2)]; kTB = [kb.sb(f"kT{i}", [48, 2048]) for i in range(2)]
    k = kb.sb("k", [128, 64, 48])
    v = kb.sb("v", [128, 64, 96]); gr = kb.sb("gr", [128, 64, 96]); ng = kb.sb("ng", [128, 96])
    tri = kb.sb("tri", [128, 128]); triC = kb.sb("triC", [128, 128])
    u = kb.sb("u", [128, 64, 64]); bc = kb.sb("bc", [128, 128]); bp = kb.sb("bp", [128, 128]); bc0 = kb.sb("bc0", [128, 128])
    pw = kb.sb("pw", [64, 64]); psc = kb.sb("psc", [64, 1])
    la = kb.sb("la", [128, 64, 48]); ogB = [kb.sb(f"og{i}", [128, 16, 96]) for i in range(2)]
    for (t_, d_, k_) in ((wa, d_wa, "wa"), (ng, d_ng, "ng"),
                         (tri, d_tri, "tri"), (triC, d_triC, "triC"), (bc, d_bc, "bc"), (bp, d_bp, "bp"), (bc0, d_bc0, "bc0"),
                         (pw, d_pw, "pw"), (psc, d_psc, "psc")):
        kb.dma("sp", t_[:], d_[:, :], w=[k_])
    for (t_, d_, k_) in ((k, d_k, "k"), (v, d_v, "v"), (gr, d_gr, "gr"), (u, d_u, "u")):
        kb.dma("sp", t_[:], d_[:, :, :], w=[k_])
    pss = [kb.ps(f"ps{i}", [128, 512]) for i in range(8)]
    for grp in range(16):
        p_ = pss[6 + grp % 2]; pk = f"ps{6 + grp % 2}"
        q4 = grp // 4; ga = gaB[q4 % 2]
        if grp % 4 == 0:
            kb.dma("sp", ga[:], d_ga[:, q4 * 2048:(q4 + 1) * 2048], w=[f"ga{q4%2}"])
        def mm(grp=grp, p_=p_, ga=ga):
            for i in range(4):
                c = (grp % 4) * 4 + i
                ins = nc.tensor.matmul(p_[:, i * 48:(i + 1) * 48], ga[:, c * 128:(c + 1) * 128], wa[:], start=True, stop=True)
            return ins
        kb.op("pe", mm, r=[f"ga{q4%2}", "wa"], w=[pk])
        kb.op("act", lambda grp=grp, p_=p_: nc.scalar.activation(out=la[:, grp * 4:(grp + 1) * 4, :].rearrange("p c d -> p (c d)"),
                                                                 in_=p_[:, 0:192], func=AF.Sigmoid), r=[pk], w=[f"sg{grp}"])
    kb.op("act", lambda: nc.scalar.activation(out=la[:].rearrange("p c d -> p (c d)"), in_=la[:].rearrange("p c d -> p (c d)"), func=AF.Ln),
          r=[f"sg{g}" for g in range(16)], w=["la"])
    kb.op("act", lambda: nc.scalar.activation(out=gr[:].rearrange("p c d -> p (c d)"), in_=gr[:].rearrange("p c d -> p (c d)"), func=AF.Silu),
          r=["gr"], w=["gr"])
    for c4 in range(4):
        kb.op("pool", lambda c4=c4: nc.gpsimd.tensor_tensor(out=gr[:, c4 * 16:(c4 + 1) * 16, :], in0=gr[:, c4 * 16:(c4 + 1) * 16, :],
                                                              in1=ng[:, None, :].to_broadcast([128, 16, 96]), op=ALU.mult),
              r=["gr", "ng"], w=["gr"])
    pooled = [kb.sb(f"pooled{i}", [64, 512]) for i in range(2)]
    opt = [kb.sb(f"opt{i}", [64, 512]) for i in range(2)]
    for blk in range(16):
        pp = pss[4]; pm = pss[5]
        def mmp(blk=blk):
            for i in range(4):
                t = blk * 4 + i
                o_ = pp[0:64, i * 128:(i + 1) * 128]
                if t == 0:
                    ins = nc.tensor.matmul(o_, u[:, 0, :], bc0[:], start=True, stop=True)
                else:
                    nc.tensor.matmul(o_, u[:, t, :], bc[:], start=True, stop=False)
                    ins = nc.tensor.matmul(o_, u[:, t - 1, :], bp[:], start=False, stop=True)
            return ins
        kb.op("pe", mmp, r=["u", "bc", "bp", "bc0"], w=["ps4"])
        pl = pooled[blk % 2]; plk = f"pooled{blk%2}"
        kb.op("act", lambda pl=pl: nc.scalar.activation(out=pl[:], in_=pp[0:64, :], func=AF.Copy), r=["ps4"], w=[plk])
        kb.op("pe", lambda pl=pl: nc.tensor.matmul(pm[0:64, :], pw[:], pl[:], start=True, stop=True), r=[plk, "pw"], w=["ps5"])
        o_ = opt[blk % 2]; ok = f"opt{blk%2}"
        kb.op("dve", lambda o_=o_: nc.vector.tensor_scalar(out=o_[:], in0=pm[0:64, :], scalar1=psc[:, 0:1], scalar2=None, op0=ALU.mult),
              r=["ps5", "psc"], w=[ok])
        kb.dma("sp", d_op[:, blk * 512:(blk + 1) * 512], o_[:], r=[ok], is_output=True)
    st = [kb.sb(f"st{i}", [48, 96]) for i in range(2)]
    eb = [kb.sb(f"eb{i}", [48, 128]) for i in range(2)]; enb = [kb.sb(f"enb{i}", [48, 128]) for i in range(2)]
    ebl = [kb.sb(f"ebl{i}", [128, 48]) for i in range(2)]
    qi = [kb.sb(f"qi{i}", [48, 128]) for i in range(2)]; ki = [kb.sb(f"ki{i}", [48, 128]) for i in range(2)]
    ko = [kb.sb(f"ko{i}", [128, 48]) for i in range(2)]; at = [kb.sb(f"at{i}", [128, 128]) for i in range(2)]
    osb = [kb.sb(f"osb{i}", [128, 96]) for i in range(2)]; junk = [kb.sb(f"junk{i}", [128, 96]) for i in range(2)]
    ss = [kb.sb(f"ss{i}", [128, 1]) for i in range(2)]; rs = [kb.sb(f"rs{i}", [128, 1]) for i in range(2)]
    kb.op("dve", lambda: nc.vector.memset(st[0][:], 0.0), w=["st0"])
    SC = 1.0 / 16.0
    for c in range(64):
        p = c % 2
        pA = pss[2 * p]; pAk = f"ps{2*p}"; pO = pss[2 * p + 1]; pOk = f"ps{2*p+1}"
        q4 = c // 16; qT = qTB[q4 % 2]; kT = kTB[q4 % 2]; og = ogB[q4 % 2]
        qTk = f"qT{q4%2}"; kTk = f"kT{q4%2}"
        if c % 16 == 0:
            kb.dma("sp", qT[:], d_qT[:, q4 * 2048:(q4 + 1) * 2048], w=[qTk])
            kb.dma("sp", kT[:], d_kT[:, q4 * 2048:(q4 + 1) * 2048], w=[kTk])
        cs = slice((c % 16) * 128, (c % 16 + 1) * 128)
        def mmb(c=c, pA=pA):
            nc.tensor.matmul(pA[0:48, 0:128], la[:, c, :], tri[:], start=True, stop=True)
            return nc.tensor.matmul(pA[:, 128:176], triC[:], la[:, c, :], start=True, stop=True)
        kb.op("pe", mmb, r=["la", "tri", "triC"], w=[pAk + "b"])
        kb.op("act", lambda: nc.scalar.activation(out=eb[p][:], in_=pA[0:48, 0:128], func=AF.Exp, scale=SC), r=[pAk + "b"], w=[f"eb{p}"])
        kb.op("act", lambda: nc.scalar.activation(out=enb[p][:], in_=pA[0:48, 0:128], func=AF.Exp, scale=-SC), r=[pAk + "b"], w=[f"enb{p}"])
        kb.op("act", lambda: nc.scalar.activation(out=ebl[p][:], in_=pA[:, 128:176], func=AF.Exp, scale=SC), r=[pAk + "b"], w=[f"ebl{p}"])
        kb.op("dve", lambda cs=cs: nc.vector.scalar_tensor_tensor(out=qi[p][:], in0=qT[:, cs], scalar=48 ** -0.5, in1=eb[p][:],
                                                                   op0=ALU.mult, op1=ALU.mult), r=[qTk, f"eb{p}"], w=[f"qi{p}"])
        kb.op("pool", lambda cs=cs: nc.gpsimd.tensor_tensor(out=ki[p][:], in0=kT[:, cs], in1=enb[p][:], op=ALU.mult), r=[kTk, f"enb{p}"], w=[f"ki{p}"])
        kb.op("pool", lambda c=c: nc.gpsimd.tensor_tensor(out=ko[p][:], in0=k[:, c, :], in1=ebl[p][:], op=ALU.mult), r=["k", f"ebl{p}"], w=[f"ko{p}"])
        kb.op("pe", lambda: nc.tensor.matmul(pA[:, 384:512], ki[p][:], qi[p][:], start=True, stop=True), r=[f"ki{p}", f"qi{p}"], w=[pAk + "a"])
        kb.op("dve", lambda: nc.vector.tensor_tensor(out=at[p][:], in0=pA[:, 384:512], in1=tri[:], op=ALU.mult), r=[pAk + "a", "tri"], w=[f"at{p}"])
        sin = st[c % 2]; sout = st[(c + 1) % 2]
        def mmo(c=c, pO=pO, sin=sin):
            nc.tensor.matmul(pO[:, 0:96], at[p][:], v[:, c, :], start=True, stop=False)
            return nc.tensor.matmul(pO[:, 0:96], qi[p][:], sin[:], start=False, stop=True)
        kb.op("pe", mmo, r=[f"at{p}", "v", f"qi{p}", f"st{c%2}"], w=[pOk])
        kb.op("pe", lambda c=c: nc.tensor.matmul(pA[0:48, 256:352], ko[p][:], v[:, c, :], start=True, stop=True), r=[f"ko{p}", "v"], w=[pAk + "k"])
        kb.op("dve", lambda sin=sin, sout=sout: nc.vector.scalar_tensor_tensor(out=sout[:], in0=sin[:], scalar=eb[p][:, 127:128], in1=pA[0:48, 256:352],
                                                                             op0=ALU.mult, op1=ALU.add),
              r=[f"st{c%2}", f"eb{p}", pAk + "k"], w=[f"st{(c+1)%2}"])
        kb.op("act", lambda: nc.scalar.activation(out=junk[p][:], in_=pO[:, 0:96], func=AF.Square, accum_out=ss[p][:]), r=[pOk], w=[f"ss{p}", f"junk{p}"])
        kb.op("dve", lambda: nc.vector.tensor_scalar(out=rs[p][:], in0=ss[p][:], scalar1=1.0 / 96.0, scalar2=1e-6, op0=ALU.mult, op1=ALU.add),
              r=[f"ss{p}"], w=[f"rs{p}a"])
        kb.op("act", lambda: nc.scalar.activation(out=ss[p][:], in_=rs[p][:], func=AF.Ln), r=[f"rs{p}a"], w=[f"ss{p}"])
        kb.op("act", lambda: nc.scalar.activation(out=rs[p][:], in_=ss[p][:], func=AF.Exp, scale=-0.5), r=[f"ss{p}"], w=[f"rs{p}"])
        kb.op("dve", lambda c=c, og=og: nc.vector.scalar_tensor_tensor(out=og[:, c % 16, :], in0=pO[:, 0:96], scalar=rs[p][:, 0:1], in1=gr[:, c, :],
                                                                op0=ALU.mult, op1=ALU.mult), r=[pOk, f"rs{p}", "gr"], w=[f"og{q4%2}_{c%16}"])
        if c % 16 == 15:
            kb.dma("sp", d_og[:, q4 * 16:(q4 + 1) * 16, :], og[:], r=[f"og{q4%2}_{i}" for i in range(16)], is_output=True)
    return kb.finish()


def tok_tiles(a):
    return np.ascontiguousarray(a.reshape(64, 128, -1).transpose(1, 0, 2))


def host_G(I, l, z):
    maps = []
    for r in range(8):
        b, h = r // 4, r % 4
        zz = z[b]
        gq = zz[:, 1158 + 48 * h:1158 + 48 * (h + 1)]; gk = zz[:, 1350 + 48 * h:1350 + 48 * (h + 1)]
        gv = zz[:, 1542 + 96 * h:1542 + 96 * (h + 1)]; grr = zz[:, 1926 + 96 * h:1926 + 96 * (h + 1)]
        ga1 = zz[:, 2310:2326]; pu = zz[:, 2326 + 64 * h:2326 + 64 * (h + 1)]
        m = {"ga1T": np.ascontiguousarray(np.concatenate([ga1.T, np.ones((1, 8192), np.float32)], axis=0)),
             "wa2": np.ascontiguousarray(np.concatenate([I['gla_w_a2'][l][:, 48 * h:48 * (h + 1)], I['gla_b_a'][l][None, 48 * h:48 * (h + 1)]], axis=0)),
             "gqT": np.ascontiguousarray(gq.T), "gkT": np.ascontiguousarray(gk.T), "gk": tok_tiles(gk), "gv": tok_tiles(gv), "gr": tok_tiles(grr),
             "normg": np.ascontiguousarray(np.broadcast_to(I['gla_norm_g'][l][None, 96 * h:96 * (h + 1)], (128, 96))),
             "pu": tok_tiles(pu), "poolw": np.ascontiguousarray(I['pool_w'][l][h]),
             "pscale": np.ascontiguousarray(I['pool_scale'][l][64 * h:64 * (h + 1), None])}
        m.update(gla_consts(h))
        maps.append(m)
    return maps


def emit_ln(kb, nc, u, uk, xn, xnk, scr, pfx):
    st, mv, lv, rstd, nmr = scr["st"], scr["mv"], scr["lv"], scr["rstd"], scr["nmr"]
    kb.op("dve", lambda: nc.vector.bn_stats(out=st[:, 0, :], in_=u[:, 0:512]), r=[uk], w=[pfx + "st0"])
    kb.op("dve", lambda: nc.vector.bn_stats(out=st[:, 1, :], in_=u[:, 512:1024]), r=[uk], w=[pfx + "st1"])
    kb.op("dve", lambda: nc.vector.bn_aggr(out=mv[:], in_=st[:].rearrange("p a b -> p (a b)")), r=[pfx + "st0", pfx + "st1"], w=[pfx + "mv"])
    kb.op("act", lambda: nc.scalar.activation(out=lv[:], in_=mv[:, 1:2], func=AF.Ln, bias=scr["eps"][:, 0:1], scale=1.0), r=[pfx + "mv", "eps"], w=[pfx + "lv"])
    kb.op("act", lambda: nc.scalar.activation(out=rstd[:], in_=lv[:], func=AF.Exp, scale=-0.5), r=[pfx + "lv"], w=[pfx + "rstd"])
    kb.op("dve", lambda: nc.vector.scalar_tensor_tensor(out=nmr[:], in0=mv[:, 0:1], scalar=-1.0, in1=rstd[:], op0=ALU.mult, op1=ALU.mult),
          r=[pfx + "mv", pfx + "rstd"], w=[pfx + "nmr"])
    kb.op("act", lambda: nc.scalar.activation(out=xn[:], in_=u[:], func=AF.Identity, scale=rstd[:, 0:1], bias=nmr[:, 0:1]),
          r=[uk, pfx + "rstd", pfx + "nmr"], w=[xnk])


def ln_scratch(kb, pfx):
    return {"st": kb.sb(pfx + "st", [128, 2, 6]), "mv": kb.sb(pfx + "mv", [128, 2]), "lv": kb.sb(pfx + "lv", [128, 1]),
            "rstd": kb.sb(pfx + "rstd", [128, 1]), "nmr": kb.sb(pfx + "nmr", [128, 1])}


def build_C():
    kb = KB()
    nc = kb.nc
    d_mix = kb.dram("mixT", [128, 8, 2048]); d_wo = kb.dram("wout", [128, 8, 1024]); d_x = kb.dram("x", [128, 16, 1024])
    d_rows = {n: kb.dram(n, [128, 1024]) for n in ("g1", "lng", "lnb", "s2", "t2")}
    d_wr = kb.dram("wr", [128, 8, 32]); d_br = kb.dram("br", [128, 32]); d_id = kb.dram("ident", [128, 128])
    d_x1 = kb.dram("x1", [128, 16, 1024], kind="ExternalOutput"); d_h2 = kb.dram("h2", [128, 16, 1024], kind="ExternalOutput")
    d_G = kb.dram("G", [128, 16, 32], kind="ExternalOutput")
    wo = kb.sb("wo", [128, 8, 1024], F32R)
    mix = [kb.sb(f"mix{i}", [128, 8, 512], F32R) for i in range(2)]
    rows = {n: kb.sb("r_" + n, [128, 1024]) for n in d_rows}
    wr = kb.sb("wr", [128, 8, 32]); br = kb.sb("br", [128, 32]); ident = kb.sb("ident", [128, 128])
    eps = kb.sb("eps", [128, 1])
    xt = [kb.sb(f"xt{i}", [128, 1024]) for i in range(2)]
    tmp = [kb.sb(f"tmp{i}", [128, 1024]) for i in range(2)]
    u = [kb.sb(f"u{i}", [128, 1024]) for i in range(2)]
    xn = [kb.sb(f"xn{i}", [128, 1024]) for i in range(2)]
    x1 = [kb.sb(f"x1{i}", [128, 1024]) for i in range(2)]
    h2 = [kb.sb(f"h2{i}", [128, 1024]) for i in range(2)]
    h2T = [kb.sb(f"h2T{i}", [128, 8, 128]) for i in range(2)]
    lg = [kb.sb(f"lg{i}", [128, 32]) for i in range(2)]; top8 = [kb.sb(f"top8{i}", [128, 8]) for i in range(2)]
    msk = [kb.sb(f"msk{i}", [128, 32]) for i in range(2)]; ex = [kb.sb(f"ex{i}", [128, 32]) for i in range(2)]
    nmx = [kb.sb(f"nmx{i}", [128, 1]) for i in range(2)]; den = [kb.sb(f"den{i}", [128, 1]) for i in range(2)]
    Gt = kb.sb("Gt", [128, 16, 32])
    scr = [ln_scratch(kb, f"ln{i}") for i in range(2)]
    for s_ in scr:
        s_["eps"] = eps
    pss = [kb.ps(f"ps{i}", [128, 512]) for i in range(8)]
    kb.op("pool", lambda: nc.gpsimd.memset(eps[:], 1e-5), w=["eps"])
    for k in range(8):
        kb.dma("pool", wo[:, k, :], d_wo[:, k, :], w=[f"wo{k}"])
    for n in d_rows:
        kb.dma("sp", rows[n][:], d_rows[n][:, :], w=["r_" + n])
    kb.dma("sp", wr[:], d_wr[:, :, :], w=["wr"]); kb.dma("sp", br[:], d_br[:, :], w=["br"]); kb.dma("sp", ident[:], d_id[:, :], w=["ident"])
    kb.op("dve", lambda: nc.vector.tensor_scalar_add(out=rows["g1"][:], in0=rows["g1"][:], scalar1=1.0), r=["r_g1"], w=["r_g1"])
    kb.op("dve", lambda: nc.vector.tensor_scalar_add(out=rows["s2"][:], in0=rows["s2"][:], scalar1=1.0), r=["r_s2"], w=["r_s2"])
    wok = [f"wo{k}" for k in range(8)]
    for t in range(16):
        p = t % 2; tb = t // 4; ti = t % 4
        mx = mix[tb % 2]; mxk = f"mix{tb%2}"
        if ti == 0:
            for k in range(8):
                kb.dma("pool", mx[:, k, :], d_mix[:, k, tb * 512:(tb + 1) * 512], w=[mxk + f"_{k}"])
        kb.dma("sp", xt[p][:], d_x[:, t, :], w=[f"xt{p}"])
        for half in range(2):
            ps_ = pss[2 * p + half]; pk = f"ps{2*p+half}"
            def mm(ps_=ps_, half=half, mx=mx, ti=ti):
                for k in range(8):
                    ins = nc.tensor.matmul(ps_[:], mx[:, k, ti * 128:(ti + 1) * 128], wo[:, k, half * 512:(half + 1) * 512],
                                           start=(k == 0), stop=(k == 7))
                return ins
            kb.op("pe", mm, r=[mxk + f"_{k}" for k in range(8)] + wok, w=[pk])
            hs = slice(half * 512, (half + 1) * 512)
            kb.op("dve", lambda ps_=ps_, hs=hs: nc.vector.tensor_tensor(out=tmp[p][:, hs], in0=ps_[:], in1=rows["g1"][:, hs], op=ALU.mult),
                  r=[pk, "r_g1"], w=[f"tmp{p}_{half}"])
        kb.op("dve", lambda: nc.vector.scalar_tensor_tensor(out=u[p][:], in0=xt[p][:], scalar=ALPHA, in1=tmp[p][:], op0=ALU.mult, op1=ALU.add),
              r=[f"xt{p}", f"tmp{p}_0", f"tmp{p}_1"], w=[f"u{p}"])
        emit_ln(kb, nc, u[p], f"u{p}", xn[p], f"xn{p}", scr[p], f"ln{p}")
        kb.op("dve", lambda: nc.vector.tensor_tensor(out=x1[p][:], in0=xn[p][:], in1=rows["lng"][:], op=ALU.mult), r=[f"xn{p}", "r_lng"], w=[f"x1{p}a"])
        kb.op("pool", lambda: nc.gpsimd.tensor_tensor(out=x1[p][:], in0=x1[p][:], in1=rows["lnb"][:], op=ALU.add), r=[f"x1{p}a", "r_lnb"], w=[f"x1{p}"])
        kb.dma("sp", d_x1[:, t, :], x1[p][:], r=[f"x1{p}"], is_output=True)
        kb.op("dve", lambda: nc.vector.tensor_tensor(out=h2[p][:], in0=x1[p][:], in1=rows["s2"][:], op=ALU.mult), r=[f"x1{p}", "r_s2"], w=[f"h2{p}a"])
        kb.op("pool", lambda: nc.gpsimd.tensor_tensor(out=h2[p][:], in0=h2[p][:], in1=rows["t2"][:], op=ALU.add), r=[f"h2{p}a", "r_t2"], w=[f"h2{p}"])
        kb.dma("sp", d_h2[:, t, :], h2[p][:], r=[f"h2{p}"], is_output=True)
        for half in range(2):
            ps_ = pss[4 + half]; pk = f"ps{4+half}"
            def tr(ps_=ps_, half=half):
                for i in range(4):
                    k = half * 4 + i
                    ins = nc.tensor.transpose(ps_[:, i * 128:(i + 1) * 128], h2[p][:, k * 128:(k + 1) * 128], ident[:])
                return ins
            kb.op("pe", tr, r=[f"h2{p}", "ident"], w=[pk])
            kb.op("act", lambda ps_=ps_, half=half: nc.scalar.activation(out=h2T[p][:, half * 4:(half + 1) * 4, :].rearrange("p a b -> p (a b)"),
                                                                         in_=ps_[:], func=AF.Copy), r=[pk], w=[f"h2T{p}_{half}"])
        pl = pss[6 + p]; plk = f"ps{6+p}"
        def mml(pl=pl):
            for k in range(8):
                ins = nc.tensor.matmul(pl[:, 0:32], h2T[p][:, k, :], wr[:, k, :], start=(k == 0), stop=(k == 7))
            return ins
        kb.op("pe", mml, r=[f"h2T{p}_0", f"h2T{p}_1", "wr"], w=[plk])
        kb.op("dve", lambda pl=pl: nc.vector.tensor_tensor(out=lg[p][:], in0=pl[:, 0:32], in1=br[:], op=ALU.add), r=[plk, "br"], w=[f"lg{p}"])
        kb.op("dve", lambda: nc.vector.max(out=top8[p][:], in_=lg[p][:]), r=[f"lg{p}"], w=[f"top8{p}"])
        kb.op("dve", lambda: nc.vector.tensor_scalar(out=msk[p][:], in0=lg[p][:], scalar1=top8[p][:, 3:4], scalar2=None, op0=ALU.is_ge),
              r=[f"lg{p}", f"top8{p}"], w=[f"msk{p}"])
        kb.op("dve", lambda: nc.vector.tensor_scalar_mul(out=nmx[p][:], in0=top8[p][:, 0:1], scalar1=-1.0), r=[f"top8{p}"], w=[f"nmx{p}"])
        kb.op("act", lambda: nc.scalar.activation(out=ex[p][:], in_=lg[p][:], func=AF.Exp, bias=nmx[p][:, 0:1], scale=1.0), r=[f"lg{p}", f"nmx{p}"], w=[f"ex{p}"])
        kb.op("dve", lambda: nc.vector.tensor_tensor(out=ex[p][:], in0=ex[p][:], in1=msk[p][:], op=ALU.mult), r=[f"ex{p}", f"msk{p}"], w=[f"em{p}"])
        kb.op("dve", lambda: nc.vector.reduce_sum(out=den[p][:], in_=ex[p][:], axis=AX.X), r=[f"em{p}"], w=[f"den{p}"])
        kb.op("dve", lambda: nc.vector.reciprocal(out=den[p][:], in_=den[p][:]), r=[f"den{p}"], w=[f"rden{p}"])
        kb.op("dve", lambda t=t: nc.vector.tensor_scalar(out=Gt[:, t, :], in0=ex[p][:], scalar1=den[p][:, 0:1], scalar2=None, op0=ALU.mult),
              r=[f"em{p}", f"rden{p}"], w=[f"G{t}"])
    kb.dma("sp", d_G[:, :, :], Gt[:], r=[f"G{t}" for t in range(16)], is_output=True)
    return kb.finish()


def rep128(v):
    return np.ascontiguousarray(np.broadcast_to(v[None, :], (128, v.shape[0])))


def core_tok_tiles(a):
    return np.ascontiguousarray(a.reshape(16, 128, -1).transpose(1, 0, 2))


def from_core_tok_tiles(a):
    return a.transpose(1, 0, 2).reshape(2048, -1)


def host_C(I, l, x, mix, mods):
    wout = np.ascontiguousarray(I['w_out'][l].reshape(8, 128, 1024).transpose(1, 0, 2))
    wr = np.ascontiguousarray(I['w_router'][l].reshape(8, 128, 32).transpose(1, 0, 2))
    maps = []
    for r in range(8):
        b, j = r // 4, r % 4
        sl = slice(j * 2048, (j + 1) * 2048)
        mixT = np.ascontiguousarray(mix[b, sl].T.reshape(8, 128, 2048).transpose(1, 0, 2))
        md = mods[l, b]
        maps.append({"mixT": mixT, "wout": wout, "x": core_tok_tiles(x[b, sl]),
                     "g1": rep128(md[2048:3072]), "lng": rep128(I['ln1_g'][l]), "lnb": rep128(I['ln1_b'][l]),
                     "s2": rep128(md[4096:5120]), "t2": rep128(md[3072:4096]),
                     "wr": wr, "br": rep128(I['b_router'][l]), "ident": np.eye(128, dtype=np.float32)})
    return maps


def post_C(res):
    def gather(name, d):
        return np.stack([np.concatenate([from_core_tok_tiles(res.results[b * 4 + j][name]) for j in range(4)], axis=0) for b in range(2)])
    return gather("x1", 1024), gather("h2", 1024), gather("G", 32)


CAP = 2816
NT = [(0, 512), (512, 512), (1024, 512), (1536, 512), (2048, 512), (2560, 256)]


def build_D():
    kb = KB()
    nc = kb.nc
    d_X = kb.dram("XT", [4, 128, 8, CAP]); d_gate = kb.dram("gate", [4, 128, CAP // 128])
    d_wgu = kb.dram("wgu", [4, 128, 8, 2048]); d_bgu = kb.dram("bgu", [4, 128, 16])
    d_wd = kb.dram("wd", [4, 128, 8, 1024]); d_bd = kb.dram("bd", [4, 128, 1024])
    d_Y = kb.dram("Y", [4, 128, CAP // 128, 1024], kind="ExternalOutput")
    wgu = kb.sb("wgu", [128, 8, 2048], F32R); wd = kb.sb("wd", [128, 8, 1024], F32R)
    bgu = kb.sb("bgu", [128, 16]); bd = kb.sb("bd", [128, 1024]); gate = kb.sb("gate", [128, CAP // 128])
    XT = [kb.sb(f"XT{i}", [128, 8, 512], F32R) for i in range(2)]
    actT = [kb.sb(f"actT{i}", [128, 8, 512], F32R) for i in range(2)]
    gl = [kb.sb(f"gl{i}", [128, 512]) for i in range(2)]; l1 = [kb.sb(f"l1{i}", [128, 512]) for i in range(2)]
    sg = [kb.sb(f"sg{i}", [128, 512]) for i in range(2)]
    ysb = [kb.sb(f"ysb{i}", [128, 1024]) for i in range(2)]
    pss = [kb.ps(f"ps{i}", [128, 512]) for i in range(8)]
    xc = 0; cc = 0; yc = 0; pc = 0
    for e in range(4):
        for k in range(8):
            kb.dma("pool", wgu[:, k, :], d_wgu[e, :, k, :], w=[f"wgu{k}"])
        for k in range(8):
            kb.dma("pool", wd[:, k, :], d_wd[e, :, k, :], w=[f"wd{k}"])
        kb.dma("sp", bgu[:], d_bgu[e, :, :], w=["bgu"]); kb.dma("sp", bd[:], d_bd[e, :, :], w=["bd"])
        kb.dma("sp", gate[:], d_gate[e, :, :], w=["gate"])
        wguk = [f"wgu{k}" for k in range(8)]; wdk = [f"wd{k}" for k in range(8)]
        for (n0, nn) in NT:
            X_ = XT[xc % 2]; Xk = f"XT{xc%2}"; A_ = actT[xc % 2]; Ak = f"actT{xc%2}"; xc += 1
            for k in range(8):
                kb.dma("pool", X_[:, k, 0:nn], d_X[e, :, k, n0:n0 + nn], w=[Xk + f"_{k}"])
            Xks = [Xk + f"_{k}" for k in range(8)]
            for c in range(8):
                pg = pss[pc % 4]; pgk = f"ps{pc%4}"; pc += 1
                pl = pss[pc % 4]; plk = f"ps{pc%4}"; pc += 1
                q = cc % 2; cc += 1
                def mmg(pg=pg, c=c, X_=X_, nn=nn):
                    for k in range(8):
                        ins = nc.tensor.matmul(pg[:, 0:nn], wgu[:, k, c * 128:(c + 1) * 128], X_[:, k, 0:nn], start=(k == 0), stop=(k == 7))
                    return ins
                kb.op("pe", mmg, r=wguk + Xks, w=[pgk])
                def mml(pl=pl, c=c, X_=X_, nn=nn):
                    for k in range(8):
                        ins = nc.tensor.matmul(pl[:, 0:nn], wgu[:, k, (8 + c) * 128:(9 + c) * 128], X_[:, k, 0:nn], start=(k == 0), stop=(k == 7))
                    return ins
                kb.op("pe", mml, r=wguk + Xks, w=[plk])
                kb.op("dve", lambda pg=pg, c=c, q=q, nn=nn: nc.vector.tensor_scalar(out=gl[q][:, 0:nn], in0=pg[:, 0:nn], scalar1=bgu[:, c:c + 1], scalar2=7.0,
                                                                                 op0=ALU.add, op1=ALU.min), r=[pgk, "bgu"], w=[f"gl{q}"])
                kb.op("dve", lambda pl=pl, c=c, q=q, nn=nn: nc.vector.tensor_scalar(out=l1[q][:, 0:nn], in0=pl[:, 0:nn], scalar1=bgu[:, 8 + c:9 + c], scalar2=7.0,
                                                                                 op0=ALU.add, op1=ALU.min), r=[plk, "bgu"], w=[f"l1{q}a"])
                kb.op("pool", lambda q=q, nn=nn: nc.gpsimd.tensor_scalar(out=l1[q][:, 0:nn], in0=l1[q][:, 0:nn], scalar1=-7.0, scalar2=1.0,
                                                                         op0=ALU.max, op1=ALU.add), r=[f"l1{q}a"], w=[f"l1{q}"])
                kb.op("act", lambda q=q, nn=nn: nc.scalar.activation(out=sg[q][:, 0:nn], in_=gl[q][:, 0:nn], func=AF.Sigmoid, scale=1.702), r=[f"gl{q}"], w=[f"sg{q}a"])
                kb.op("pool", lambda q=q, nn=nn: nc.gpsimd.tensor_tensor(out=sg[q][:, 0:nn], in0=sg[q][:, 0:nn], in1=gl[q][:, 0:nn], op=ALU.mult),
                      r=[f"sg{q}a", f"gl{q}"], w=[f"sg{q}"])
                kb.op("dve", lambda q=q, nn=nn, c=c, A_=A_: nc.vector.tensor_tensor(out=A_[:, c, 0:nn], in0=sg[q][:, 0:nn], in1=l1[q][:, 0:nn], op=ALU.mult),
                      r=[f"sg{q}", f"l1{q}"], w=[Ak + f"_{c}"])
            Aks = [Ak + f"_{c}" for c in range(8)]
            for si in range(nn // 128):
                s = n0 // 128 + si
                y_ = ysb[yc % 2]; yk = f"ysb{yc%2}"; yc += 1
                for half in range(2):
                    py = pss[4 + pc % 4]; pyk = f"ps{4 + pc%4}"; pc += 1
                    def mmy(py=py, half=half, si=si, A_=A_):
                        for k in range(8):
                            ins = nc.tensor.matmul(py[:], A_[:, k, si * 128:(si + 1) * 128], wd[:, k, half * 512:(half + 1) * 512], start=(k == 0), stop=(k == 7))
                        return ins
                    kb.op("pe", mmy, r=Aks + wdk, w=[pyk])
                    hs = slice(half * 512, (half + 1) * 512)
                    kb.op("dve", lambda py=py, hs=hs, y_=y_: nc.vector.tensor_tensor(out=y_[:, hs], in0=py[:], in1=bd[:, hs], op=ALU.add), r=[pyk, "bd"], w=[yk + f"a{half}"])
                kb.op("act", lambda y_=y_, s=s: nc.scalar.activation(out=y_[:], in_=y_[:], func=AF.Copy, scale=gate[:, s:s + 1]),
                      r=[yk + "a0", yk + "a1", "gate"], w=[yk])
                kb.dma("sp", d_Y[e, :, s, :], y_[:], r=[yk], is_output=True)
    return kb.finish()


def route_host(G):
    return [np.nonzero(G[:, e] > 0)[0] for e in range(32)]


def host_D(I, l, h2f, G, idxs, off=0):
    maps = []
    for r in range(8):
        XT = np.zeros((4, 128, 8, CAP), np.float32); gate = np.zeros((4, 128, CAP // 128), np.float32)
        for i in range(4):
            e = 4 * r + i
            idx = idxs[e][off:off + CAP]
            n = len(idx)
            rowsT = h2f[idx].T
            XT[i, :, :, :n] = rowsT.reshape(8, 128, n).transpose(1, 0, 2)
            gg = np.zeros(CAP, np.float32); gg[:n] = G[idx, e]
            gate[i] = gg.reshape(CAP // 128, 128).T
        es = slice(4 * r, 4 * r + 4)
        wgu = np.ascontiguousarray(I['w_gate_up'][l][es].reshape(4, 8, 128, 2048).transpose(0, 2, 1, 3))
        wd = np.ascontiguousarray(I['w_down'][l][es].reshape(4, 8, 128, 1024).transpose(0, 2, 1, 3))
        bgu = np.ascontiguousarray(I['b_gate_up'][l][es].reshape(4, 16, 128).transpose(0, 2, 1))
        bd = np.ascontiguousarray(np.broadcast_to(I['b_down'][l][es][:, None, :], (4, 128, 1024)))
        maps.append({"XT": XT, "gate": gate, "wgu": wgu, "bgu": bgu, "wd": wd, "bd": bd})
    return maps


def post_D(res, idxs, Y4=None, fill=None, off=0):
    if Y4 is None:
        Y4 = np.zeros((16384, 4, 1024), np.float32)
        fill = np.zeros(16384, np.int64)
    for r in range(8):
        Y = res.results[r]["Y"]
        for i in range(4):
            e = 4 * r + i
            idx = idxs[e][off:off + CAP]
            rows = Y[i].transpose(1, 0, 2).reshape(CAP, 1024)[:len(idx)]
            Y4[idx, fill[idx]] = rows
            fill[idx] += 1
    return Y4, fill


def build_E():
    kb = KB()
    nc = kb.nc
    d_x1 = kb.dram("x1", [128, 16, 1024]); d_Y4 = kb.dram("Y4", [128, 16, 4, 1024])
    d_rows = {n: kb.dram(n, [128, 1024]) for n in ("g2", "lng", "lnb")}
    d_x2 = kb.dram("x2", [128, 16, 1024], kind="ExternalOutput")
    rows = {n: kb.sb("r_" + n, [128, 1024]) for n in d_rows}
    eps = kb.sb("eps", [128, 1])
    xt = [kb.sb(f"xt{i}", [128, 1024]) for i in range(2)]
    y4 = [kb.sb(f"y4{i}", [128, 4, 1024]) for i in range(2)]
    sa = [kb.sb(f"sa{i}", [128, 1024]) for i in range(2)]; sb_ = [kb.sb(f"sb{i}", [128, 1024]) for i in range(2)]
    u = [kb.sb(f"u{i}", [128, 1024]) for i in range(2)]; xn = [kb.sb(f"xn{i}", [128, 1024]) for i in range(2)]
    scr = [ln_scratch(kb, f"ln{i}") for i in range(2)]
    for s_ in scr:
        s_["eps"] = eps
    kb.op("pool", lambda: nc.gpsimd.memset(eps[:], 1e-5), w=["eps"])
    for n in d_rows:
        kb.dma("sp", rows[n][:], d_rows[n][:, :], w=["r_" + n])
    kb.op("dve", lambda: nc.vector.tensor_scalar_add(out=rows["g2"][:], in0=rows["g2"][:], scalar1=1.0), r=["r_g2"], w=["r_g2"])
    for t in range(16):
        p = t % 2
        kb.dma("sp", xt[p][:], d_x1[:, t, :], w=[f"xt{p}"])
        kb.dma("sp", y4[p][:], d_Y4[:, t, :, :], w=[f"y4{p}"])
        kb.op("dve", lambda: nc.vector.tensor_tensor(out=sa[p][:], in0=y4[p][:, 0, :], in1=y4[p][:, 1, :], op=ALU.add), r=[f"y4{p}"], w=[f"sa{p}a"])
        kb.op("pool", lambda: nc.gpsimd.tensor_tensor(out=sb_[p][:], in0=y4[p][:, 2, :], in1=y4[p][:, 3, :], op=ALU.add), r=[f"y4{p}"], w=[f"sb{p}"])
        kb.op("pool", lambda: nc.gpsimd.tensor_tensor(out=sa[p][:], in0=sa[p][:], in1=sb_[p][:], op=ALU.add), r=[f"sa{p}a", f"sb{p}"], w=[f"sa{p}b"])
        kb.op("pool", lambda: nc.gpsimd.tensor_tensor(out=sa[p][:], in0=sa[p][:], in1=rows["g2"][:], op=ALU.mult), r=[f"sa{p}b", "r_g2"], w=[f"sa{p}"])
        kb.op("dve", lambda: nc.vector.scalar_tensor_tensor(out=u[p][:], in0=xt[p][:], scalar=ALPHA, in1=sa[p][:], op0=ALU.mult, op1=ALU.add),
              r=[f"xt{p}", f"sa{p}"], w=[f"u{p}"])
        emit_ln(kb, nc, u[p], f"u{p}", xn[p], f"xn{p}", scr[p], f"ln{p}")
        kb.op("dve", lambda: nc.vector.tensor_tensor(out=xn[p][:], in0=xn[p][:], in1=rows["lng"][:], op=ALU.mult), r=[f"xn{p}", "r_lng"], w=[f"xn{p}b"])
        kb.op("pool", lambda: nc.gpsimd.tensor_tensor(out=xn[p][:], in0=xn[p][:], in1=rows["lnb"][:], op=ALU.add), r=[f"xn{p}b", "r_lnb"], w=[f"xn{p}c"])
        kb.dma("sp", d_x2[:, t, :], xn[p][:], r=[f"xn{p}c"], is_output=True)
    return kb.finish()


def host_E(I, l, x1, Y4, mods):
    maps = []
    for r in range(8):
        b, j = r // 4, r % 4
        sl = slice(j * 2048, (j + 1) * 2048)
        y = Y4[b * 8192 + j * 2048: b * 8192 + (j + 1) * 2048]
        maps.append({"x1": core_tok_tiles(x1[b, sl]),
                     "Y4": np.ascontiguousarray(y.reshape(16, 128, 4, 1024).transpose(1, 0, 2, 3)),
                     "g2": rep128(mods[l, b, 5120:6144]), "lng": rep128(I['ln2_g'][l]), "lnb": rep128(I['ln2_b'][l])})
    return maps


def post_E(res):
    return np.stack([np.concatenate([from_core_tok_tiles(res.results[b * 4 + j]["x2"]) for j in range(4)], axis=0) for b in range(2)])


_PROGS = {}


def _prog(name, fn):
    if name not in _PROGS:
        _PROGS[name] = fn()
    return _PROGS[name]


def kernel(**I):
    I = {k: np.asarray(v) for k, v in I.items()}
    x = np.ascontiguousarray(I['x'], dtype=np.float32)
    mods = post_M(run(_prog("M", build_M), host_M(I)))
    pairs_all = [(b, h) for b in range(2) for h in range(6)]
    for l in range(4):
        z = post_A(run(_prog("A", build_A), host_A(I, l, x, mods)))
        mix = np.zeros((2, 8192, 1024), np.float32)
        for grp in (pairs_all[0:8], pairs_all[8:12] + pairs_all[8:12]):
            res = run(_prog("B", build_B), host_B(z, grp))
            for i, (b, h) in enumerate(grp):
                mix[b, :, h * 64:(h + 1) * 64] = res.results[i]["oT"].T
        res = run(_prog("G", build_G), host_G(I, l, z))
        for r in range(8):
            b, h = r // 4, r % 4
            mix[b, :, 384 + h * 96:384 + (h + 1) * 96] = res.results[r]["og"].transpose(1, 0, 2).reshape(8192, 96)
            mix[b, :, 768 + h * 64:768 + (h + 1) * 64] = res.results[r]["opT"].T
        del z
        x1, h2, G = post_C(run(_prog("C", build_C), host_C(I, l, x, mix, mods)))
        Gf = G.reshape(-1, 32); h2f = h2.reshape(-1, 1024)
        idxs = route_host(Gf)
        Y4 = None; fill = None; off = 0
        nmax = max(len(i) for i in idxs)
        while True:
            res = run(_prog("D", build_D), host_D(I, l, h2f, Gf, idxs, off))
            Y4, fill = post_D(res, idxs, Y4, fill, off)
            off += CAP
            if off >= nmax:
                break
        x = post_E(run(_prog("E", build_E), host_E(I, l, x1, Y4, mods)))
    return np.ascontiguousarray(x, dtype=np.float32)
```

```python
import numpy as np
import concourse.bass as bass
import concourse.mybir as mybir
from concourse.bass_utils import run_bass_kernel_spmd

F32 = mybir.dt.float32
F32R = mybir.dt.float32r
I32 = mybir.dt.int32
AF = mybir.ActivationFunctionType
ALU = mybir.AluOpType
AX = mybir.AxisListType


class KB:
    NDMA = 24

    def __init__(self):
        nc = bass.Bass("TRN2", target_bir_lowering=False)
        self.nc = nc
        self.eng = {"pe": nc.tensor, "act": nc.scalar, "dve": nc.vector, "pool": nc.gpsimd, "sp": nc.sync}
        self.psem = {}
        self.pcnt = {}
        for e in ("pe", "act", "dve", "pool"):
            self.psem[e] = [nc.alloc_semaphore(f"prog_{e}")]
            self.pcnt[e] = 0
        self.dsem = [nc.alloc_semaphore(f"dma_{i}") for i in range(self.NDMA)]
        self.dcnt = [0] * self.NDMA
        self.dnext = 0
        self.seen = {e: {} for e in self.eng}
        self.buf = {}
        self.out_toks = []
        self._ctx = []

    def dram(self, name, shape, dt=F32, kind="ExternalInput"):
        return self.nc.dram_tensor(name, list(shape), dt, kind=kind).ap()

    def sb(self, name, shape, dt=F32):
        g = self.nc.sbuf_tensor("sb_" + name, list(shape), dt)
        t = g.__enter__()
        self._ctx.append(g)
        return t

    def ps(self, name, shape, dt=F32):
        g = self.nc.psum_tensor("pp_" + name, list(shape), dt)
        t = g.__enter__()
        self._ctx.append(g)
        return t

    def _wait(self, e, toks, raw_keys_same_engine=True):
        eng = self.eng[e]
        best = {}
        for t in toks:
            if t is None:
                continue
            sem, val, prod = t
            k = id(sem)
            if self.seen[e].get(k, 0) >= val:
                continue
            if k not in best or best[k][1] < val:
                best[k] = (sem, val)
        for k, (sem, val) in best.items():
            eng.wait_ge(sem, val)
            self.seen[e][k] = val

    def _deps(self, e, r, w):
        toks = []
        for k in r:
            b = self.buf.get(k)
            if b and b["w"] is not None:
                toks.append(b["w"])
        for k in w:
            b = self.buf.get(k)
            if b:
                if b["w"] is not None and (e == "dma" or b["w"][2] != e):
                    toks.append(b["w"])
                for pe_, t in b["r"].items():
                    if e == "dma" or isinstance(pe_, tuple) or pe_ != e:
                        toks.append(t)
        return toks

    def _record(self, tok, r, w):
        e = tok[2]
        for k in r:
            b = self.buf.setdefault(k, {"w": None, "r": {}})
            b["r"][e if e != "dma" else ("dma", id(tok[0]))] = tok
        for k in w:
            self.buf[k] = {"w": tok, "r": {}}

    def op(self, e, fn, r=(), w=()):
        self._wait(e, self._deps(e, r, w))
        ins = fn()
        self.pcnt[e] += 1
        sem = self.psem[e][-1]
        ins.then_inc(sem, 1)
        tok = (sem, self.pcnt[e], e)
        self._record(tok, r, w)
        return tok

    def dma(self, e, out, in_, r=(), w=(), is_output=False, **kw):
        slot = self.dnext
        self.dnext = (self.dnext + 1) % self.NDMA
        sem = self.dsem[slot]
        toks = self._deps("dma", r, w)
        if self.dcnt[slot] > 0:
            toks.append((sem, self.dcnt[slot], "dma"))
        self._wait(e, toks)
        ins = self.eng[e].dma_start(out=out, in_=in_, **kw)
        self.dcnt[slot] += 16
        ins.then_inc(sem, 16)
        tok = (sem, self.dcnt[slot], "dma")
        self._record(tok, r, w)
        if is_output:
            self.out_toks.append(tok)
        return tok

    def finish(self):
        toks = list(self.out_toks)
        for i, s in enumerate(self.dsem):
            if self.dcnt[i] > 0:
                toks.append((s, self.dcnt[i], "dma"))
        self._wait("sp", toks)
        toks = []
        for e in ("pe", "act", "dve", "pool"):
            if self.pcnt[e] > 0:
                toks.append((self.psem[e][-1], self.pcnt[e], e))
        self._wait("sp", toks)
        for g in reversed(self._ctx):
            g.__exit__(None, None, None)
        self._ctx = []
        return self.nc


def run(nc, in_maps, trace=False):
    res = run_bass_kernel_spmd(nc, in_maps, core_ids=list(range(len(in_maps))), trace=trace)
    return res


D = 1024
NIN = 2582
ALPHA = 8 ** 0.25


def build_M():
    kb = KB()
    nc = kb.nc
    cT = kb.dram("cT", [128, 8, 2])
    Wd = kb.dram("W", [128, 8, 3072])
    bd = kb.dram("bias", [128, 24])
    od = kb.dram("modT", [128, 24, 2], kind="ExternalOutput")
    ct = kb.sb("ct", [128, 8, 2])
    cs = kb.sb("cs", [128, 8, 2])
    Wt = kb.sb("Wt", [128, 8, 3072])
    bt = kb.sb("bt", [128, 24])
    ot = kb.sb("ot", [128, 24, 2])
    ps = kb.ps("ps", [128, 512])
    kb.dma("sp", ct[:], cT[:, :, :], w=["ct"])
    kb.dma("sp", bt[:], bd[:, :], w=["bt"])
    for k in range(8):
        kb.dma("sp", Wt[:, k, :], Wd[:, k, :], w=[f"W{k}"])
    kb.op("act", lambda: nc.scalar.activation(out=cs[:], in_=ct[:], func=AF.Silu), r=["ct"], w=["cs"])
    for j in range(24):
        def mm(j=j):
            for k in range(8):
                ins = nc.tensor.matmul(ps[:, 2 * j:2 * j + 2], Wt[:, k, j * 128:(j + 1) * 128], cs[:, k, :],
                                       start=(k == 0), stop=(k == 7))
            return ins
        kb.op("pe", mm, r=["cs"] + [f"W{k}" for k in range(8)], w=["ps"])
    for b in range(2):
        kb.op("dve", lambda b=b: nc.vector.tensor_tensor(out=ot[:, :, b], in0=ps[:, b:48:2], in1=bt[:], op=ALU.add),
              r=["ps", "bt"], w=[f"ot{b}"])
    kb.dma("sp", od[:, :, :], ot[:], r=["ot0", "ot1"], is_output=True)
    return kb.finish()


def host_M(I):
    Wall = np.concatenate([I['w_ada'][l] for l in range(4)], axis=1)
    ball = np.concatenate([I['b_ada'][l] for l in range(4)], axis=0)
    cT = np.ascontiguousarray(I['c'].T.reshape(8, 128, 2).transpose(1, 0, 2))
    maps = []
    for r in range(8):
        W = np.ascontiguousarray(Wall[:, r * 3072:(r + 1) * 3072].reshape(8, 128, 3072).transpose(1, 0, 2))
        bb = np.ascontiguousarray(ball[r * 3072:(r + 1) * 3072].reshape(24, 128).T)
        maps.append({"cT": cT, "W": W, "bias": bb})
    return maps


def post_M(res):
    cols = []
    for r in range(8):
        m = res.results[r]["modT"]
        cols.append(m.transpose(2, 1, 0).reshape(2, 3072))
    allm = np.concatenate(cols, axis=1)
    return allm.reshape(2, 4, 6144).transpose(1, 0, 2)


GROUPS = [(0, 512), (512, 512), (1024, 512), (1536, 512), (2048, 512), (2560, 22)]


def build_A():
    kb = KB()
    nc = kb.nc
    xT = kb.dram("xT", [128, 8, 2048])
    scd = kb.dram("sc", [128, 8])
    shd = kb.dram("sh", [128, 8])
    wd = kb.dram("w", [128, 8, NIN])
    bd = kb.dram("bias", [128, NIN])
    zd = kb.dram("z", [2048, NIN], kind="ExternalOutput")
    sc = kb.sb("sc", [128, 8]); sc1 = kb.sb("sc1", [128, 8]); sh = kb.sb("sh", [128, 8])
    wt = kb.sb("wt", [128, 8, NIN], F32R)
    bt = kb.sb("bt", [128, NIN])
    xb = [kb.sb(f"xb{i}", [128, 8, 512]) for i in range(2)]
    hT = [kb.sb(f"hT{i}", [128, 8, 512], F32R) for i in range(2)]
    zt = [kb.sb(f"zt{i}", [128, NIN]) for i in range(2)]
    pss = [kb.ps(f"ps{i}", [128, 512]) for i in range(4)]
    kb.dma("sp", sc[:], scd[:, :], w=["sc"])
    kb.dma("sp", sh[:], shd[:, :], w=["sh"])
    kb.dma("sp", bt[:], bd[:, :], w=["bt"])
    for k in range(8):
        for (c0, n) in ((0, 1291), (1291, 1291)):
            kb.dma("pool", wt[:, k, c0:c0 + n], wd[:, k, c0:c0 + n], w=[f"w{k}_{c0}"])
    wkeys = [f"w{k}_{c0}" for k in range(8) for c0 in (0, 1291)]
    kb.op("dve", lambda: nc.vector.tensor_scalar_add(out=sc1[:], in0=sc[:], scalar1=1.0), r=["sc"], w=["sc1"])
    pcnt = 0
    for tb in range(4):
        x_ = xb[tb % 2]; h_ = hT[tb % 2]
        kb.dma("sp", x_[:], xT[:, :, tb * 512:(tb + 1) * 512], w=[f"xb{tb%2}"])
        for k in range(8):
            kb.op("act", lambda k=k: nc.scalar.activation(out=h_[:, k, :], in_=x_[:, k, :], func=AF.Identity,
                                                          scale=sc1[:, k:k + 1], bias=sh[:, k:k + 1]),
                  r=[f"xb{tb%2}", "sc1", "sh"], w=[f"hT{tb%2}_{k}"])
        for ti in range(4):
            tile = tb * 4 + ti
            z_ = zt[tile % 2]
            for gi, (c0, n) in enumerate(GROUPS):
                p_ = pss[pcnt % 4]; pk = f"ps{pcnt%4}"; pcnt += 1
                def mm(p_=p_, c0=c0, n=n, ti=ti):
                    for k in range(8):
                        ins = nc.tensor.matmul(p_[:, 0:n], h_[:, k, ti * 128:(ti + 1) * 128], wt[:, k, c0:c0 + n],
                                               start=(k == 0), stop=(k == 7))
                    return ins
                kb.op("pe", mm, r=[f"hT{tb%2}_{k}" for k in range(8)] + wkeys, w=[pk])
                kb.op("dve", lambda p_=p_, c0=c0, n=n: nc.vector.tensor_tensor(out=z_[:, c0:c0 + n], in0=p_[:, 0:n],
                                                                               in1=bt[:, c0:c0 + n], op=ALU.add),
                      r=[pk, "bt"], w=[f"zt{tile%2}_{gi}"])
            kb.dma("sp", zd[tile * 128:(tile + 1) * 128, :], z_[:], r=[f"zt{tile%2}_{gi}" for gi in range(6)],
                   is_output=True)
    return kb.finish()


def cols128(v):
    return np.ascontiguousarray(v.reshape(8, 128).T)


def host_A(I, l, x, mods):
    maps = []
    w = np.ascontiguousarray(I['w_in'][l].reshape(8, 128, NIN).transpose(1, 0, 2))
    bias = np.ascontiguousarray(np.broadcast_to(I['b_in'][l][None, :], (128, NIN)))
    for r in range(8):
        b, j = r // 4, r % 4
        xs = x[b, j * 2048:(j + 1) * 2048, :]
        xT = np.ascontiguousarray(xs.T.reshape(8, 128, 2048).transpose(1, 0, 2))
        maps.append({"xT": xT, "sc": cols128(mods[l, b, 1024:2048]), "sh": cols128(mods[l, b, 0:1024]),
                     "w": w, "bias": bias})
    return maps


def post_A(res):
    z = np.stack([np.concatenate([res.results[b * 4 + j]["z"] for j in range(4)], axis=0) for b in range(2)])
    return z


def fox_consts():
    s = np.arange(128)[:, None]; m = np.arange(128)[None, :]
    tri = (s <= m).astype(np.float32)
    ones = np.ones((128, 128), np.float32)
    ident = np.eye(128, dtype=np.float32)
    t = np.arange(512)[None, None, :]; d = np.arange(4)[None, :, None]; ss = np.arange(128)[:, None, None]
    maskadd = np.where(128 * d + ss <= t, 0.0, -30000.0).astype(np.float32)
    return {"tri": tri, "ones": ones, "ident": ident, "maskadd": np.ascontiguousarray(maskadd)}


def build_B():
    kb = KB()
    nc = kb.nc
    S = 8192
    qd = kb.dram("qT", [64, S]); kd = kb.dram("kT", [64, S]); vd = kb.dram("v", [128, 64, 64]); fd = kb.dram("ff", [128, 64])
    trid = kb.dram("tri", [128, 128]); onesd = kb.dram("ones", [128, 128]); identd = kb.dram("ident", [128, 128])
    maskd = kb.dram("maskadd", [128, 4, 512])
    od = kb.dram("oT", [64, S], kind="ExternalOutput")
    qa = kb.sb("qa", [66, S], F32R); ka = kb.sb("ka", [66, S], F32R); va = kb.sb("va", [128, 64, 65], F32R)
    stg = kb.sb("stg", [128, 4224])
    tri = kb.sb("tri", [128, 128]); ones = kb.sb("ones", [128, 128]); ident = kb.sb("ident", [128, 128])
    identR = kb.sb("identR", [128, 128], F32R)
    maskadd = kb.sb("maskadd", [128, 4, 512])
    ff = kb.sb("ff", [128, 64]); sg = kb.sb("sg", [128, 64]); ls = kb.sb("ls", [128, 64])
    tot = kb.sb("tot", [128, 64]); incl = kb.sb("incl", [128, 64]); tmpc = kb.sb("tmpc", [128, 64])
    Fc = kb.sb("Fc", [128, 64]); negF = kb.sb("negF", [128, 64]); F8 = kb.sb("F8", [128, 64]); lo8 = kb.sb("lo8", [128, 64])
    X = kb.sb("X", [128, 64, 66], F32R)
    P = [kb.sb(f"P{i}", [128, 512], F32R) for i in range(4)]
    tmpd = [kb.sb(f"tmpd{i}", [128, 512]) for i in range(2)]
    Osb = [kb.sb(f"Osb{i}", [65, 512]) for i in range(2)]
    rec = [kb.sb(f"rec{i}", [65, 512]) for i in range(2)]
    ot = [kb.sb(f"ot{i}", [65, 512]) for i in range(2)]
    psS = [kb.ps(f"psS{i}", [128, 512]) for i in range(4)]
    psO = [kb.ps(f"psO{i}", [128, 512]) for i in range(2)]
    psM = [kb.ps(f"psM{i}", [128, 512]) for i in range(2)]

    for (t_, d_, k_) in ((tri, trid, "tri"), (ones, onesd, "ones"), (ident, identd, "ident")):
        kb.dma("sp", t_[:], d_[:, :], w=[k_])
    kb.dma("sp", maskadd[:], maskd[:, :, :], w=["maskadd"])
    kb.dma("sp", ff[:], fd[:, :], w=["ff"])
    kb.op("dve", lambda: nc.vector.tensor_copy(out=identR[:], in_=ident[:]), r=["ident"], w=["identR"])
    kb.op("pool", lambda: nc.gpsimd.memset(stg[:], 1.0), w=["stg"])
    for hf in range(2):
        kb.op("dve", lambda hf=hf: nc.vector.tensor_copy(out=ka[64:66, hf * 4096:(hf + 1) * 4096], in_=stg[64:66, 0:4096]), r=["stg"], w=["ka_hi"])
    kb.op("dve", lambda: nc.vector.tensor_copy(out=va[:, :, 0], in_=stg[:, 0:64]), r=["stg"], w=["va_1"])
    kb.op("pool", lambda: nc.gpsimd.memset(stg[:], 0.0), r=[], w=["stg"])
    kb.op("dve", lambda: nc.vector.tensor_copy(out=X[:].rearrange("p t c -> p (t c)"), in_=stg[:, 0:4224]), r=["stg"], w=["X"])
    for nm, src, dst in (("q", qd, qa), ("k", kd, ka)):
        for hf in range(2):
            kb.dma("sp", stg[0:64, 0:4096], src[:, hf * 4096:(hf + 1) * 4096], w=["stg"])
            kb.op("dve", lambda dst=dst, hf=hf: nc.vector.tensor_copy(out=dst[0:64, hf * 4096:(hf + 1) * 4096], in_=stg[0:64, 0:4096]),
                  r=["stg"], w=[f"{nm}a_lo"])
    kb.dma("sp", stg[:, 0:4096], vd.rearrange("p t d -> p (t d)"), w=["stg"])
    kb.op("dve", lambda: nc.vector.tensor_copy(out=va[:, :, 1:65], in_=stg[:, 0:4096].rearrange("p (t d) -> p t d", d=64)),
          r=["stg"], w=["va_v"])
    kb.op("act", lambda: nc.scalar.activation(out=sg[:], in_=ff[:], func=AF.Sigmoid), r=["ff"], w=["sg"])
    kb.op("act", lambda: nc.scalar.activation(out=ls[:], in_=sg[:], func=AF.Ln), r=["sg"], w=["ls"])
    def mmc():
        nc.tensor.matmul(psM[0][:, 0:64], tri[:], ls[:], start=True, stop=True)
        return nc.tensor.matmul(psM[0][:, 64:128], ones[:], ls[:], start=True, stop=True)
    kb.op("pe", mmc, r=["tri", "ones", "ls"], w=["psM0"])
    kb.op("dve", lambda: nc.vector.tensor_copy(out=tot[:], in_=psM[0][:, 64:128]), r=["psM0"], w=["tot"])
    kb.op("dve", lambda: nc.vector.tensor_tensor_scan(out=incl[:], data0=ones[:, 0:64], data1=tot[:], initial=0.0,
                                                      op0=ALU.mult, op1=ALU.add), r=["tot", "ones"], w=["incl"])
    kb.op("dve", lambda: nc.vector.tensor_tensor(out=tmpc[:], in0=incl[:], in1=tot[:], op=ALU.subtract), r=["incl", "tot"], w=["tmpc"])
    kb.op("dve", lambda: nc.vector.tensor_tensor(out=Fc[:], in0=psM[0][:, 0:64], in1=tmpc[:], op=ALU.add), r=["psM0", "tmpc"], w=["Fc"])
    kb.op("dve", lambda: nc.vector.tensor_scalar_mul(out=negF[:], in0=Fc[:], scalar1=-1.0), r=["Fc"], w=["negF"])
    kb.op("dve", lambda: nc.vector.tensor_scalar_mul(out=F8[:], in0=Fc[:], scalar1=8.0), r=["Fc"], w=["F8"])
    kb.op("dve", lambda: nc.vector.tensor_copy(out=X[:, :, 64], in_=F8[:]), r=["F8", "X"], w=["Xhi"])
    kb.op("dve", lambda: nc.vector.tensor_tensor(out=lo8[:], in0=F8[:], in1=X[:, :, 64].bitcast(F32), op=ALU.subtract),
          r=["F8", "Xhi"], w=["lo8"])
    kb.op("dve", lambda: nc.vector.tensor_copy(out=X[:, :, 65], in_=lo8[:]), r=["lo8", "X"], w=["Xlo"])
    for blk in range(16):
        pm = psM[1]
        def mmx(blk=blk):
            for i in range(4):
                t = blk * 4 + i
                ins = nc.tensor.matmul(pm[0:66, i * 128:(i + 1) * 128], X[:, t, :], identR[:], start=True, stop=True)
            return ins
        kb.op("pe", mmx, r=["X", "Xhi", "Xlo", "identR"], w=["psM1"])
        kb.op("act", lambda blk=blk: nc.scalar.activation(out=qa[64:66, blk * 512:(blk + 1) * 512], in_=pm[64:66, :], func=AF.Copy),
              r=["psM1"], w=[f"qa_hi{blk}"])
    tiles = [(qb, kt) for qb in range(16) for kt in range(4 * qb + 4)]
    LOOK = 2
    st_ = {"dc": 0}

    def emit_qk(i):
        qb, kt = tiles[i]
        pS = psS[i % 4]; pSk = f"psS{i%4}"; P_ = P[i % 4]; Pk = f"P{i%4}"
        kb.op("pe", lambda: nc.tensor.matmul(pS[:], ka[:, kt * 128:(kt + 1) * 128], qa[:, qb * 512:(qb + 1) * 512], start=True, stop=True),
              r=["qa_lo", "ka_lo", "ka_hi", f"qa_hi{qb}"], w=[pSk])
        d = kt - 4 * qb
        if d >= 0:
            dc = st_["dc"]; st_["dc"] += 1
            td = tmpd[dc % 2]; tdk = f"tmpd{dc%2}"
            kb.op("dve", lambda: nc.vector.scalar_tensor_tensor(out=td[:], in0=pS[:], scalar=0.125, in1=maskadd[:, d, :], op0=ALU.mult, op1=ALU.add),
                  r=[pSk, "maskadd"], w=[tdk])
            kb.op("act", lambda: nc.scalar.activation(out=P_[:], in_=td[:], func=AF.Exp, bias=negF[:, kt:kt + 1], scale=1.0), r=[tdk, "negF"], w=[Pk])
        else:
            kb.op("act", lambda: nc.scalar.activation(out=P_[:], in_=pS[:], func=AF.Exp, bias=negF[:, kt:kt + 1], scale=0.125), r=[pSk, "negF"], w=[Pk])

    def emit_pv(i):
        qb, kt = tiles[i]
        nk = 4 * qb + 4
        P_ = P[i % 4]; Pk = f"P{i%4}"
        pO = psO[qb % 2]; pOk = f"psO{qb%2}"
        kb.op("pe", lambda: nc.tensor.matmul(pO[0:65, :], va[:, kt, :], P_[:], start=(kt == 0), stop=(kt == nk - 1)), r=[Pk, "va_v", "va_1"], w=[pOk])
        if kt == nk - 1:
            O_ = Osb[qb % 2]; Ok = f"Osb{qb%2}"
            kb.op("dve", lambda: nc.vector.tensor_copy(out=O_[:], in_=pO[0:65, :]), r=[pOk], w=[Ok])
            kb.op("pe", lambda: nc.tensor.matmul(psM[0][0:65, :], ones[0:1, 0:65], O_[0:1, :], start=True, stop=True), r=[Ok, "ones"], w=["psM0"])
            r_ = rec[qb % 2]; rk = f"rec{qb%2}"
            kb.op("dve", lambda: nc.vector.reciprocal(out=r_[:], in_=psM[0][0:65, :]), r=["psM0"], w=[rk])
            o_ = ot[qb % 2]; ok = f"ot{qb%2}"
            kb.op("pool", lambda: nc.gpsimd.tensor_tensor(out=o_[:], in0=O_[:], in1=r_[:], op=ALU.mult), r=[Ok, rk], w=[ok])
            kb.dma("sp", od[:, qb * 512:(qb + 1) * 512], o_[1:65, :], r=[ok], is_output=True)

    n = len(tiles)
    for i in range(n + LOOK):
        if i < n:
            emit_qk(i)
        if i - LOOK >= 0:
            emit_pv(i - LOOK)
    return kb.finish()


def host_B(z, pairs):
    C = fox_consts()
    maps = []
    for (b, h) in pairs:
        q = z[b, :, h * 64:(h + 1) * 64]; k = z[b, :, 384 + h * 64:384 + (h + 1) * 64]; v = z[b, :, 768 + h * 64:768 + (h + 1) * 64]
        ff = z[b, :, 1152 + h]
        m = {"qT": np.ascontiguousarray(q.T), "kT": np.ascontiguousarray(k.T),
             "v": np.ascontiguousarray(v.reshape(64, 128, 64).transpose(1, 0, 2)),
             "ff": np.ascontiguousarray(ff.reshape(64, 128).T)}
        m.update(C)
        maps.append(m)
    return maps


def gla_consts(g):
    w = (2, 4, 8, 16)[g]
    s = np.arange(128)[:, None]; t = np.arange(128)[None, :]
    tri = (s <= t).astype(np.float32)
    triC = (s > t).astype(np.float32)
    eye = np.eye(128, dtype=np.float32)
    bandCur = np.where((s <= t) & (s >= t - w + 1), 1.0 / w, 0.0).astype(np.float32) - eye
    bandPrev = np.where(s >= 128 + t - w + 1, 1.0 / w, 0.0).astype(np.float32)
    cnt = np.minimum(t + 1, w).astype(np.float32)
    bandCur0 = np.where((s <= t) & (s >= t - w + 1), 1.0 / cnt, 0.0).astype(np.float32) - eye
    return {"tri": tri, "triC": triC, "bandCur": bandCur, "bandPrev": bandPrev, "bandCur0": bandCur0}


def build_G():
    kb = KB()
    nc = kb.nc
    S = 8192
    d_ga = kb.dram("ga1T", [17, S]); d_wa = kb.dram("wa2", [17, 48])
    d_qT = kb.dram("gqT", [48, S]); d_kT = kb.dram("gkT", [48, S]); d_k = kb.dram("gk", [128, 64, 48])
    d_v = kb.dram("gv", [128, 64, 96]); d_gr = kb.dram("gr", [128, 64, 96]); d_ng = kb.dram("normg", [128, 96])
    d_tri = kb.dram("tri", [128, 128]); d_triC = kb.dram("triC", [128, 128])
    d_u = kb.dram("pu", [128, 64, 64]); d_bc = kb.dram("bandCur", [128, 128]); d_bp = kb.dram("bandPrev", [128, 128])
    d_bc0 = kb.dram("bandCur0", [128, 128]); d_pw = kb.dram("poolw", [64, 64]); d_psc = kb.dram("pscale", [64, 1])
    d_og = kb.dram("og", [128, 64, 96], kind="ExternalOutput"); d_op = kb.dram("opT", [64, S], kind="ExternalOutput")

    gaB = [kb.sb(f"ga{i}", [17, 2048]) for i in range(2)]; wa = kb.sb("wa", [17, 48])
    qTB = [kb.sb(f"qT{i}", [48, 2048]) for i in range(2)]; kTB = [kb.sb(f"kT{i}", [48, 2048]) for i in range(2)]
    k = kb.sb("k", [128, 64, 48])
    v = kb.sb("v", [128, 64, 96]); gr = kb.sb("gr", [128, 64, 96]); ng = kb.sb("ng", [128, 96])
    tri = kb.sb("tri", [128, 128]); triC = kb.sb("triC", [128, 128])
    u = kb.sb("u", [128, 64, 64]); bc = kb.sb("bc", [128, 128]); bp = kb.sb("bp", [128, 128]); bc0 = kb.sb("bc0", [128, 128])
    pw = kb.sb("pw", [64, 64]); psc = kb.sb("psc", [64, 1])
    la = kb.sb("la", [128, 64, 48]); ogB = [kb.sb(f"og{i}", [128, 16, 96]) for i in range(2)]
    for (t_, d_, k_) in ((wa, d_wa, "wa"), (ng, d_ng, "ng"),
                         (tri, d_tri, "tri"), (triC, d_triC, "triC"), (bc, d_bc, "bc"), (bp, d_bp, "bp"), (bc0, d_bc0, "bc0"),
                         (pw, d_pw, "pw"), (psc, d_psc, "psc")):
        kb.dma("sp", t_[:], d_[:, :], w=[k_])
    for (t_, d_, k_) in ((k, d_k, "k"), (v, d_v, "v"), (gr, d_gr, "gr"), (u, d_u, "u")):
        kb.dma("sp", t_[:], d_[:, :, :], w=[k_])
    pss = [kb.ps(f"ps{i}", [128, 512]) for i in range(8)]
    for grp in range(16):
        p_ = pss[6 + grp % 2]; pk = f"ps{6 + grp % 2}"
        q4 = grp // 4; ga = gaB[q4 % 2]
        if grp % 4 == 0:
            kb.dma("sp", ga[:], d_ga[:, q4 * 2048:(q4 + 1) * 2048], w=[f"ga{q4%2}"])
        def mm(grp=grp, p_=p_, ga=ga):
            for i in range(4):
                c = (grp % 4) * 4 + i
                ins = nc.tensor.matmul(p_[:, i * 48:(i + 1) * 48], ga[:, c * 128:(c + 1) * 128], wa[:], start=True, stop=True)
            return ins
        kb.op("pe", mm, r=[f"ga{q4%2}", "wa"], w=[pk])
        kb.op("act", lambda grp=grp, p_=p_: nc.scalar.activation(out=la[:, grp * 4:(grp + 1) * 4, :].rearrange("p c d -> p (c d)"),
                                                                 in_=p_[:, 0:192], func=AF.Sigmoid), r=[pk], w=[f"sg{grp}"])
    kb.op("act", lambda: nc.scalar.activation(out=la[:].rearrange("p c d -> p (c d)"), in_=la[:].rearrange("p c d -> p (c d)"), func=AF.Ln),
          r=[f"sg{g}" for g in range(16)], w=["la"])
    kb.op("act", lambda: nc.scalar.activation(out=gr[:].rearrange("p c d -> p (c d)"), in_=gr[:].rearrange("p c d -> p (c d)"), func=AF.Silu),
          r=["gr"], w=["gr"])
    for c4 in range(4):
        kb.op("pool", lambda c4=c4: nc.gpsimd.tensor_tensor(out=gr[:, c4 * 16:(c4 + 1) * 16, :], in0=gr[:, c4 * 16:(c4 + 1) * 16, :],
                                                              in1=ng[:, None, :].to_broadcast([128, 16, 96]), op=ALU.mult),
              r=["gr", "ng"], w=["gr"])
    pooled = [kb.sb(f"pooled{i}", [64, 512]) for i in range(2)]
    opt = [kb.sb(f"opt{i}", [64, 512]) for i in range(2)]
    for blk in range(16):
        pp = pss[4]; pm = pss[5]
        def mmp(blk=blk):
            for i in range(4):
                t = blk * 4 + i
                o_ = pp[0:64, i * 128:(i + 1) * 128]
                if t == 0:
                    ins = nc.tensor.matmul(o_, u[:, 0, :], bc0[:], start=True, stop=True)
                else:
                    nc.tensor.matmul(o_, u[:, t, :], bc[:], start=True, stop=False)
                    ins = nc.tensor.matmul(o_, u[:, t - 1, :], bp[:], start=False, stop=True)
            return ins
        kb.op("pe", mmp, r=["u", "bc", "bp", "bc0"], w=["ps4"])
        pl = pooled[blk % 2]; plk = f"pooled{blk%2}"
        kb.op("act", lambda pl=pl: nc.scalar.activation(out=pl[:], in_=pp[0:64, :], func=AF.Copy), r=["ps4"], w=[plk])
        kb.op("pe", lambda pl=pl: nc.tensor.matmul(pm[0:64, :], pw[:], pl[:], start=True, stop=True), r=[plk, "pw"], w=["ps5"])
        o_ = opt[blk % 2]; ok = f"opt{blk%2}"
        kb.op("dve", lambda o_=o_: nc.vector.tensor_scalar(out=o_[:], in0=pm[0:64, :], scalar1=psc[:, 0:1], scalar2=None, op0=ALU.mult),
              r=["ps5", "psc"], w=[ok])
        kb.dma("sp", d_op[:, blk * 512:(blk + 1) * 512], o_[:], r=[ok], is_output=True)
    st = [kb.sb(f"st{i}", [48, 96]) for i in range(2)]
    eb = [kb.sb(f"eb{i}", [48, 128]) for i in range(2)]; enb = [kb.sb(f"enb{i}", [48, 128]) for i in range(2)]
    ebl = [kb.sb(f"ebl{i}", [128, 48]) for i in range(2)]
    qi = [kb.sb(f"qi{i}", [48, 128]) for i in range(2)]; ki = [kb.sb(f"ki{i}", [48, 128]) for i in range(2)]
    ko = [kb.sb(f"ko{i}", [128, 48]) for i in range(2)]; at = [kb.sb(f"at{i}", [128, 128]) for i in range(2)]
    osb = [kb.sb(f"osb{i}", [128, 96]) for i in range(2)]; junk = [kb.sb(f"junk{i}", [128, 96]) for i in range(2)]
    ss = [kb.sb(f"ss{i}", [128, 1]) for i in range(2)]; rs = [kb.sb(f"rs{i}", [128, 1]) for i in range(2)]
    kb.op("dve", lambda: nc.vector.memset(st[0][:], 0.0), w=["st0"])
    SC = 1.0 / 16.0
    for c in range(64):
        p = c % 2
        pA = pss[2 * p]; pAk = f"ps{2*p}"; pO = pss[2 * p + 1]; pOk = f"ps{2*p+1}"
        q4 = c // 16; qT = qTB[q4 % 2]; kT = kTB[q4 % 2]; og = ogB[q4 % 2]
        qTk = f"qT{q4%2}"; kTk = f"kT{q4%2}"
        if c % 16 == 0:
            kb.dma("sp", qT[:], d_qT[:, q4 * 2048:(q4 + 1) * 2048], w=[qTk])
            kb.dma("sp", kT[:], d_kT[:, q4 * 2048:(q4 + 1) * 2048], w=[kTk])
        cs = slice((c % 16) * 128, (c % 16 + 1) * 128)
        def mmb(c=c, pA=pA):
            nc.tensor.matmul(pA[0:48, 0:128], la[:, c, :], tri[:], start=True, stop=True)
            return nc.tensor.matmul(pA[:, 128:176], triC[:], la[:, c, :], start=True, stop=True)
        kb.op("pe", mmb, r=["la", "tri", "triC"], w=[pAk + "b"])
        kb.op("act", lambda: nc.scalar.activation(out=eb[p][:], in_=pA[0:48, 0:128], func=AF.Exp, scale=SC), r=[pAk + "b"], w=[f"eb{p}"])
        kb.op("act", lambda: nc.scalar.activation(out=enb[p][:], in_=pA[0:48, 0:128], func=AF.Exp, scale=-SC), r=[pAk + "b"], w=[f"enb{p}"])
        kb.op("act", lambda: nc.scalar.activation(out=ebl[p][:], in_=pA[:, 128:176], func=AF.Exp, scale=SC), r=[pAk + "b"], w=[f"ebl{p}"])
        kb.op("dve", lambda cs=cs: nc.vector.scalar_tensor_tensor(out=qi[p][:], in0=qT[:, cs], scalar=48 ** -0.5, in1=eb[p][:],
                                                                   op0=ALU.mult, op1=ALU.mult), r=[qTk, f"eb{p}"], w=[f"qi{p}"])
        kb.op("pool", lambda cs=cs: nc.gpsimd.tensor_tensor(out=ki[p][:], in0=kT[:, cs], in1=enb[p][:], op=ALU.mult), r=[kTk, f"enb{p}"], w=[f"ki{p}"])
        kb.op("pool", lambda c=c: nc.gpsimd.tensor_tensor(out=ko[p][:], in0=k[:, c, :], in1=ebl[p][:], op=ALU.mult), r=["k", f"ebl{p}"], w=[f"ko{p}"])
        kb.op("pe", lambda: nc.tensor.matmul(pA[:, 384:512], ki[p][:], qi[p][:], start=True, stop=True), r=[f"ki{p}", f"qi{p}"], w=[pAk + "a"])
        kb.op("dve", lambda: nc.vector.tensor_tensor(out=at[p][:], in0=pA[:, 384:512], in1=tri[:], op=ALU.mult), r=[pAk + "a", "tri"], w=[f"at{p}"])
        sin = st[c % 2]; sout = st[(c + 1) % 2]
        def mmo(c=c, pO=pO, sin=sin):
            nc.tensor.matmul(pO[:, 0:96], at[p][:], v[:, c, :], start=True, stop=False)
            return nc.tensor.matmul(pO[:, 0:96], qi[p][:], sin[:], start=False, stop=True)
        kb.op("pe", mmo, r=[f"at{p}", "v", f"qi{p}", f"st{c%2}"], w=[pOk])
        kb.op("pe", lambda c=c: nc.tensor.matmul(pA[0:48, 256:352], ko[p][:], v[:, c, :], start=True, stop=True), r=[f"ko{p}", "v"], w=[pAk + "k"])
        kb.op("dve", lambda sin=sin, sout=sout: nc.vector.scalar_tensor_tensor(out=sout[:], in0=sin[:], scalar=eb[p][:, 127:128], in1=pA[0:48, 256:352],
                                                                             op0=ALU.mult, op1=ALU.add),
              r=[f"st{c%2}", f"eb{p}", pAk + "k"], w=[f"st{(c+1)%2}"])
        kb.op("act", lambda: nc.scalar.activation(out=junk[p][:], in_=pO[:, 0:96], func=AF.Square, accum_out=ss[p][:]), r=[pOk], w=[f"ss{p}", f"junk{p}"])
        kb.op("dve", lambda: nc.vector.tensor_scalar(out=rs[p][:], in0=ss[p][:], scalar1=1.0 / 96.0, scalar2=1e-6, op0=ALU.mult, op1=ALU.add),
              r=[f"ss{p}"], w=[f"rs{p}a"])
        kb.op("act", lambda: nc.scalar.activation(out=ss[p][:], in_=rs[p][:], func=AF.Ln), r=[f"rs{p}a"], w=[f"ss{p}"])
        kb.op("act", lambda: nc.scalar.activation(out=rs[p][:], in_=ss[p][:], func=AF.Exp, scale=-0.5), r=[f"ss{p}"], w=[f"rs{p}"])
        kb.op("dve", lambda c=c, og=og: nc.vector.scalar_tensor_tensor(out=og[:, c % 16, :], in0=pO[:, 0:96], scalar=rs[p][:, 0:1], in1=gr[:, c, :],
                                                                op0=ALU.mult, op1=ALU.mult), r=[pOk, f"rs{p}", "gr"], w=[f"og{q4%2}_{c%16}"])
        if c % 16 == 15:
            kb.dma("sp", d_og[:, q4 * 16:(q4 + 1) * 16, :], og[:], r=[f"og{q4%2}_{i}" for i in range(16)], is_output=True)
    return kb.finish()


def tok_tiles(a):
    return np.ascontiguousarray(a.reshape(64, 128, -1).transpose(1, 0, 2))


def host_G(I, l, z):
    maps = []
    for r in range(8):
        b, h = r // 4, r % 4
        zz = z[b]
        gq = zz[:, 1158 + 48 * h:1158 + 48 * (h + 1)]; gk = zz[:, 1350 + 48 * h:1350 + 48 * (h + 1)]
        gv = zz[:, 1542 + 96 * h:1542 + 96 * (h + 1)]; grr = zz[:, 1926 + 96 * h:1926 + 96 * (h + 1)]
        ga1 = zz[:, 2310:2326]; pu = zz[:, 2326 + 64 * h:2326 + 64 * (h + 1)]
        m = {"ga1T": np.ascontiguousarray(np.concatenate([ga1.T, np.ones((1, 8192), np.float32)], axis=0)),
             "wa2": np.ascontiguousarray(np.concatenate([I['gla_w_a2'][l][:, 48 * h:48 * (h + 1)], I['gla_b_a'][l][None, 48 * h:48 * (h + 1)]], axis=0)),
             "gqT": np.ascontiguousarray(gq.T), "gkT": np.ascontiguousarray(gk.T), "gk": tok_tiles(gk), "gv": tok_tiles(gv), "gr": tok_tiles(grr),
             "normg": np.ascontiguousarray(np.broadcast_to(I['gla_norm_g'][l][None, 96 * h:96 * (h + 1)], (128, 96))),
             "pu": tok_tiles(pu), "poolw": np.ascontiguousarray(I['pool_w'][l][h]),
             "pscale": np.ascontiguousarray(I['pool_scale'][l][64 * h:64 * (h + 1), None])}
        m.update(gla_consts(h))
        maps.append(m)
    return maps


def emit_ln(kb, nc, u, uk, xn, xnk, scr, pfx):
    st, mv, lv, rstd, nmr = scr["st"], scr["mv"], scr["lv"], scr["rstd"], scr["nmr"]
    kb.op("dve", lambda: nc.vector.bn_stats(out=st[:, 0, :], in_=u[:, 0:512]), r=[uk], w=[pfx + "st0"])
    kb.op("dve", lambda: nc.vector.bn_stats(out=st[:, 1, :], in_=u[:, 512:1024]), r=[uk], w=[pfx + "st1"])
    kb.op("dve", lambda: nc.vector.bn_aggr(out=mv[:], in_=st[:].rearrange("p a b -> p (a b)")), r=[pfx + "st0", pfx + "st1"], w=[pfx + "mv"])
    kb.op("act", lambda: nc.scalar.activation(out=lv[:], in_=mv[:, 1:2], func=AF.Ln, bias=scr["eps"][:, 0:1], scale=1.0), r=[pfx + "mv", "eps"], w=[pfx + "lv"])
    kb.op("act", lambda: nc.scalar.activation(out=rstd[:], in_=lv[:], func=AF.Exp, scale=-0.5), r=[pfx + "lv"], w=[pfx + "rstd"])
    kb.op("dve", lambda: nc.vector.scalar_tensor_tensor(out=nmr[:], in0=mv[:, 0:1], scalar=-1.0, in1=rstd[:], op0=ALU.mult, op1=ALU.mult),
          r=[pfx + "mv", pfx + "rstd"], w=[pfx + "nmr"])
    kb.op("act", lambda: nc.scalar.activation(out=xn[:], in_=u[:], func=AF.Identity, scale=rstd[:, 0:1], bias=nmr[:, 0:1]),
          r=[uk, pfx + "rstd", pfx + "nmr"], w=[xnk])


def ln_scratch(kb, pfx):
    return {"st": kb.sb(pfx + "st", [128, 2, 6]), "mv": kb.sb(pfx + "mv", [128, 2]), "lv": kb.sb(pfx + "lv", [128, 1]),
            "rstd": kb.sb(pfx + "rstd", [128, 1]), "nmr": kb.sb(pfx + "nmr", [128, 1])}


def build_C():
    kb = KB()
    nc = kb.nc
    d_mix = kb.dram("mixT", [128, 8, 2048]); d_wo = kb.dram("wout", [128, 8, 1024]); d_x = kb.dram("x", [128, 16, 1024])
    d_rows = {n: kb.dram(n, [128, 1024]) for n in ("g1", "lng", "lnb", "s2", "t2")}
    d_wr = kb.dram("wr", [128, 8, 32]); d_br = kb.dram("br", [128, 32]); d_id = kb.dram("ident", [128, 128])
    d_x1 = kb.dram("x1", [128, 16, 1024], kind="ExternalOutput"); d_h2 = kb.dram("h2", [128, 16, 1024], kind="ExternalOutput")
    d_G = kb.dram("G", [128, 16, 32], kind="ExternalOutput")
    wo = kb.sb("wo", [128, 8, 1024], F32R)
    mix = [kb.sb(f"mix{i}", [128, 8, 512], F32R) for i in range(2)]
    rows = {n: kb.sb("r_" + n, [128, 1024]) for n in d_rows}
    wr = kb.sb("wr", [128, 8, 32]); br = kb.sb("br", [128, 32]); ident = kb.sb("ident", [128, 128])
    eps = kb.sb("eps", [128, 1])
    xt = [kb.sb(f"xt{i}", [128, 1024]) for i in range(2)]
    tmp = [kb.sb(f"tmp{i}", [128, 1024]) for i in range(2)]
    u = [kb.sb(f"u{i}", [128, 1024]) for i in range(2)]
    xn = [kb.sb(f"xn{i}", [128, 1024]) for i in range(2)]
    x1 = [kb.sb(f"x1{i}", [128, 1024]) for i in range(2)]
    h2 = [kb.sb(f"h2{i}", [128, 1024]) for i in range(2)]
    h2T = [kb.sb(f"h2T{i}", [128, 8, 128]) for i in range(2)]
    lg = [kb.sb(f"lg{i}", [128, 32]) for i in range(2)]; top8 = [kb.sb(f"top8{i}", [128, 8]) for i in range(2)]
    msk = [kb.sb(f"msk{i}", [128, 32]) for i in range(2)]; ex = [kb.sb(f"ex{i}", [128, 32]) for i in range(2)]
    nmx = [kb.sb(f"nmx{i}", [128, 1]) for i in range(2)]; den = [kb.sb(f"den{i}", [128, 1]) for i in range(2)]
    Gt = kb.sb("Gt", [128, 16, 32])
    scr = [ln_scratch(kb, f"ln{i}") for i in range(2)]
    for s_ in scr:
        s_["eps"] = eps
    pss = [kb.ps(f"ps{i}", [128, 512]) for i in range(8)]
    kb.op("pool", lambda: nc.gpsimd.memset(eps[:], 1e-5), w=["eps"])
    for k in range(8):
        kb.dma("pool", wo[:, k, :], d_wo[:, k, :], w=[f"wo{k}"])
    for n in d_rows:
        kb.dma("sp", rows[n][:], d_rows[n][:, :], w=["r_" + n])
    kb.dma("sp", wr[:], d_wr[:, :, :], w=["wr"]); kb.dma("sp", br[:], d_br[:, :], w=["br"]); kb.dma("sp", ident[:], d_id[:, :], w=["ident"])
    kb.op("dve", lambda: nc.vector.tensor_scalar_add(out=rows["g1"][:], in0=rows["g1"][:], scalar1=1.0), r=["r_g1"], w=["r_g1"])
    kb.op("dve", lambda: nc.vector.tensor_scalar_add(out=rows["s2"][:], in0=rows["s2"][:], scalar1=1.0), r=["r_s2"], w=["r_s2"])
    wok = [f"wo{k}" for k in range(8)]
    for t in range(16):
        p = t % 2; tb = t // 4; ti = t % 4
        mx = mix[tb % 2]; mxk = f"mix{tb%2}"
        if ti == 0:
            for k in range(8):
                kb.dma("pool", mx[:, k, :], d_mix[:, k, tb * 512:(tb + 1) * 512], w=[mxk + f"_{k}"])
        kb.dma("sp", xt[p][:], d_x[:, t, :], w=[f"xt{p}"])
        for half in range(2):
            ps_ = pss[2 * p + half]; pk = f"ps{2*p+half}"
            def mm(ps_=ps_, half=half, mx=mx, ti=ti):
                for k in range(8):
                    ins = nc.tensor.matmul(ps_[:], mx[:, k, ti * 128:(ti + 1) * 128], wo[:, k, half * 512:(half + 1) * 512],
                                           start=(k == 0), stop=(k == 7))
                return ins
            kb.op("pe", mm, r=[mxk + f"_{k}" for k in range(8)] + wok, w=[pk])
            hs = slice(half * 512, (half + 1) * 512)
            kb.op("dve", lambda ps_=ps_, hs=hs: nc.vector.tensor_tensor(out=tmp[p][:, hs], in0=ps_[:], in1=rows["g1"][:, hs], op=ALU.mult),
                  r=[pk, "r_g1"], w=[f"tmp{p}_{half}"])
        kb.op("dve", lambda: nc.vector.scalar_tensor_tensor(out=u[p][:], in0=xt[p][:], scalar=ALPHA, in1=tmp[p][:], op0=ALU.mult, op1=ALU.add),
              r=[f"xt{p}", f"tmp{p}_0", f"tmp{p}_1"], w=[f"u{p}"])
        emit_ln(kb, nc, u[p], f"u{p}", xn[p], f"xn{p}", scr[p], f"ln{p}")
        kb.op("dve", lambda: nc.vector.tensor_tensor(out=x1[p][:], in0=xn[p][:], in1=rows["lng"][:], op=ALU.mult), r=[f"xn{p}", "r_lng"], w=[f"x1{p}a"])
        kb.op("pool", lambda: nc.gpsimd.tensor_tensor(out=x1[p][:], in0=x1[p][:], in1=rows["lnb"][:], op=ALU.add), r=[f"x1{p}a", "r_lnb"], w=[f"x1{p}"])
        kb.dma("sp", d_x1[:, t, :], x1[p][:], r=[f"x1{p}"], is_output=True)
        kb.op("dve", lambda: nc.vector.tensor_tensor(out=h2[p][:], in0=x1[p][:], in1=rows["s2"][:], op=ALU.mult), r=[f"x1{p}", "r_s2"], w=[f"h2{p}a"])
        kb.op("pool", lambda: nc.gpsimd.tensor_tensor(out=h2[p][:], in0=h2[p][:], in1=rows["t2"][:], op=ALU.add), r=[f"h2{p}a", "r_t2"], w=[f"h2{p}"])
        kb.dma("sp", d_h2[:, t, :], h2[p][:], r=[f"h2{p}"], is_output=True)
        for half in range(2):
            ps_ = pss[4 + half]; pk = f"ps{4+half}"
            def tr(ps_=ps_, half=half):
                for i in range(4):
                    k = half * 4 + i
                    ins = nc.tensor.transpose(ps_[:, i * 128:(i + 1) * 128], h2[p][:, k * 128:(k + 1) * 128], ident[:])
                return ins
            kb.op("pe", tr, r=[f"h2{p}", "ident"], w=[pk])
            kb.op("act", lambda ps_=ps_, half=half: nc.scalar.activation(out=h2T[p][:, half * 4:(half + 1) * 4, :].rearrange("p a b -> p (a b)"),
                                                                         in_=ps_[:], func=AF.Copy), r=[pk], w=[f"h2T{p}_{half}"])
        pl = pss[6 + p]; plk = f"ps{6+p}"
        def mml(pl=pl):
            for k in range(8):
                ins = nc.tensor.matmul(pl[:, 0:32], h2T[p][:, k, :], wr[:, k, :], start=(k == 0), stop=(k == 7))
            return ins
        kb.op("pe", mml, r=[f"h2T{p}_0", f"h2T{p}_1", "wr"], w=[plk])
        kb.op("dve", lambda pl=pl: nc.vector.tensor_tensor(out=lg[p][:], in0=pl[:, 0:32], in1=br[:], op=ALU.add), r=[plk, "br"], w=[f"lg{p}"])
        kb.op("dve", lambda: nc.vector.max(out=top8[p][:], in_=lg[p][:]), r=[f"lg{p}"], w=[f"top8{p}"])
        kb.op("dve", lambda: nc.vector.tensor_scalar(out=msk[p][:], in0=lg[p][:], scalar1=top8[p][:, 3:4], scalar2=None, op0=ALU.is_ge),
              r=[f"lg{p}", f"top8{p}"], w=[f"msk{p}"])
        kb.op("dve", lambda: nc.vector.tensor_scalar_mul(out=nmx[p][:], in0=top8[p][:, 0:1], scalar1=-1.0), r=[f"top8{p}"], w=[f"nmx{p}"])
        kb.op("act", lambda: nc.scalar.activation(out=ex[p][:], in_=lg[p][:], func=AF.Exp, bias=nmx[p][:, 0:1], scale=1.0), r=[f"lg{p}", f"nmx{p}"], w=[f"ex{p}"])
        kb.op("dve", lambda: nc.vector.tensor_tensor(out=ex[p][:], in0=ex[p][:], in1=msk[p][:], op=ALU.mult), r=[f"ex{p}", f"msk{p}"], w=[f"em{p}"])
        kb.op("dve", lambda: nc.vector.reduce_sum(out=den[p][:], in_=ex[p][:], axis=AX.X), r=[f"em{p}"], w=[f"den{p}"])
        kb.op("dve", lambda: nc.vector.reciprocal(out=den[p][:], in_=den[p][:]), r=[f"den{p}"], w=[f"rden{p}"])
        kb.op("dve", lambda t=t: nc.vector.tensor_scalar(out=Gt[:, t, :], in0=ex[p][:], scalar1=den[p][:, 0:1], scalar2=None, op0=ALU.mult),
              r=[f"em{p}", f"rden{p}"], w=[f"G{t}"])
    kb.dma("sp", d_G[:, :, :], Gt[:], r=[f"G{t}" for t in range(16)], is_output=True)
    return kb.finish()


def rep128(v):
    return np.ascontiguousarray(np.broadcast_to(v[None, :], (128, v.shape[0])))


def core_tok_tiles(a):
    return np.ascontiguousarray(a.reshape(16, 128, -1).transpose(1, 0, 2))


def from_core_tok_tiles(a):
    return a.transpose(1, 0, 2).reshape(2048, -1)


def host_C(I, l, x, mix, mods):
    wout = np.ascontiguousarray(I['w_out'][l].reshape(8, 128, 1024).transpose(1, 0, 2))
    wr = np.ascontiguousarray(I['w_router'][l].reshape(8, 128, 32).transpose(1, 0, 2))
    maps = []
    for r in range(8):
        b, j = r // 4, r % 4
        sl = slice(j * 2048, (j + 1) * 2048)
        mixT = np.ascontiguousarray(mix[b, sl].T.reshape(8, 128, 2048).transpose(1, 0, 2))
        md = mods[l, b]
        maps.append({"mixT": mixT, "wout": wout, "x": core_tok_tiles(x[b, sl]),
                     "g1": rep128(md[2048:3072]), "lng": rep128(I['ln1_g'][l]), "lnb": rep128(I['ln1_b'][l]),
                     "s2": rep128(md[4096:5120]), "t2": rep128(md[3072:4096]),
                     "wr": wr, "br": rep128(I['b_router'][l]), "ident": np.eye(128, dtype=np.float32)})
    return maps


def post_C(res):
    def gather(name, d):
        return np.stack([np.concatenate([from_core_tok_tiles(res.results[b * 4 + j][name]) for j in range(4)], axis=0) for b in range(2)])
    return gather("x1", 1024), gather("h2", 1024), gather("G", 32)


CAP = 2816
NT = [(0, 512), (512, 512), (1024, 512), (1536, 512), (2048, 512), (2560, 256)]


def build_D():
    kb = KB()
    nc = kb.nc
    BF = mybir.dt.bfloat16
    d_X = kb.dram("XT", [4, 128, 8, CAP]); d_gate = kb.dram("gate", [4, 128, CAP // 128])
    d_wgu = kb.dram("wgu", [4, 128, 8, 2048]); d_bgu = kb.dram("bgu", [4, 128, 16])
    d_wd = kb.dram("wd", [4, 128, 8, 1024]); d_bd = kb.dram("bd", [4, 128, 1024])
    d_Y = kb.dram("Y", [4, 128, CAP // 128, 1024], kind="ExternalOutput")
    wguB = [kb.sb(f"wgu{i}", [128, 8, 2048], BF) for i in range(2)]; wdB = [kb.sb(f"wd{i}", [128, 8, 1024], BF) for i in range(2)]
    bguB = [kb.sb(f"bgu{i}", [128, 16]) for i in range(2)]; bdB = [kb.sb(f"bd{i}", [128, 1024]) for i in range(2)]
    gateB = [kb.sb(f"gate{i}", [128, CAP // 128]) for i in range(2)]
    XT = [kb.sb(f"XT{i}", [128, 8, 512], BF) for i in range(3)]
    actT = [kb.sb(f"actT{i}", [128, 8, 512], BF) for i in range(2)]
    gl = [kb.sb(f"gl{i}", [128, 512]) for i in range(2)]; l1 = [kb.sb(f"l1{i}", [128, 512]) for i in range(2)]
    sg = [kb.sb(f"sg{i}", [128, 512]) for i in range(2)]
    ysb = [kb.sb(f"ysb{i}", [128, 1024]) for i in range(3)]
    pss = [kb.ps(f"ps{i}", [128, 512]) for i in range(8)]
    st_ = {"xc": 0, "cc": 0, "yc": 0, "pc": 0, "py": 0}

    def load_w(e):
        b = e % 2
        for k in range(8):
            kb.dma("pool", wguB[b][:, k, :], d_wgu[e, :, k, :], w=[f"wgu{b}_{k}"])
        for k in range(8):
            kb.dma("pool", wdB[b][:, k, :], d_wd[e, :, k, :], w=[f"wd{b}_{k}"])
        kb.dma("sp", bguB[b][:], d_bgu[e, :, :], w=[f"bgu{b}"]); kb.dma("sp", bdB[b][:], d_bd[e, :, :], w=[f"bd{b}"])
        kb.dma("sp", gateB[b][:], d_gate[e, :, :], w=[f"gate{b}"])

    def load_x(e, ti):
        n0, nn = NT[ti]
        xi = st_["xc"] % 3; st_["xc"] += 1
        for k in range(8):
            kb.dma("pool", XT[xi][:, k, 0:nn], d_X[e, :, k, n0:n0 + nn], w=[f"XT{xi}_{k}"])
        return xi

    units = [(e, ti) for e in range(4) for ti in range(len(NT))]
    load_w(0)
    xq = [load_x(*units[0])]
    for ui, (e, ti) in enumerate(units):
        b = e % 2
        wgu = wguB[b]; wd = wdB[b]; bgu = bguB[b]; bd = bdB[b]; gate = gateB[b]
        if ti == 0 and e + 1 < 4:
            load_w(e + 1)
        if ui + 1 < len(units):
            xq.append(load_x(*units[ui + 1]))
        xi = xq[ui]
        n0, nn = NT[ti]
        X_ = XT[xi]; Xks = [f"XT{xi}_{k}" for k in range(8)]
        ai = ui % 2; A_ = actT[ai]; Ak = f"actT{ai}"
        wguk = [f"wgu{b}_{k}" for k in range(8)]; wdk = [f"wd{b}_{k}" for k in range(8)]
        for c in range(8):
            pc = st_["pc"]; pg = pss[pc % 4]; pgk = f"ps{pc%4}"; pl = pss[(pc + 1) % 4]; plk = f"ps{(pc+1)%4}"; st_["pc"] += 2
            q = st_["cc"] % 2; st_["cc"] += 1
            def mmg(pg=pg, c=c):
                for k in range(8):
                    ins = nc.tensor.matmul(pg[:, 0:nn], wgu[:, k, c * 128:(c + 1) * 128], X_[:, k, 0:nn], start=(k == 0), stop=(k == 7))
                return ins
            kb.op("pe", mmg, r=wguk + Xks, w=[pgk])
            def mml(pl=pl, c=c):
                for k in range(8):
                    ins = nc.tensor.matmul(pl[:, 0:nn], wgu[:, k, (8 + c) * 128:(9 + c) * 128], X_[:, k, 0:nn], start=(k == 0), stop=(k == 7))
                return ins
            kb.op("pe", mml, r=wguk + Xks, w=[plk])
            kb.op("dve", lambda pg=pg, c=c, q=q: nc.vector.tensor_scalar(out=gl[q][:, 0:nn], in0=pg[:, 0:nn], scalar1=bgu[:, c:c + 1], scalar2=7.0,
                                                                      op0=ALU.add, op1=ALU.min), r=[pgk, f"bgu{b}"], w=[f"gl{q}"])
            kb.op("dve", lambda pl=pl, c=c, q=q: nc.vector.tensor_scalar(out=l1[q][:, 0:nn], in0=pl[:, 0:nn], scalar1=bgu[:, 8 + c:9 + c], scalar2=7.0,
                                                                      op0=ALU.add, op1=ALU.min), r=[plk, f"bgu{b}"], w=[f"l1{q}a"])
            kb.op("act", lambda q=q: nc.scalar.activation(out=sg[q][:, 0:nn], in_=gl[q][:, 0:nn], func=AF.Sigmoid, scale=1.702), r=[f"gl{q}"], w=[f"sg{q}a"])
            kb.op("dve", lambda q=q: nc.vector.tensor_scalar(out=l1[q][:, 0:nn], in0=l1[q][:, 0:nn], scalar1=-7.0, scalar2=1.0,
                                                             op0=ALU.max, op1=ALU.add), r=[f"l1{q}a"], w=[f"l1{q}"])
            kb.op("dve", lambda q=q: nc.vector.tensor_tensor(out=sg[q][:, 0:nn], in0=sg[q][:, 0:nn], in1=gl[q][:, 0:nn], op=ALU.mult),
                  r=[f"sg{q}a", f"gl{q}"], w=[f"sg{q}"])
            kb.op("dve", lambda q=q, c=c: nc.vector.tensor_tensor(out=A_[:, c, 0:nn], in0=sg[q][:, 0:nn], in1=l1[q][:, 0:nn], op=ALU.mult),
                  r=[f"sg{q}", f"l1{q}"], w=[Ak + f"_{c}"])
        Aks = [Ak + f"_{c}" for c in range(8)]
        for si in range(nn // 128):
            s = n0 // 128 + si
            yi = st_["yc"] % 3; st_["yc"] += 1
            y_ = ysb[yi]; yk = f"ysb{yi}"
            for half in range(2):
                pyi = 4 + st_["py"] % 4; st_["py"] += 1
                py = pss[pyi]; pyk = f"ps{pyi}"
                def mmy(py=py, half=half, si=si):
                    for k in range(8):
                        ins = nc.tensor.matmul(py[:], A_[:, k, si * 128:(si + 1) * 128], wd[:, k, half * 512:(half + 1) * 512], start=(k == 0), stop=(k == 7))
                    return ins
                kb.op("pe", mmy, r=Aks + wdk, w=[pyk])
                hs = slice(half * 512, (half + 1) * 512)
                kb.op("dve", lambda py=py, hs=hs: nc.vector.tensor_tensor(out=y_[:, hs], in0=py[:], in1=bd[:, hs], op=ALU.add), r=[pyk, f"bd{b}"], w=[yk + f"a{half}"])
            kb.op("act", lambda s=s: nc.scalar.activation(out=y_[:], in_=y_[:], func=AF.Copy, scale=gate[:, s:s + 1]),
                  r=[yk + "a0", yk + "a1", f"gate{b}"], w=[yk])
            kb.dma("sp", d_Y[e, :, s, :], y_[:], r=[yk], is_output=True)
    return kb.finish()


def route_host(G):
    return [np.nonzero(G[:, e] > 0)[0] for e in range(32)]


def host_D(I, l, h2f, G, idxs, off=0):
    maps = []
    for r in range(8):
        XT = np.zeros((4, 128, 8, CAP), np.float32); gate = np.zeros((4, 128, CAP // 128), np.float32)
        for i in range(4):
            e = 4 * r + i
            idx = idxs[e][off:off + CAP]
            n = len(idx)
            rowsT = h2f[idx].T
            XT[i, :, :, :n] = rowsT.reshape(8, 128, n).transpose(1, 0, 2)
            gg = np.zeros(CAP, np.float32); gg[:n] = G[idx, e]
            gate[i] = gg.reshape(CAP // 128, 128).T
        es = slice(4 * r, 4 * r + 4)
        wgu = np.ascontiguousarray(I['w_gate_up'][l][es].reshape(4, 8, 128, 2048).transpose(0, 2, 1, 3))
        wd = np.ascontiguousarray(I['w_down'][l][es].reshape(4, 8, 128, 1024).transpose(0, 2, 1, 3))
        bgu = np.ascontiguousarray(I['b_gate_up'][l][es].reshape(4, 16, 128).transpose(0, 2, 1))
        bd = np.ascontiguousarray(np.broadcast_to(I['b_down'][l][es][:, None, :], (4, 128, 1024)))
        maps.append({"XT": XT, "gate": gate, "wgu": wgu, "bgu": bgu, "wd": wd, "bd": bd})
    return maps


def post_D(res, idxs, Y4=None, fill=None, off=0):
    if Y4 is None:
        Y4 = np.zeros((16384, 4, 1024), np.float32)
        fill = np.zeros(16384, np.int64)
    for r in range(8):
        Y = res.results[r]["Y"]
        for i in range(4):
            e = 4 * r + i
            idx = idxs[e][off:off + CAP]
            rows = Y[i].transpose(1, 0, 2).reshape(CAP, 1024)[:len(idx)]
            Y4[idx, fill[idx]] = rows
            fill[idx] += 1
    return Y4, fill


def build_E():
    kb = KB()
    nc = kb.nc
    d_x1 = kb.dram("x1", [128, 16, 1024]); d_Y4 = kb.dram("Y4", [128, 16, 4, 1024])
    d_rows = {n: kb.dram(n, [128, 1024]) for n in ("g2", "lng", "lnb")}
    d_x2 = kb.dram("x2", [128, 16, 1024], kind="ExternalOutput")
    rows = {n: kb.sb("r_" + n, [128, 1024]) for n in d_rows}
    eps = kb.sb("eps", [128, 1])
    xt = [kb.sb(f"xt{i}", [128, 1024]) for i in range(2)]
    y4 = [kb.sb(f"y4{i}", [128, 4, 1024]) for i in range(2)]
    sa = [kb.sb(f"sa{i}", [128, 1024]) for i in range(2)]; sb_ = [kb.sb(f"sb{i}", [128, 1024]) for i in range(2)]
    u = [kb.sb(f"u{i}", [128, 1024]) for i in range(2)]; xn = [kb.sb(f"xn{i}", [128, 1024]) for i in range(2)]
    scr = [ln_scratch(kb, f"ln{i}") for i in range(2)]
    for s_ in scr:
        s_["eps"] = eps
    kb.op("pool", lambda: nc.gpsimd.memset(eps[:], 1e-5), w=["eps"])
    for n in d_rows:
        kb.dma("sp", rows[n][:], d_rows[n][:, :], w=["r_" + n])
    kb.op("dve", lambda: nc.vector.tensor_scalar_add(out=rows["g2"][:], in0=rows["g2"][:], scalar1=1.0), r=["r_g2"], w=["r_g2"])
    for t in range(16):
        p = t % 2
        kb.dma("sp", xt[p][:], d_x1[:, t, :], w=[f"xt{p}"])
        kb.dma("sp", y4[p][:], d_Y4[:, t, :, :], w=[f"y4{p}"])
        kb.op("dve", lambda: nc.vector.tensor_tensor(out=sa[p][:], in0=y4[p][:, 0, :], in1=y4[p][:, 1, :], op=ALU.add), r=[f"y4{p}"], w=[f"sa{p}a"])
        kb.op("pool", lambda: nc.gpsimd.tensor_tensor(out=sb_[p][:], in0=y4[p][:, 2, :], in1=y4[p][:, 3, :], op=ALU.add), r=[f"y4{p}"], w=[f"sb{p}"])
        kb.op("pool", lambda: nc.gpsimd.tensor_tensor(out=sa[p][:], in0=sa[p][:], in1=sb_[p][:], op=ALU.add), r=[f"sa{p}a", f"sb{p}"], w=[f"sa{p}b"])
        kb.op("pool", lambda: nc.gpsimd.tensor_tensor(out=sa[p][:], in0=sa[p][:], in1=rows["g2"][:], op=ALU.mult), r=[f"sa{p}b", "r_g2"], w=[f"sa{p}"])
        kb.op("dve", lambda: nc.vector.scalar_tensor_tensor(out=u[p][:], in0=xt[p][:], scalar=ALPHA, in1=sa[p][:], op0=ALU.mult, op1=ALU.add),
              r=[f"xt{p}", f"sa{p}"], w=[f"u{p}"])
        emit_ln(kb, nc, u[p], f"u{p}", xn[p], f"xn{p}", scr[p], f"ln{p}")
        kb.op("dve", lambda: nc.vector.tensor_tensor(out=xn[p][:], in0=xn[p][:], in1=rows["lng"][:], op=ALU.mult), r=[f"xn{p}", "r_lng"], w=[f"xn{p}b"])
        kb.op("pool", lambda: nc.gpsimd.tensor_tensor(out=xn[p][:], in0=xn[p][:], in1=rows["lnb"][:], op=ALU.add), r=[f"xn{p}b", "r_lnb"], w=[f"xn{p}c"])
        kb.dma("sp", d_x2[:, t, :], xn[p][:], r=[f"xn{p}c"], is_output=True)
    return kb.finish()


def host_E(I, l, x1, Y4, mods):
    maps = []
    for r in range(8):
        b, j = r // 4, r % 4
        sl = slice(j * 2048, (j + 1) * 2048)
        y = Y4[b * 8192 + j * 2048: b * 8192 + (j + 1) * 2048]
        maps.append({"x1": core_tok_tiles(x1[b, sl]),
                     "Y4": np.ascontiguousarray(y.reshape(16, 128, 4, 1024).transpose(1, 0, 2, 3)),
                     "g2": rep128(mods[l, b, 5120:6144]), "lng": rep128(I['ln2_g'][l]), "lnb": rep128(I['ln2_b'][l])})
    return maps


def post_E(res):
    return np.stack([np.concatenate([from_core_tok_tiles(res.results[b * 4 + j]["x2"]) for j in range(4)], axis=0) for b in range(2)])


_PROGS = {}


def _prog(name, fn):
    if name not in _PROGS:
        _PROGS[name] = fn()
    return _PROGS[name]


def kernel(**I):
    I = {k: np.asarray(v) for k, v in I.items()}
    x = np.ascontiguousarray(I['x'], dtype=np.float32)
    mods = post_M(run(_prog("M", build_M), host_M(I)))
    pairs_all = [(b, h) for b in range(2) for h in range(6)]
    for l in range(4):
        z = post_A(run(_prog("A", build_A), host_A(I, l, x, mods)))
        mix = np.zeros((2, 8192, 1024), np.float32)
        for grp in (pairs_all[0:8], pairs_all[8:12] + pairs_all[8:12]):
            res = run(_prog("B", build_B), host_B(z, grp))
            for i, (b, h) in enumerate(grp):
                mix[b, :, h * 64:(h + 1) * 64] = res.results[i]["oT"].T
        res = run(_prog("G", build_G), host_G(I, l, z))
        for r in range(8):
            b, h = r // 4, r % 4
            mix[b, :, 384 + h * 96:384 + (h + 1) * 96] = res.results[r]["og"].transpose(1, 0, 2).reshape(8192, 96)
            mix[b, :, 768 + h * 64:768 + (h + 1) * 64] = res.results[r]["opT"].T
        del z
        x1, h2, G = post_C(run(_prog("C", build_C), host_C(I, l, x, mix, mods)))
        Gf = G.reshape(-1, 32); h2f = h2.reshape(-1, 1024)
        idxs = route_host(Gf)
        Y4 = None; fill = None; off = 0
        nmax = max(len(i) for i in idxs)
        while True:
            res = run(_prog("D", build_D), host_D(I, l, h2f, Gf, idxs, off))
            Y4, fill = post_D(res, idxs, Y4, fill, off)
            off += CAP
            if off >= nmax:
                break
        x = post_E(run(_prog("E", build_E), host_E(I, l, x1, Y4, mods)))
    return np.ascontiguousarray(x, dtype=np.float32)
```

```python
import numpy as np
import concourse.bass as bass
import concourse.mybir as mybir
from concourse.bass_utils import run_bass_kernel_spmd

F32 = mybir.dt.float32
F32R = mybir.dt.float32r
I32 = mybir.dt.int32
AF = mybir.ActivationFunctionType
ALU = mybir.AluOpType
AX = mybir.AxisListType


class KB:
    NDMA = 24

    def __init__(self):
        nc = bass.Bass("TRN2", target_bir_lowering=False)
        self.nc = nc
        self.eng = {"pe": nc.tensor, "act": nc.scalar, "dve": nc.vector, "pool": nc.gpsimd, "sp": nc.sync}
        self.psem = {}
        self.pcnt = {}
        for e in ("pe", "act", "dve", "pool"):
            self.psem[e] = [nc.alloc_semaphore(f"prog_{e}")]
            self.pcnt[e] = 0
        self.dsem = [nc.alloc_semaphore(f"dma_{i}") for i in range(self.NDMA)]
        self.dcnt = [0] * self.NDMA
        self.dnext = 0
        self.seen = {e: {} for e in self.eng}
        self.buf = {}
        self.out_toks = []
        self._ctx = []

    def dram(self, name, shape, dt=F32, kind="ExternalInput"):
        return self.nc.dram_tensor(name, list(shape), dt, kind=kind).ap()

    def sb(self, name, shape, dt=F32):
        g = self.nc.sbuf_tensor("sb_" + name, list(shape), dt)
        t = g.__enter__()
        self._ctx.append(g)
        return t

    def ps(self, name, shape, dt=F32):
        g = self.nc.psum_tensor("pp_" + name, list(shape), dt)
        t = g.__enter__()
        self._ctx.append(g)
        return t

    def _wait(self, e, toks, raw_keys_same_engine=True):
        eng = self.eng[e]
        best = {}
        for t in toks:
            if t is None:
                continue
            sem, val, prod = t
            k = id(sem)
            if self.seen[e].get(k, 0) >= val:
                continue
            if k not in best or best[k][1] < val:
                best[k] = (sem, val)
        for k, (sem, val) in best.items():
            eng.wait_ge(sem, val)
            self.seen[e][k] = val

    def _deps(self, e, r, w):
        toks = []
        for k in r:
            b = self.buf.get(k)
            if b and b["w"] is not None:
                toks.append(b["w"])
        for k in w:
            b = self.buf.get(k)
            if b:
                if b["w"] is not None and (e == "dma" or b["w"][2] != e):
                    toks.append(b["w"])
                for pe_, t in b["r"].items():
                    if e == "dma" or isinstance(pe_, tuple) or pe_ != e:
                        toks.append(t)
        return toks

    def _record(self, tok, r, w):
        e = tok[2]
        for k in r:
            b = self.buf.setdefault(k, {"w": None, "r": {}})
            b["r"][e if e != "dma" else ("dma", id(tok[0]))] = tok
        for k in w:
            self.buf[k] = {"w": tok, "r": {}}

    def op(self, e, fn, r=(), w=()):
        self._wait(e, self._deps(e, r, w))
        ins = fn()
        self.pcnt[e] += 1
        sem = self.psem[e][-1]
        ins.then_inc(sem, 1)
        tok = (sem, self.pcnt[e], e)
        self._record(tok, r, w)
        return tok

    def dma(self, e, out, in_, r=(), w=(), is_output=False, **kw):
        slot = self.dnext
        self.dnext = (self.dnext + 1) % self.NDMA
        sem = self.dsem[slot]
        toks = self._deps("dma", r, w)
        if self.dcnt[slot] > 0:
            toks.append((sem, self.dcnt[slot], "dma"))
        self._wait(e, toks)
        ins = self.eng[e].dma_start(out=out, in_=in_, **kw)
        self.dcnt[slot] += 16
        ins.then_inc(sem, 16)
        tok = (sem, self.dcnt[slot], "dma")
        self._record(tok, r, w)
        if is_output:
            self.out_toks.append(tok)
        return tok

    def finish(self):
        toks = list(self.out_toks)
        for i, s in enumerate(self.dsem):
            if self.dcnt[i] > 0:
                toks.append((s, self.dcnt[i], "dma"))
        self._wait("sp", toks)
        toks = []
        for e in ("pe", "act", "dve", "pool"):
            if self.pcnt[e] > 0:
                toks.append((self.psem[e][-1], self.pcnt[e], e))
        self._wait("sp", toks)
        for g in reversed(self._ctx):
            g.__exit__(None, None, None)
        self._ctx = []
        return self.nc


def run(nc, in_maps, trace=False):
    res = run_bass_kernel_spmd(nc, in_maps, core_ids=list(range(len(in_maps))), trace=trace)
    return res


D = 1024
NIN = 2582
ALPHA = 8 ** 0.25


def build_M():
    kb = KB()
    nc = kb.nc
    cT = kb.dram("cT", [128, 8, 2])
    Wd = kb.dram("W", [128, 8, 3072])
    bd = kb.dram("bias", [128, 24])
    od = kb.dram("modT", [128, 24, 2], kind="ExternalOutput")
    ct = kb.sb("ct", [128, 8, 2])
    cs = kb.sb("cs", [128, 8, 2])
    Wt = kb.sb("Wt", [128, 8, 3072])
    bt = kb.sb("bt", [128, 24])
    ot = kb.sb("ot", [128, 24, 2])
    ps = kb.ps("ps", [128, 512])
    kb.dma("sp", ct[:], cT[:, :, :], w=["ct"])
    kb.dma("sp", bt[:], bd[:, :], w=["bt"])
    for k in range(8):
        kb.dma("sp", Wt[:, k, :], Wd[:, k, :], w=[f"W{k}"])
    kb.op("act", lambda: nc.scalar.activation(out=cs[:], in_=ct[:], func=AF.Silu), r=["ct"], w=["cs"])
    for j in range(24):
        def mm(j=j):
            for k in range(8):
                ins = nc.tensor.matmul(ps[:, 2 * j:2 * j + 2], Wt[:, k, j * 128:(j + 1) * 128], cs[:, k, :],
                                       start=(k == 0), stop=(k == 7))
            return ins
        kb.op("pe", mm, r=["cs"] + [f"W{k}" for k in range(8)], w=["ps"])
    for b in range(2):
        kb.op("dve", lambda b=b: nc.vector.tensor_tensor(out=ot[:, :, b], in0=ps[:, b:48:2], in1=bt[:], op=ALU.add),
              r=["ps", "bt"], w=[f"ot{b}"])
    kb.dma("sp", od[:, :, :], ot[:], r=["ot0", "ot1"], is_output=True)
    return kb.finish()


def host_M(I):
    Wall = np.concatenate([I['w_ada'][l] for l in range(4)], axis=1)
    ball = np.concatenate([I['b_ada'][l] for l in range(4)], axis=0)
    cT = np.ascontiguousarray(I['c'].T.reshape(8, 128, 2).transpose(1, 0, 2))
    maps = []
    for r in range(8):
        W = np.ascontiguousarray(Wall[:, r * 3072:(r + 1) * 3072].reshape(8, 128, 3072).transpose(1, 0, 2))
        bb = np.ascontiguousarray(ball[r * 3072:(r + 1) * 3072].reshape(24, 128).T)
        maps.append({"cT": cT, "W": W, "bias": bb})
    return maps


def post_M(res):
    cols = []
    for r in range(8):
        m = res.results[r]["modT"]
        cols.append(m.transpose(2, 1, 0).reshape(2, 3072))
    allm = np.concatenate(cols, axis=1)
    return allm.reshape(2, 4, 6144).transpose(1, 0, 2)


GROUPS = [(0, 512), (512, 512), (1024, 512), (1536, 512), (2048, 512), (2560, 22)]


def build_A():
    kb = KB()
    nc = kb.nc
    xT = kb.dram("xT", [128, 8, 2048])
    scd = kb.dram("sc", [128, 8])
    shd = kb.dram("sh", [128, 8])
    wd = kb.dram("w", [128, 8, NIN])
    bd = kb.dram("bias", [128, NIN])
    zd = kb.dram("z", [2048, NIN], kind="ExternalOutput")
    sc = kb.sb("sc", [128, 8]); sc1 = kb.sb("sc1", [128, 8]); sh = kb.sb("sh", [128, 8])
    wt = kb.sb("wt", [128, 8, NIN], F32R)
    bt = kb.sb("bt", [128, NIN])
    xb = [kb.sb(f"xb{i}", [128, 8, 512]) for i in range(2)]
    hT = [kb.sb(f"hT{i}", [128, 8, 512], F32R) for i in range(2)]
    zt = [kb.sb(f"zt{i}", [128, NIN]) for i in range(2)]
    pss = [kb.ps(f"ps{i}", [128, 512]) for i in range(4)]
    kb.dma("sp", sc[:], scd[:, :], w=["sc"])
    kb.dma("sp", sh[:], shd[:, :], w=["sh"])
    kb.dma("sp", bt[:], bd[:, :], w=["bt"])
    for gi, (c0, n) in enumerate(GROUPS):
        for k in range(8):
            kb.dma("pool", wt[:, k, c0:c0 + n], wd[:, k, c0:c0 + n], w=[f"w{k}_{gi}"])
    kb.op("dve", lambda: nc.vector.tensor_scalar_add(out=sc1[:], in0=sc[:], scalar1=1.0), r=["sc"], w=["sc1"])
    pcnt = 0
    for tb in range(4):
        x_ = xb[tb % 2]; h_ = hT[tb % 2]
        kb.dma("sp", x_[:], xT[:, :, tb * 512:(tb + 1) * 512], w=[f"xb{tb%2}"])
        for k in range(8):
            kb.op("act", lambda k=k: nc.scalar.activation(out=h_[:, k, :], in_=x_[:, k, :], func=AF.Identity,
                                                          scale=sc1[:, k:k + 1], bias=sh[:, k:k + 1]),
                  r=[f"xb{tb%2}", "sc1", "sh"], w=[f"hT{tb%2}_{k}"])
        for ti in range(4):
            tile = tb * 4 + ti
            z_ = zt[tile % 2]
            for gi, (c0, n) in enumerate(GROUPS):
                p_ = pss[pcnt % 4]; pk = f"ps{pcnt%4}"; pcnt += 1
                def mm(p_=p_, c0=c0, n=n, ti=ti):
                    for k in range(8):
                        ins = nc.tensor.matmul(p_[:, 0:n], h_[:, k, ti * 128:(ti + 1) * 128], wt[:, k, c0:c0 + n],
                                               start=(k == 0), stop=(k == 7))
                    return ins
                kb.op("pe", mm, r=[f"hT{tb%2}_{k}" for k in range(8)] + [f"w{k}_{gi}" for k in range(8)], w=[pk])
                kb.op("dve", lambda p_=p_, c0=c0, n=n: nc.vector.tensor_tensor(out=z_[:, c0:c0 + n], in0=p_[:, 0:n],
                                                                               in1=bt[:, c0:c0 + n], op=ALU.add),
                      r=[pk, "bt"], w=[f"zt{tile%2}_{gi}"])
            kb.dma("sp", zd[tile * 128:(tile + 1) * 128, :], z_[:], r=[f"zt{tile%2}_{gi}" for gi in range(6)],
                   is_output=True)
    return kb.finish()


def cols128(v):
    return np.ascontiguousarray(v.reshape(8, 128).T)


def host_A(I, l, x, mods):
    maps = []
    w = np.ascontiguousarray(I['w_in'][l].reshape(8, 128, NIN).transpose(1, 0, 2))
    bias = np.ascontiguousarray(np.broadcast_to(I['b_in'][l][None, :], (128, NIN)))
    for r in range(8):
        b, j = r // 4, r % 4
        xs = x[b, j * 2048:(j + 1) * 2048, :]
        xT = np.ascontiguousarray(xs.T.reshape(8, 128, 2048).transpose(1, 0, 2))
        maps.append({"xT": xT, "sc": cols128(mods[l, b, 1024:2048]), "sh": cols128(mods[l, b, 0:1024]),
                     "w": w, "bias": bias})
    return maps


def post_A(res):
    z = np.stack([np.concatenate([res.results[b * 4 + j]["z"] for j in range(4)], axis=0) for b in range(2)])
    return z


def fox_consts():
    s = np.arange(128)[:, None]; m = np.arange(128)[None, :]
    tri = (s <= m).astype(np.float32)
    ones = np.ones((128, 128), np.float32)
    ident = np.eye(128, dtype=np.float32)
    t = np.arange(512)[None, None, :]; d = np.arange(4)[None, :, None]; ss = np.arange(128)[:, None, None]
    maskadd = np.where(128 * d + ss <= t, 0.0, -30000.0).astype(np.float32)
    return {"tri": tri, "ones": ones, "ident": ident, "maskadd": np.ascontiguousarray(maskadd)}


def build_B(half=False):
    kb = KB()
    nc = kb.nc
    S = 8192
    NQ = 8 if half else 16
    NM = 8 if half else 4
    SQ = NQ * 512
    qd = kb.dram("qT", [64, SQ]); kd = kb.dram("kT", [64, S]); vd = kb.dram("v", [128, 64, 64]); fd = kb.dram("ff", [128, 64])
    trid = kb.dram("tri", [128, 128]); onesd = kb.dram("ones", [128, 128]); identd = kb.dram("ident", [128, 128])
    maskd = kb.dram("maskadd", [128, NM, 512])
    od = kb.dram("oT", [64, SQ], kind="ExternalOutput")
    qa = kb.sb("qa", [66, SQ], F32R); ka = kb.sb("ka", [66, S], F32R); va = kb.sb("va", [128, 64, 65], F32R)
    stg = kb.sb("stg", [128, 4224])
    tri = kb.sb("tri", [128, 128]); ones = kb.sb("ones", [128, 128]); ident = kb.sb("ident", [128, 128])
    identR = kb.sb("identR", [128, 128], F32R)
    maskadd = kb.sb("maskadd", [128, NM, 512])
    ff = kb.sb("ff", [128, 64]); sg = kb.sb("sg", [128, 64]); ls = kb.sb("ls", [128, 64])
    tot = kb.sb("tot", [128, 64]); incl = kb.sb("incl", [128, 64]); tmpc = kb.sb("tmpc", [128, 64])
    Fc = kb.sb("Fc", [128, 64]); negF = kb.sb("negF", [128, 64]); F8 = kb.sb("F8", [128, 64]); lo8 = kb.sb("lo8", [128, 64])
    NX = NQ * 4
    X = kb.sb("X", [128, NX, 66], F32R)
    F8s = kb.sb("F8s", [128, 32])
    if half:
        w0d = kb.dram("w0", [128, 1]); w1d = kb.dram("w1", [128, 1])
        w0 = kb.sb("w0", [128, 1]); w1 = kb.sb("w1", [128, 1])
        kb.dma("sp", w0[:], w0d[:, :], w=["w0"]); kb.dma("sp", w1[:], w1d[:, :], w=["w1"])
    P = [kb.sb(f"P{i}", [128, 512], F32R) for i in range(6)]
    tmpd = [kb.sb(f"tmpd{i}", [128, 512]) for i in range(2)]
    Osb = [kb.sb(f"Osb{i}", [65, 512]) for i in range(2)]
    rec = [kb.sb(f"rec{i}", [65, 512]) for i in range(2)]
    ot = [kb.sb(f"ot{i}", [65, 512]) for i in range(2)]
    psS = [kb.ps(f"psS{i}", [128, 512]) for i in range(4)]
    psO = [kb.ps(f"psO{i}", [128, 512]) for i in range(2)]
    psM = [kb.ps(f"psM{i}", [128, 512]) for i in range(2)]

    for (t_, d_, k_) in ((tri, trid, "tri"), (ones, onesd, "ones"), (ident, identd, "ident")):
        kb.dma("sp", t_[:], d_[:, :], w=[k_])
    kb.dma("sp", maskadd[:], maskd[:, :, :], w=["maskadd"])
    kb.dma("sp", ff[:], fd[:, :], w=["ff"])
    kb.op("dve", lambda: nc.vector.tensor_copy(out=identR[:], in_=ident[:]), r=["ident"], w=["identR"])
    kb.op("pool", lambda: nc.gpsimd.memset(stg[:], 1.0), w=["stg"])
    for hf in range(2):
        kb.op("dve", lambda hf=hf: nc.vector.tensor_copy(out=ka[64:66, hf * 4096:(hf + 1) * 4096], in_=stg[64:66, 0:4096]), r=["stg"], w=["ka_hi"])
    kb.op("dve", lambda: nc.vector.tensor_copy(out=va[:, :, 0], in_=stg[:, 0:64]), r=["stg"], w=["va_1"])
    kb.op("pool", lambda: nc.gpsimd.memset(stg[:], 0.0), r=[], w=["stg"])
    kb.op("dve", lambda: nc.vector.tensor_copy(out=X[:].rearrange("p t c -> p (t c)"), in_=stg[:, 0:NX * 66]), r=["stg"], w=["X"])
    for nm, src, dst in (("q", qd, qa), ("k", kd, ka)):
        for hf in range(1 if (half and nm == "q") else 2):
            kb.dma("sp", stg[0:64, 0:4096], src[:, hf * 4096:(hf + 1) * 4096], w=["stg"])
            kb.op("dve", lambda dst=dst, hf=hf: nc.vector.tensor_copy(out=dst[0:64, hf * 4096:(hf + 1) * 4096], in_=stg[0:64, 0:4096]),
                  r=["stg"], w=[f"{nm}a_lo"])
    kb.dma("sp", stg[:, 0:4096], vd.rearrange("p t d -> p (t d)"), w=["stg"])
    kb.op("dve", lambda: nc.vector.tensor_copy(out=va[:, :, 1:65], in_=stg[:, 0:4096].rearrange("p (t d) -> p t d", d=64)),
          r=["stg"], w=["va_v"])
    kb.op("act", lambda: nc.scalar.activation(out=sg[:], in_=ff[:], func=AF.Sigmoid), r=["ff"], w=["sg"])
    kb.op("act", lambda: nc.scalar.activation(out=ls[:], in_=sg[:], func=AF.Ln), r=["sg"], w=["ls"])
    def mmc():
        nc.tensor.matmul(psM[0][:, 0:64], tri[:], ls[:], start=True, stop=True)
        return nc.tensor.matmul(psM[0][:, 64:128], ones[:], ls[:], start=True, stop=True)
    kb.op("pe", mmc, r=["tri", "ones", "ls"], w=["psM0"])
    kb.op("dve", lambda: nc.vector.tensor_copy(out=tot[:], in_=psM[0][:, 64:128]), r=["psM0"], w=["tot"])
    kb.op("dve", lambda: nc.vector.tensor_tensor_scan(out=incl[:], data0=ones[:, 0:64], data1=tot[:], initial=0.0,
                                                      op0=ALU.mult, op1=ALU.add), r=["tot", "ones"], w=["incl"])
    kb.op("dve", lambda: nc.vector.tensor_tensor(out=tmpc[:], in0=incl[:], in1=tot[:], op=ALU.subtract), r=["incl", "tot"], w=["tmpc"])
    kb.op("dve", lambda: nc.vector.tensor_tensor(out=Fc[:], in0=psM[0][:, 0:64], in1=tmpc[:], op=ALU.add), r=["psM0", "tmpc"], w=["Fc"])
    kb.op("dve", lambda: nc.vector.tensor_scalar_mul(out=negF[:], in0=Fc[:], scalar1=-1.0), r=["Fc"], w=["negF"])
    kb.op("dve", lambda: nc.vector.tensor_scalar_mul(out=F8[:], in0=Fc[:], scalar1=8.0), r=["Fc"], w=["F8"])
    if half:
        F8v = F8[:].rearrange("p (i two f) -> p i two f", two=2, f=4)
        F8sv = F8s[:].rearrange("p (i f) -> p i f", f=4)
        kb.op("dve", lambda: nc.vector.tensor_scalar(out=F8sv, in0=F8v[:, :, 0, :], scalar1=w0[:, 0:1], scalar2=None, op0=ALU.mult), r=["F8", "w0"], w=["F8sa"])
        kb.op("dve", lambda: nc.vector.scalar_tensor_tensor(out=F8sv, in0=F8v[:, :, 1, :], scalar=w1[:, 0:1], in1=F8sv, op0=ALU.mult, op1=ALU.add),
              r=["F8", "w1", "F8sa"], w=["F8s"])
        Fq = F8s[:, 0:NX]; Fqk = "F8s"
    else:
        Fq = F8[:, 0:NX]; Fqk = "F8"
    kb.op("dve", lambda: nc.vector.tensor_copy(out=X[:, :, 64], in_=Fq), r=[Fqk, "X"], w=["Xhi"])
    kb.op("dve", lambda: nc.vector.tensor_tensor(out=lo8[:, 0:NX], in0=Fq, in1=X[:, :, 64].bitcast(F32), op=ALU.subtract),
          r=[Fqk, "Xhi"], w=["lo8"])
    kb.op("dve", lambda: nc.vector.tensor_copy(out=X[:, :, 65], in_=lo8[:, 0:NX]), r=["lo8", "X"], w=["Xlo"])
    for blk in range(NQ):
        pm = psM[1]
        def mmx(blk=blk):
            for i in range(4):
                t = blk * 4 + i
                ins = nc.tensor.matmul(pm[0:66, i * 128:(i + 1) * 128], X[:, t, :], identR[:], start=True, stop=True)
            return ins
        kb.op("pe", mmx, r=["X", "Xhi", "Xlo", "identR"], w=["psM1"])
        kb.op("act", lambda blk=blk: nc.scalar.activation(out=qa[64:66, blk * 512:(blk + 1) * 512], in_=pm[64:66, :], func=AF.Copy),
              r=["psM1"], w=[f"qa_hi{blk}"])
    def nkof(qb):
        return 8 * qb + 8 if half else 4 * qb + 4

    def slot(qb, kt):
        return kt - 8 * qb if half else kt - 4 * qb

    tiles = [(qb, kt) for qb in range(NQ) for kt in range(nkof(qb))]
    LOOK = 3
    st_ = {"dc": 0}

    def emit_qk(i):
        qb, kt = tiles[i]
        pS = psS[i % 4]; pSk = f"psS{i%4}"; P_ = P[i % 6]; Pk = f"P{i%6}"
        kb.op("pe", lambda: nc.tensor.matmul(pS[:], ka[:, kt * 128:(kt + 1) * 128], qa[:, qb * 512:(qb + 1) * 512], start=True, stop=True),
              r=["qa_lo", "ka_lo", "ka_hi", f"qa_hi{qb}"], w=[pSk])
        d = slot(qb, kt)
        if d >= 0:
            dc = st_["dc"]; st_["dc"] += 1
            td = tmpd[dc % 2]; tdk = f"tmpd{dc%2}"
            kb.op("dve", lambda: nc.vector.scalar_tensor_tensor(out=td[:], in0=pS[:], scalar=0.125, in1=maskadd[:, d, :], op0=ALU.mult, op1=ALU.add),
                  r=[pSk, "maskadd"], w=[tdk])
            kb.op("act", lambda: nc.scalar.activation(out=P_[:], in_=td[:], func=AF.Exp, bias=negF[:, kt:kt + 1], scale=1.0), r=[tdk, "negF"], w=[Pk])
        else:
            kb.op("act", lambda: nc.scalar.activation(out=P_[:], in_=pS[:], func=AF.Exp, bias=negF[:, kt:kt + 1], scale=0.125), r=[pSk, "negF"], w=[Pk])

    def emit_pv(i):
        qb, kt = tiles[i]
        nk = nkof(qb)
        P_ = P[i % 6]; Pk = f"P{i%6}"
        pO = psO[qb % 2]; pOk = f"psO{qb%2}"
        kb.op("pe", lambda: nc.tensor.matmul(pO[0:65, :], va[:, kt, :], P_[:], start=(kt == 0), stop=(kt == nk - 1)), r=[Pk, "va_v", "va_1"], w=[pOk])
        if kt == nk - 1:
            O_ = Osb[qb % 2]; Ok = f"Osb{qb%2}"
            kb.op("dve", lambda: nc.vector.tensor_copy(out=O_[:], in_=pO[0:65, :]), r=[pOk], w=[Ok])
            kb.op("pe", lambda: nc.tensor.matmul(psM[0][0:65, :], ones[0:1, 0:65], O_[0:1, :], start=True, stop=True), r=[Ok, "ones"], w=["psM0"])
            r_ = rec[qb % 2]; rk = f"rec{qb%2}"
            kb.op("dve", lambda: nc.vector.reciprocal(out=r_[:], in_=psM[0][0:65, :]), r=["psM0"], w=[rk])
            o_ = ot[qb % 2]; ok = f"ot{qb%2}"
            kb.op("pool", lambda: nc.gpsimd.tensor_tensor(out=o_[:], in0=O_[:], in1=r_[:], op=ALU.mult), r=[Ok, rk], w=[ok])
            kb.dma("sp", od[:, qb * 512:(qb + 1) * 512], o_[1:65, :], r=[ok], is_output=True)

    n = len(tiles)
    for i in range(n + LOOK):
        if i < n:
            emit_qk(i)
        if i - LOOK >= 0:
            emit_pv(i - LOOK)
    return kb.finish()


def host_B2(z, pairs4):
    C = fox_consts()
    big = np.full((128, 4, 512), -30000.0, np.float32); zero = np.zeros((128, 4, 512), np.float32)
    maps = []
    for r in range(8):
        (b, h) = pairs4[r // 2]; par = r % 2
        q = z[b, :, h * 64:(h + 1) * 64]; k = z[b, :, 384 + h * 64:384 + (h + 1) * 64]; v = z[b, :, 768 + h * 64:768 + (h + 1) * 64]
        ff = z[b, :, 1152 + h]
        qsel = np.concatenate([q[(2 * i + par) * 512:(2 * i + par + 1) * 512] for i in range(8)], axis=0)
        m = {"qT": np.ascontiguousarray(qsel.T), "kT": np.ascontiguousarray(k.T),
             "v": np.ascontiguousarray(v.reshape(64, 128, 64).transpose(1, 0, 2)),
             "ff": np.ascontiguousarray(ff.reshape(64, 128).T),
             "w0": np.full((128, 1), 1.0 - par, np.float32), "w1": np.full((128, 1), float(par), np.float32)}
        m.update(C)
        m["maskadd"] = np.ascontiguousarray(np.concatenate([C["maskadd"], big] if par == 0 else [zero, C["maskadd"]], axis=1))
        maps.append(m)
    return maps


def host_B(z, pairs):
    C = fox_consts()
    maps = []
    for (b, h) in pairs:
        q = z[b, :, h * 64:(h + 1) * 64]; k = z[b, :, 384 + h * 64:384 + (h + 1) * 64]; v = z[b, :, 768 + h * 64:768 + (h + 1) * 64]
        ff = z[b, :, 1152 + h]
        m = {"qT": np.ascontiguousarray(q.T), "kT": np.ascontiguousarray(k.T),
             "v": np.ascontiguousarray(v.reshape(64, 128, 64).transpose(1, 0, 2)),
             "ff": np.ascontiguousarray(ff.reshape(64, 128).T)}
        m.update(C)
        maps.append(m)
    return maps


def gla_consts(g):
    w = (2, 4, 8, 16)[g]
    s = np.arange(128)[:, None]; t = np.arange(128)[None, :]
    tri = (s <= t).astype(np.float32)
    triC = (s > t).astype(np.float32)
    eye = np.eye(128, dtype=np.float32)
    bandCur = np.where((s <= t) & (s >= t - w + 1), 1.0 / w, 0.0).astype(np.float32) - eye
    bandPrev = np.where(s >= 128 + t - w + 1, 1.0 / w, 0.0).astype(np.float32)
    cnt = np.minimum(t + 1, w).astype(np.float32)
    bandCur0 = np.where((s <= t) & (s >= t - w + 1), 1.0 / cnt, 0.0).astype(np.float32) - eye
    return {"tri": tri, "triC": triC, "bandCur": bandCur, "bandPrev": bandPrev, "bandCur0": bandCur0}


def build_G():
    kb = KB()
    nc = kb.nc
    S = 8192
    d_ga = kb.dram("ga1T", [17, S]); d_wa = kb.dram("wa2", [17, 48])
    d_qT = kb.dram("gqT", [48, S]); d_kT = kb.dram("gkT", [48, S]); d_k = kb.dram("gk", [128, 64, 48])
    d_v = kb.dram("gv", [128, 64, 96]); d_gr = kb.dram("gr", [128, 64, 96]); d_ng = kb.dram("normg", [128, 96])
    d_tri = kb.dram("tri", [128, 128]); d_triC = kb.dram("triC", [128, 128])
    d_u = kb.dram("pu", [128, 64, 64]); d_bc = kb.dram("bandCur", [128, 128]); d_bp = kb.dram("bandPrev", [128, 128])
    d_bc0 = kb.dram("bandCur0", [128, 128]); d_pw = kb.dram("poolw", [64, 64]); d_psc = kb.dram("pscale", [64, 1])
    d_og = kb.dram("og", [128, 64, 96], kind="ExternalOutput"); d_op = kb.dram("opT", [64, S], kind="ExternalOutput")

    gaB = [kb.sb(f"ga{i}", [17, 2048]) for i in range(2)]; wa = kb.sb("wa", [17, 48])
    qTB = [kb.sb(f"qT{i}", [48, 2048]) for i in range(2)]; kTB = [kb.sb(f"kT{i}", [48, 2048]) for i in range(2)]
    k = kb.sb("k", [128, 64, 48])
    v = kb.sb("v", [128, 64, 96]); gr = kb.sb("gr", [128, 64, 96]); ng = kb.sb("ng", [128, 96])
    tri = kb.sb("tri", [128, 128]); triC = kb.sb("triC", [128, 128])
    u = kb.sb("u", [128, 64, 64]); bc = kb.sb("bc", [128, 128]); bp = kb.sb("bp", [128, 128]); bc0 = kb.sb("bc0", [128, 128])
    pw = kb.sb("pw", [64, 64]); psc = kb.sb("psc", [64, 1])
    la = kb.sb("la", [128, 64, 48]); ogB = [kb.sb(f"og{i}", [128, 16, 96]) for i in range(2)]
    for (t_, d_, k_) in ((wa, d_wa, "wa"), (ng, d_ng, "ng"),
                         (tri, d_tri, "tri"), (triC, d_triC, "triC"), (bc, d_bc, "bc"), (bp, d_bp, "bp"), (bc0, d_bc0, "bc0"),
                         (pw, d_pw, "pw"), (psc, d_psc, "psc")):
        kb.dma("sp", t_[:], d_[:, :], w=[k_])
    for (t_, d_, k_) in ((k, d_k, "k"), (v, d_v, "v"), (gr, d_gr, "gr"), (u, d_u, "u")):
        kb.dma("sp", t_[:], d_[:, :, :], w=[k_])
    pss = [kb.ps(f"ps{i}", [128, 512]) for i in range(8)]
    for grp in range(16):
        p_ = pss[6 + grp % 2]; pk = f"ps{6 + grp % 2}"
        q4 = grp // 4; ga = gaB[q4 % 2]
        if grp % 4 == 0:
            kb.dma("sp", ga[:], d_ga[:, q4 * 2048:(q4 + 1) * 2048], w=[f"ga{q4%2}"])
        def mm(grp=grp, p_=p_, ga=ga):
            for i in range(4):
                c = (grp % 4) * 4 + i
                ins = nc.tensor.matmul(p_[:, i * 48:(i + 1) * 48], ga[:, c * 128:(c + 1) * 128], wa[:], start=True, stop=True)
            return ins
        kb.op("pe", mm, r=[f"ga{q4%2}", "wa"], w=[pk])
        kb.op("act", lambda grp=grp, p_=p_: nc.scalar.activation(out=la[:, grp * 4:(grp + 1) * 4, :].rearrange("p c d -> p (c d)"),
                                                                 in_=p_[:, 0:192], func=AF.Sigmoid), r=[pk], w=[f"sg{grp}"])
    kb.op("act", lambda: nc.scalar.activation(out=la[:].rearrange("p c d -> p (c d)"), in_=la[:].rearrange("p c d -> p (c d)"), func=AF.Ln),
          r=[f"sg{g}" for g in range(16)], w=["la"])
    kb.op("act", lambda: nc.scalar.activation(out=gr[:].rearrange("p c d -> p (c d)"), in_=gr[:].rearrange("p c d -> p (c d)"), func=AF.Silu),
          r=["gr"], w=["gr"])
    for c4 in range(4):
        kb.op("pool", lambda c4=c4: nc.gpsimd.tensor_tensor(out=gr[:, c4 * 16:(c4 + 1) * 16, :], in0=gr[:, c4 * 16:(c4 + 1) * 16, :],
                                                              in1=ng[:, None, :].to_broadcast([128, 16, 96]), op=ALU.mult),
              r=["gr", "ng"], w=["gr"])
    pooled = [kb.sb(f"pooled{i}", [64, 512]) for i in range(2)]
    opt = [kb.sb(f"opt{i}", [64, 512]) for i in range(2)]
    for blk in range(16):
        pp = pss[4]; pm = pss[5]
        def mmp(blk=blk):
            for i in range(4):
                t = blk * 4 + i
                o_ = pp[0:64, i * 128:(i + 1) * 128]
                if t == 0:
                    ins = nc.tensor.matmul(o_, u[:, 0, :], bc0[:], start=True, stop=True)
                else:
                    nc.tensor.matmul(o_, u[:, t, :], bc[:], start=True, stop=False)
                    ins = nc.tensor.matmul(o_, u[:, t - 1, :], bp[:], start=False, stop=True)
            return ins
        kb.op("pe", mmp, r=["u", "bc", "bp", "bc0"], w=["ps4"])
        pl = pooled[blk % 2]; plk = f"pooled{blk%2}"
        kb.op("act", lambda pl=pl: nc.scalar.activation(out=pl[:], in_=pp[0:64, :], func=AF.Copy), r=["ps4"], w=[plk])
        kb.op("pe", lambda pl=pl: nc.tensor.matmul(pm[0:64, :], pw[:], pl[:], start=True, stop=True), r=[plk, "pw"], w=["ps5"])
        o_ = opt[blk % 2]; ok = f"opt{blk%2}"
        kb.op("dve", lambda o_=o_: nc.vector.tensor_scalar(out=o_[:], in0=pm[0:64, :], scalar1=psc[:, 0:1], scalar2=None, op0=ALU.mult),
              r=["ps5", "psc"], w=[ok])
        kb.dma("sp", d_op[:, blk * 512:(blk + 1) * 512], o_[:], r=[ok], is_output=True)
    DEP = 3
    st = [kb.sb(f"st{i}", [48, 96]) for i in range(2)]
    eb = [kb.sb(f"eb{i}", [48, 128]) for i in range(DEP)]; enb = [kb.sb(f"enb{i}", [48, 128]) for i in range(DEP)]
    ebl = [kb.sb(f"ebl{i}", [128, 48]) for i in range(DEP)]
    qi = [kb.sb(f"qi{i}", [48, 128]) for i in range(DEP)]; ki = [kb.sb(f"ki{i}", [48, 128]) for i in range(DEP)]
    ko = [kb.sb(f"ko{i}", [128, 48]) for i in range(DEP)]; at = [kb.sb(f"at{i}", [128, 128]) for i in range(DEP)]
    junk = [kb.sb(f"junk{i}", [128, 96]) for i in range(DEP)]
    ss = [kb.sb(f"ss{i}", [128, 1]) for i in range(DEP)]; rs = [kb.sb(f"rs{i}", [128, 1]) for i in range(DEP)]
    kb.op("dve", lambda: nc.vector.memset(st[0][:], 0.0), w=["st0"])
    SC = 1.0 / 16.0
    pXb = [0, 1]; pYb = [2, 3]; pOb = [6, 7]

    def stage1(c):
        p = c % DEP
        pA = pss[pXb[c % 2]]; pAk = f"ps{pXb[c % 2]}"
        q4 = c // 16; qT = qTB[q4 % 2]; kT = kTB[q4 % 2]
        qTk = f"qT{q4%2}"; kTk = f"kT{q4%2}"
        if c % 16 == 0:
            kb.dma("sp", qT[:], d_qT[:, q4 * 2048:(q4 + 1) * 2048], w=[qTk])
            kb.dma("sp", kT[:], d_kT[:, q4 * 2048:(q4 + 1) * 2048], w=[kTk])
        cs = slice((c % 16) * 128, (c % 16 + 1) * 128)
        def mmb():
            nc.tensor.matmul(pA[0:48, 0:128], la[:, c, :], tri[:], start=True, stop=True)
            return nc.tensor.matmul(pA[:, 128:176], triC[:], la[:, c, :], start=True, stop=True)
        kb.op("pe", mmb, r=["la", "tri", "triC"], w=[pAk])
        kb.op("act", lambda: nc.scalar.activation(out=eb[p][:], in_=pA[0:48, 0:128], func=AF.Exp, scale=SC), r=[pAk], w=[f"eb{p}"])
        kb.op("act", lambda: nc.scalar.activation(out=enb[p][:], in_=pA[0:48, 0:128], func=AF.Exp, scale=-SC), r=[pAk], w=[f"enb{p}"])
        kb.op("act", lambda: nc.scalar.activation(out=ebl[p][:], in_=pA[:, 128:176], func=AF.Exp, scale=SC), r=[pAk], w=[f"ebl{p}"])
        kb.op("dve", lambda: nc.vector.scalar_tensor_tensor(out=qi[p][:], in0=qT[:, cs], scalar=48 ** -0.5, in1=eb[p][:],
                                                            op0=ALU.mult, op1=ALU.mult), r=[qTk, f"eb{p}"], w=[f"qi{p}"])
        kb.op("pool", lambda: nc.gpsimd.tensor_tensor(out=ki[p][:], in0=kT[:, cs], in1=enb[p][:], op=ALU.mult), r=[kTk, f"enb{p}"], w=[f"ki{p}"])
        kb.op("pool", lambda: nc.gpsimd.tensor_tensor(out=ko[p][:], in0=k[:, c, :], in1=ebl[p][:], op=ALU.mult), r=["k", f"ebl{p}"], w=[f"ko{p}"])

    def stage2(c):
        p = c % DEP
        pA = pss[pYb[c % 2]]; pAk = f"ps{pYb[c % 2]}"
        def mm2():
            nc.tensor.matmul(pA[:, 384:512], ki[p][:], qi[p][:], start=True, stop=True)
            return nc.tensor.matmul(pA[0:48, 256:352], ko[p][:], v[:, c, :], start=True, stop=True)
        kb.op("pe", mm2, r=[f"ki{p}", f"qi{p}", f"ko{p}", "v"], w=[pAk])
        kb.op("dve", lambda: nc.vector.tensor_tensor(out=at[p][:], in0=pA[:, 384:512], in1=tri[:], op=ALU.mult), r=[pAk, "tri"], w=[f"at{p}"])

    def stage3(c):
        p = c % DEP
        pA = pss[pYb[c % 2]]; pAk = f"ps{pYb[c % 2]}"; pO = pss[pOb[c % 2]]; pOk = f"ps{pOb[c % 2]}"
        q4 = c // 16; og = ogB[q4 % 2]
        sin = st[c % 2]; sout = st[(c + 1) % 2]
        def mmo():
            nc.tensor.matmul(pO[:, 0:96], at[p][:], v[:, c, :], start=True, stop=False)
            return nc.tensor.matmul(pO[:, 0:96], qi[p][:], sin[:], start=False, stop=True)
        kb.op("pe", mmo, r=[f"at{p}", "v", f"qi{p}", f"st{c%2}"], w=[pOk])
        kb.op("dve", lambda: nc.vector.scalar_tensor_tensor(out=sout[:], in0=sin[:], scalar=eb[p][:, 127:128], in1=pA[0:48, 256:352],
                                                            op0=ALU.mult, op1=ALU.add),
              r=[f"st{c%2}", f"eb{p}", pAk], w=[f"st{(c+1)%2}"])
        kb.op("act", lambda: nc.scalar.activation(out=junk[p][:], in_=pO[:, 0:96], func=AF.Square, accum_out=ss[p][:]), r=[pOk], w=[f"ss{p}", f"junk{p}"])
        kb.op("dve", lambda: nc.vector.tensor_scalar(out=rs[p][:], in0=ss[p][:], scalar1=1.0 / 96.0, scalar2=1e-6, op0=ALU.mult, op1=ALU.add),
              r=[f"ss{p}"], w=[f"rs{p}a"])
        kb.op("act", lambda: nc.scalar.activation(out=ss[p][:], in_=rs[p][:], func=AF.Ln), r=[f"rs{p}a"], w=[f"ss{p}"])
        kb.op("act", lambda: nc.scalar.activation(out=rs[p][:], in_=ss[p][:], func=AF.Exp, scale=-0.5), r=[f"ss{p}"], w=[f"rs{p}"])
        kb.op("dve", lambda: nc.vector.scalar_tensor_tensor(out=og[:, c % 16, :], in0=pO[:, 0:96], scalar=rs[p][:, 0:1], in1=gr[:, c, :],
                                                            op0=ALU.mult, op1=ALU.mult), r=[pOk, f"rs{p}", "gr"], w=[f"og{q4%2}_{c%16}"])
        if c % 16 == 15:
            kb.dma("sp", d_og[:, q4 * 16:(q4 + 1) * 16, :], og[:], r=[f"og{q4%2}_{i}" for i in range(16)], is_output=True)

    for i in range(64 + 2):
        if i < 64:
            stage1(i)
        if 0 <= i - 1 < 64:
            stage2(i - 1)
        if 0 <= i - 2 < 64:
            stage3(i - 2)
    return kb.finish()


def tok_tiles(a):
    return np.ascontiguousarray(a.reshape(64, 128, -1).transpose(1, 0, 2))


def host_G(I, l, z):
    maps = []
    for r in range(8):
        b, h = r // 4, r % 4
        zz = z[b]
        gq = zz[:, 1158 + 48 * h:1158 + 48 * (h + 1)]; gk = zz[:, 1350 + 48 * h:1350 + 48 * (h + 1)]
        gv = zz[:, 1542 + 96 * h:1542 + 96 * (h + 1)]; grr = zz[:, 1926 + 96 * h:1926 + 96 * (h + 1)]
        ga1 = zz[:, 2310:2326]; pu = zz[:, 2326 + 64 * h:2326 + 64 * (h + 1)]
        m = {"ga1T": np.ascontiguousarray(np.concatenate([ga1.T, np.ones((1, 8192), np.float32)], axis=0)),
             "wa2": np.ascontiguousarray(np.concatenate([I['gla_w_a2'][l][:, 48 * h:48 * (h + 1)], I['gla_b_a'][l][None, 48 * h:48 * (h + 1)]], axis=0)),
             "gqT": np.ascontiguousarray(gq.T), "gkT": np.ascontiguousarray(gk.T), "gk": tok_tiles(gk), "gv": tok_tiles(gv), "gr": tok_tiles(grr),
             "normg": np.ascontiguousarray(np.broadcast_to(I['gla_norm_g'][l][None, 96 * h:96 * (h + 1)], (128, 96))),
             "pu": tok_tiles(pu), "poolw": np.ascontiguousarray(I['pool_w'][l][h]),
             "pscale": np.ascontiguousarray(I['pool_scale'][l][64 * h:64 * (h + 1), None])}
        m.update(gla_consts(h))
        maps.append(m)
    return maps


def emit_ln(kb, nc, u, uk, xn, xnk, scr, pfx):
    st, mv, lv, rstd, nmr = scr["st"], scr["mv"], scr["lv"], scr["rstd"], scr["nmr"]
    kb.op("dve", lambda: nc.vector.bn_stats(out=st[:, 0, :], in_=u[:, 0:512]), r=[uk], w=[pfx + "st0"])
    kb.op("dve", lambda: nc.vector.bn_stats(out=st[:, 1, :], in_=u[:, 512:1024]), r=[uk], w=[pfx + "st1"])
    kb.op("dve", lambda: nc.vector.bn_aggr(out=mv[:], in_=st[:].rearrange("p a b -> p (a b)")), r=[pfx + "st0", pfx + "st1"], w=[pfx + "mv"])
    kb.op("act", lambda: nc.scalar.activation(out=lv[:], in_=mv[:, 1:2], func=AF.Ln, bias=scr["eps"][:, 0:1], scale=1.0), r=[pfx + "mv", "eps"], w=[pfx + "lv"])
    kb.op("act", lambda: nc.scalar.activation(out=rstd[:], in_=lv[:], func=AF.Exp, scale=-0.5), r=[pfx + "lv"], w=[pfx + "rstd"])
    kb.op("dve", lambda: nc.vector.scalar_tensor_tensor(out=nmr[:], in0=mv[:, 0:1], scalar=-1.0, in1=rstd[:], op0=ALU.mult, op1=ALU.mult),
          r=[pfx + "mv", pfx + "rstd"], w=[pfx + "nmr"])
    kb.op("act", lambda: nc.scalar.activation(out=xn[:], in_=u[:], func=AF.Identity, scale=rstd[:, 0:1], bias=nmr[:, 0:1]),
          r=[uk, pfx + "rstd", pfx + "nmr"], w=[xnk])


def ln_scratch(kb, pfx):
    return {"st": kb.sb(pfx + "st", [128, 2, 6]), "mv": kb.sb(pfx + "mv", [128, 2]), "lv": kb.sb(pfx + "lv", [128, 1]),
            "rstd": kb.sb(pfx + "rstd", [128, 1]), "nmr": kb.sb(pfx + "nmr", [128, 1])}


def build_C():
    kb = KB()
    nc = kb.nc
    d_mix = kb.dram("mixT", [128, 8, 2048]); d_wo = kb.dram("wout", [128, 8, 1024]); d_x = kb.dram("x", [128, 16, 1024])
    d_rows = {n: kb.dram(n, [128, 1024]) for n in ("g1", "lng", "lnb", "s2", "t2")}
    d_wr = kb.dram("wr", [128, 8, 32]); d_br = kb.dram("br", [128, 32]); d_id = kb.dram("ident", [128, 128])
    d_x1 = kb.dram("x1", [128, 16, 1024], kind="ExternalOutput"); d_h2 = kb.dram("h2", [128, 16, 1024], kind="ExternalOutput")
    d_G = kb.dram("G", [128, 16, 32], kind="ExternalOutput")
    DEP = 3
    wo = kb.sb("wo", [128, 8, 1024], F32R)
    mix = [kb.sb(f"mix{i}", [128, 8, 512], F32R) for i in range(2)]
    rows = {n: kb.sb("r_" + n, [128, 1024]) for n in d_rows}
    wr = kb.sb("wr", [128, 8, 32]); br = kb.sb("br", [128, 32]); ident = kb.sb("ident", [128, 128])
    eps = kb.sb("eps", [128, 1])
    xt = [kb.sb(f"xt{i}", [128, 1024]) for i in range(DEP)]
    tmp = [kb.sb(f"tmp{i}", [128, 1024]) for i in range(DEP)]
    u = [kb.sb(f"u{i}", [128, 1024]) for i in range(DEP)]
    xn = [kb.sb(f"xn{i}", [128, 1024]) for i in range(DEP)]
    x1 = [kb.sb(f"x1{i}", [128, 1024]) for i in range(DEP)]
    h2 = [kb.sb(f"h2{i}", [128, 1024]) for i in range(DEP)]
    h2T = [kb.sb(f"h2T{i}", [128, 8, 128]) for i in range(DEP)]
    lg = [kb.sb(f"lg{i}", [128, 32]) for i in range(DEP)]; top8 = [kb.sb(f"top8{i}", [128, 8]) for i in range(DEP)]
    msk = [kb.sb(f"msk{i}", [128, 32]) for i in range(DEP)]; ex = [kb.sb(f"ex{i}", [128, 32]) for i in range(DEP)]
    nmx = [kb.sb(f"nmx{i}", [128, 1]) for i in range(DEP)]; den = [kb.sb(f"den{i}", [128, 1]) for i in range(DEP)]
    Gt = kb.sb("Gt", [128, 16, 32])
    scr = [ln_scratch(kb, f"ln{i}") for i in range(DEP)]
    for s_ in scr:
        s_["eps"] = eps
    pss = [kb.ps(f"ps{i}", [128, 512]) for i in range(8)]
    kb.op("pool", lambda: nc.gpsimd.memset(eps[:], 1e-5), w=["eps"])
    for k in range(8):
        kb.dma("pool", wo[:, k, :], d_wo[:, k, :], w=[f"wo{k}"])
    for n in d_rows:
        kb.dma("sp", rows[n][:], d_rows[n][:, :], w=["r_" + n])
    kb.dma("sp", wr[:], d_wr[:, :, :], w=["wr"]); kb.dma("sp", br[:], d_br[:, :], w=["br"]); kb.dma("sp", ident[:], d_id[:, :], w=["ident"])
    kb.op("dve", lambda: nc.vector.tensor_scalar_add(out=rows["g1"][:], in0=rows["g1"][:], scalar1=1.0), r=["r_g1"], w=["r_g1"])
    kb.op("dve", lambda: nc.vector.tensor_scalar_add(out=rows["s2"][:], in0=rows["s2"][:], scalar1=1.0), r=["r_s2"], w=["r_s2"])
    wok = [f"wo{k}" for k in range(8)]

    def stage1(t):
        p = t % DEP; tb = t // 4; ti = t % 4
        mx = mix[tb % 2]; mxk = f"mix{tb%2}"
        if ti == 0:
            for k in range(8):
                kb.dma("pool", mx[:, k, :], d_mix[:, k, tb * 512:(tb + 1) * 512], w=[mxk + f"_{k}"])
        kb.dma("sp", xt[p][:], d_x[:, t, :], w=[f"xt{p}"])
        for half in range(2):
            bi = 2 * (t % 2) + half
            ps_ = pss[bi]; pk = f"ps{bi}"
            def mm(ps_=ps_, half=half):
                for k in range(8):
                    ins = nc.tensor.matmul(ps_[:], mx[:, k, ti * 128:(ti + 1) * 128], wo[:, k, half * 512:(half + 1) * 512],
                                           start=(k == 0), stop=(k == 7))
                return ins
            kb.op("pe", mm, r=[mxk + f"_{k}" for k in range(8)] + wok, w=[pk])
            hs = slice(half * 512, (half + 1) * 512)
            kb.op("dve", lambda ps_=ps_, hs=hs: nc.vector.tensor_tensor(out=tmp[p][:, hs], in0=ps_[:], in1=rows["g1"][:, hs], op=ALU.mult),
                  r=[pk, "r_g1"], w=[f"tmp{p}_{half}"])
        kb.op("dve", lambda: nc.vector.scalar_tensor_tensor(out=u[p][:], in0=xt[p][:], scalar=ALPHA, in1=tmp[p][:], op0=ALU.mult, op1=ALU.add),
              r=[f"xt{p}", f"tmp{p}_0", f"tmp{p}_1"], w=[f"u{p}"])
        emit_ln(kb, nc, u[p], f"u{p}", xn[p], f"xn{p}", scr[p], f"ln{p}")

    def stage2(t):
        p = t % DEP
        kb.op("dve", lambda: nc.vector.tensor_tensor(out=x1[p][:], in0=xn[p][:], in1=rows["lng"][:], op=ALU.mult), r=[f"xn{p}", "r_lng"], w=[f"x1{p}a"])
        kb.op("pool", lambda: nc.gpsimd.tensor_tensor(out=x1[p][:], in0=x1[p][:], in1=rows["lnb"][:], op=ALU.add), r=[f"x1{p}a", "r_lnb"], w=[f"x1{p}"])
        kb.dma("sp", d_x1[:, t, :], x1[p][:], r=[f"x1{p}"], is_output=True)
        kb.op("dve", lambda: nc.vector.tensor_tensor(out=h2[p][:], in0=x1[p][:], in1=rows["s2"][:], op=ALU.mult), r=[f"x1{p}", "r_s2"], w=[f"h2{p}a"])
        kb.op("pool", lambda: nc.gpsimd.tensor_tensor(out=h2[p][:], in0=h2[p][:], in1=rows["t2"][:], op=ALU.add), r=[f"h2{p}a", "r_t2"], w=[f"h2{p}"])
        kb.dma("sp", d_h2[:, t, :], h2[p][:], r=[f"h2{p}"], is_output=True)
        for half in range(2):
            ps_ = pss[4 + half]; pk = f"ps{4+half}"
            def tr(ps_=ps_, half=half):
                for i in range(4):
                    k = half * 4 + i
                    ins = nc.tensor.transpose(ps_[:, i * 128:(i + 1) * 128], h2[p][:, k * 128:(k + 1) * 128], ident[:])
                return ins
            kb.op("pe", tr, r=[f"h2{p}", "ident"], w=[pk])
            kb.op("act", lambda ps_=ps_, half=half: nc.scalar.activation(out=h2T[p][:, half * 4:(half + 1) * 4, :].rearrange("p a b -> p (a b)"),
                                                                         in_=ps_[:], func=AF.Copy), r=[pk], w=[f"h2T{p}_{half}"])

    def stage3(t):
        p = t % DEP
        pl = pss[6 + t % 2]; plk = f"ps{6 + t % 2}"
        def mml():
            for k in range(8):
                ins = nc.tensor.matmul(pl[:, 0:32], h2T[p][:, k, :], wr[:, k, :], start=(k == 0), stop=(k == 7))
            return ins
        kb.op("pe", mml, r=[f"h2T{p}_0", f"h2T{p}_1", "wr"], w=[plk])
        kb.op("dve", lambda: nc.vector.tensor_tensor(out=lg[p][:], in0=pl[:, 0:32], in1=br[:], op=ALU.add), r=[plk, "br"], w=[f"lg{p}"])
        kb.op("dve", lambda: nc.vector.max(out=top8[p][:], in_=lg[p][:]), r=[f"lg{p}"], w=[f"top8{p}"])
        kb.op("dve", lambda: nc.vector.tensor_scalar(out=msk[p][:], in0=lg[p][:], scalar1=top8[p][:, 3:4], scalar2=None, op0=ALU.is_ge),
              r=[f"lg{p}", f"top8{p}"], w=[f"msk{p}"])
        kb.op("dve", lambda: nc.vector.tensor_scalar_mul(out=nmx[p][:], in0=top8[p][:, 0:1], scalar1=-1.0), r=[f"top8{p}"], w=[f"nmx{p}"])
        kb.op("act", lambda: nc.scalar.activation(out=ex[p][:], in_=lg[p][:], func=AF.Exp, bias=nmx[p][:, 0:1], scale=1.0), r=[f"lg{p}", f"nmx{p}"], w=[f"ex{p}"])
        kb.op("dve", lambda: nc.vector.tensor_tensor(out=ex[p][:], in0=ex[p][:], in1=msk[p][:], op=ALU.mult), r=[f"ex{p}", f"msk{p}"], w=[f"em{p}"])
        kb.op("dve", lambda: nc.vector.reduce_sum(out=den[p][:], in_=ex[p][:], axis=AX.X), r=[f"em{p}"], w=[f"den{p}"])
        kb.op("dve", lambda: nc.vector.reciprocal(out=den[p][:], in_=den[p][:]), r=[f"den{p}"], w=[f"rden{p}"])
        kb.op("dve", lambda: nc.vector.tensor_scalar(out=Gt[:, t, :], in0=ex[p][:], scalar1=den[p][:, 0:1], scalar2=None, op0=ALU.mult),
              r=[f"em{p}", f"rden{p}"], w=[f"G{t}"])

    for i in range(16 + 2):
        if i < 16:
            stage1(i)
        if 0 <= i - 1 < 16:
            stage2(i - 1)
        if 0 <= i - 2 < 16:
            stage3(i - 2)
    kb.dma("sp", d_G[:, :, :], Gt[:], r=[f"G{t}" for t in range(16)], is_output=True)
    return kb.finish()


def rep128(v):
    return np.ascontiguousarray(np.broadcast_to(v[None, :], (128, v.shape[0])))


def core_tok_tiles(a):
    return np.ascontiguousarray(a.reshape(16, 128, -1).transpose(1, 0, 2))


def from_core_tok_tiles(a):
    return a.transpose(1, 0, 2).reshape(2048, -1)


def host_C(I, l, x, mix, mods):
    wout = np.ascontiguousarray(I['w_out'][l].reshape(8, 128, 1024).transpose(1, 0, 2))
    wr = np.ascontiguousarray(I['w_router'][l].reshape(8, 128, 32).transpose(1, 0, 2))
    maps = []
    for r in range(8):
        b, j = r // 4, r % 4
        sl = slice(j * 2048, (j + 1) * 2048)
        mixT = np.ascontiguousarray(mix[b, sl].T.reshape(8, 128, 2048).transpose(1, 0, 2))
        md = mods[l, b]
        maps.append({"mixT": mixT, "wout": wout, "x": core_tok_tiles(x[b, sl]),
                     "g1": rep128(md[2048:3072]), "lng": rep128(I['ln1_g'][l]), "lnb": rep128(I['ln1_b'][l]),
                     "s2": rep128(md[4096:5120]), "t2": rep128(md[3072:4096]),
                     "wr": wr, "br": rep128(I['b_router'][l]), "ident": np.eye(128, dtype=np.float32)})
    return maps


def post_C(res):
    def gather(name, d):
        return np.stack([np.concatenate([from_core_tok_tiles(res.results[b * 4 + j][name]) for j in range(4)], axis=0) for b in range(2)])
    return gather("x1", 1024), gather("h2", 1024), gather("G", 32)


CAPS = (2304, 2560, 2816, 3072, 3584, 4096)


def cap_tiles(cap):
    nt = [(i * 512, 512) for i in range(cap // 512)]
    if cap % 512:
        nt.append((cap - 256, 256))
    return nt


def pick_cap(nmax):
    for c in CAPS:
        if nmax <= c:
            return c
    return CAPS[-1]


def build_D(CAP=2816):
    NT = cap_tiles(CAP)
    kb = KB()
    nc = kb.nc
    BF = mybir.dt.bfloat16
    d_X = kb.dram("XT", [4, 128, 8, CAP]); d_gate = kb.dram("gate", [4, 128, CAP // 128])
    d_wgu = kb.dram("wgu", [4, 128, 8, 2048]); d_bgu = kb.dram("bgu", [4, 128, 16])
    d_wd = kb.dram("wd", [4, 128, 8, 1024]); d_bd = kb.dram("bd", [4, 128, 1024])
    d_Y = kb.dram("Y", [4, 128, CAP // 128, 1024], kind="ExternalOutput")
    wguB = [kb.sb(f"wgu{i}", [128, 8, 2048], BF) for i in range(2)]; wdB = [kb.sb(f"wd{i}", [128, 8, 1024], BF) for i in range(2)]
    bguB = [kb.sb(f"bgu{i}", [128, 16]) for i in range(2)]; bdB = [kb.sb(f"bd{i}", [128, 1024]) for i in range(2)]
    gateB = [kb.sb(f"gate{i}", [128, CAP // 128]) for i in range(2)]
    XT = [kb.sb(f"XT{i}", [128, 8, 512], BF) for i in range(3)]
    actT = [kb.sb(f"actT{i}", [128, 8, 512], BF) for i in range(2)]
    gl = [kb.sb(f"gl{i}", [128, 512]) for i in range(2)]; l1 = [kb.sb(f"l1{i}", [128, 512]) for i in range(2)]
    sg = [kb.sb(f"sg{i}", [128, 512]) for i in range(2)]
    ysb = [kb.sb(f"ysb{i}", [128, 1024]) for i in range(3)]
    pss = [kb.ps(f"ps{i}", [128, 512]) for i in range(8)]
    st_ = {"xc": 0, "cc": 0, "yc": 0, "pc": 0, "py": 0}

    def load_w(e):
        b = e % 2
        for k in range(8):
            kb.dma("pool", wguB[b][:, k, :], d_wgu[e, :, k, :], w=[f"wgu{b}_{k}"])
        for k in range(8):
            kb.dma("pool", wdB[b][:, k, :], d_wd[e, :, k, :], w=[f"wd{b}_{k}"])
        kb.dma("sp", bguB[b][:], d_bgu[e, :, :], w=[f"bgu{b}"]); kb.dma("sp", bdB[b][:], d_bd[e, :, :], w=[f"bd{b}"])
        kb.dma("sp", gateB[b][:], d_gate[e, :, :], w=[f"gate{b}"])

    def load_x(e, ti):
        n0, nn = NT[ti]
        xi = st_["xc"] % 3; st_["xc"] += 1
        for k in range(8):
            kb.dma("pool", XT[xi][:, k, 0:nn], d_X[e, :, k, n0:n0 + nn], w=[f"XT{xi}_{k}"])
        return xi

    units = [(e, ti) for e in range(4) for ti in range(len(NT))]
    load_w(0)
    xq = [load_x(*units[0])]
    for ui, (e, ti) in enumerate(units):
        b = e % 2
        wgu = wguB[b]; wd = wdB[b]; bgu = bguB[b]; bd = bdB[b]; gate = gateB[b]
        if ti == 0 and e + 1 < 4:
            load_w(e + 1)
        if ui + 1 < len(units):
            xq.append(load_x(*units[ui + 1]))
        xi = xq[ui]
        n0, nn = NT[ti]
        X_ = XT[xi]; Xks = [f"XT{xi}_{k}" for k in range(8)]
        ai = ui % 2; A_ = actT[ai]; Ak = f"actT{ai}"
        wguk = [f"wgu{b}_{k}" for k in range(8)]; wdk = [f"wd{b}_{k}" for k in range(8)]
        for c in range(8):
            pc = st_["pc"]; pg = pss[pc % 4]; pgk = f"ps{pc%4}"; pl = pss[(pc + 1) % 4]; plk = f"ps{(pc+1)%4}"; st_["pc"] += 2
            q = st_["cc"] % 2; st_["cc"] += 1
            def mmg(pg=pg, c=c):
                for k in range(8):
                    ins = nc.tensor.matmul(pg[:, 0:nn], wgu[:, k, c * 128:(c + 1) * 128], X_[:, k, 0:nn], start=(k == 0), stop=(k == 7))
                return ins
            kb.op("pe", mmg, r=wguk + Xks, w=[pgk])
            def mml(pl=pl, c=c):
                for k in range(8):
                    ins = nc.tensor.matmul(pl[:, 0:nn], wgu[:, k, (8 + c) * 128:(9 + c) * 128], X_[:, k, 0:nn], start=(k == 0), stop=(k == 7))
                return ins
            kb.op("pe", mml, r=wguk + Xks, w=[plk])
            kb.op("dve", lambda pg=pg, c=c, q=q: nc.vector.tensor_scalar(out=gl[q][:, 0:nn], in0=pg[:, 0:nn], scalar1=bgu[:, c:c + 1], scalar2=7.0,
                                                                      op0=ALU.add, op1=ALU.min), r=[pgk, f"bgu{b}"], w=[f"gl{q}"])
            kb.op("dve", lambda pl=pl, c=c, q=q: nc.vector.tensor_scalar(out=l1[q][:, 0:nn], in0=pl[:, 0:nn], scalar1=bgu[:, 8 + c:9 + c], scalar2=7.0,
                                                                      op0=ALU.add, op1=ALU.min), r=[plk, f"bgu{b}"], w=[f"l1{q}a"])
            kb.op("act", lambda q=q: nc.scalar.activation(out=sg[q][:, 0:nn], in_=gl[q][:, 0:nn], func=AF.Sigmoid, scale=1.702), r=[f"gl{q}"], w=[f"sg{q}a"])
            kb.op("dve", lambda q=q: nc.vector.tensor_scalar(out=l1[q][:, 0:nn], in0=l1[q][:, 0:nn], scalar1=-7.0, scalar2=1.0,
                                                             op0=ALU.max, op1=ALU.add), r=[f"l1{q}a"], w=[f"l1{q}"])
            kb.op("dve", lambda q=q: nc.vector.tensor_tensor(out=sg[q][:, 0:nn], in0=sg[q][:, 0:nn], in1=gl[q][:, 0:nn], op=ALU.mult),
                  r=[f"sg{q}a", f"gl{q}"], w=[f"sg{q}"])
            kb.op("dve", lambda q=q, c=c: nc.vector.tensor_tensor(out=A_[:, c, 0:nn], in0=sg[q][:, 0:nn], in1=l1[q][:, 0:nn], op=ALU.mult),
                  r=[f"sg{q}", f"l1{q}"], w=[Ak + f"_{c}"])
        Aks = [Ak + f"_{c}" for c in range(8)]
        for si in range(nn // 128):
            s = n0 // 128 + si
            yi = st_["yc"] % 3; st_["yc"] += 1
            y_ = ysb[yi]; yk = f"ysb{yi}"
            for half in range(2):
                pyi = 4 + st_["py"] % 4; st_["py"] += 1
                py = pss[pyi]; pyk = f"ps{pyi}"
                def mmy(py=py, half=half, si=si):
                    for k in range(8):
                        ins = nc.tensor.matmul(py[:], A_[:, k, si * 128:(si + 1) * 128], wd[:, k, half * 512:(half + 1) * 512], start=(k == 0), stop=(k == 7))
                    return ins
                kb.op("pe", mmy, r=Aks + wdk, w=[pyk])
                hs = slice(half * 512, (half + 1) * 512)
                kb.op("dve", lambda py=py, hs=hs: nc.vector.tensor_tensor(out=y_[:, hs], in0=py[:], in1=bd[:, hs], op=ALU.add), r=[pyk, f"bd{b}"], w=[yk + f"a{half}"])
            kb.op("act", lambda s=s: nc.scalar.activation(out=y_[:], in_=y_[:], func=AF.Copy, scale=gate[:, s:s + 1]),
                  r=[yk + "a0", yk + "a1", f"gate{b}"], w=[yk])
            kb.dma("sp", d_Y[e, :, s, :], y_[:], r=[yk], is_output=True)
    return kb.finish()


def route_host(G):
    return [np.nonzero(G[:, e] > 0)[0] for e in range(32)]


def host_D(I, l, h2f, G, idxs, off=0, CAP=2816):
    maps = []
    for r in range(8):
        XT = np.zeros((4, 128, 8, CAP), np.float32); gate = np.zeros((4, 128, CAP // 128), np.float32)
        for i in range(4):
            e = 4 * r + i
            idx = idxs[e][off:off + CAP]
            n = len(idx)
            rowsT = h2f[idx].T
            XT[i, :, :, :n] = rowsT.reshape(8, 128, n).transpose(1, 0, 2)
            gg = np.zeros(CAP, np.float32); gg[:n] = G[idx, e]
            gate[i] = gg.reshape(CAP // 128, 128).T
        es = slice(4 * r, 4 * r + 4)
        wgu = np.ascontiguousarray(I['w_gate_up'][l][es].reshape(4, 8, 128, 2048).transpose(0, 2, 1, 3))
        wd = np.ascontiguousarray(I['w_down'][l][es].reshape(4, 8, 128, 1024).transpose(0, 2, 1, 3))
        bgu = np.ascontiguousarray(I['b_gate_up'][l][es].reshape(4, 16, 128).transpose(0, 2, 1))
        bd = np.ascontiguousarray(np.broadcast_to(I['b_down'][l][es][:, None, :], (4, 128, 1024)))
        maps.append({"XT": XT, "gate": gate, "wgu": wgu, "bgu": bgu, "wd": wd, "bd": bd})
    return maps


def post_D(res, idxs, Y4=None, fill=None, off=0, CAP=2816):
    if Y4 is None:
        Y4 = np.zeros((16384, 4, 1024), np.float32)
        fill = np.zeros(16384, np.int64)
    for r in range(8):
        Y = res.results[r]["Y"]
        for i in range(4):
            e = 4 * r + i
            idx = idxs[e][off:off + CAP]
            rows = Y[i].transpose(1, 0, 2).reshape(CAP, 1024)[:len(idx)]
            Y4[idx, fill[idx]] = rows
            fill[idx] += 1
    return Y4, fill


def build_E():
    kb = KB()
    nc = kb.nc
    d_x1 = kb.dram("x1", [128, 16, 1024]); d_Y4 = kb.dram("Y4", [128, 16, 4, 1024])
    d_rows = {n: kb.dram(n, [128, 1024]) for n in ("g2", "lng", "lnb")}
    d_x2 = kb.dram("x2", [128, 16, 1024], kind="ExternalOutput")
    rows = {n: kb.sb("r_" + n, [128, 1024]) for n in d_rows}
    eps = kb.sb("eps", [128, 1])
    xt = [kb.sb(f"xt{i}", [128, 1024]) for i in range(2)]
    y4 = [kb.sb(f"y4{i}", [128, 4, 1024]) for i in range(2)]
    sa = [kb.sb(f"sa{i}", [128, 1024]) for i in range(2)]; sb_ = [kb.sb(f"sb{i}", [128, 1024]) for i in range(2)]
    u = [kb.sb(f"u{i}", [128, 1024]) for i in range(2)]; xn = [kb.sb(f"xn{i}", [128, 1024]) for i in range(2)]
    scr = [ln_scratch(kb, f"ln{i}") for i in range(2)]
    for s_ in scr:
        s_["eps"] = eps
    kb.op("pool", lambda: nc.gpsimd.memset(eps[:], 1e-5), w=["eps"])
    for n in d_rows:
        kb.dma("sp", rows[n][:], d_rows[n][:, :], w=["r_" + n])
    kb.op("dve", lambda: nc.vector.tensor_scalar_add(out=rows["g2"][:], in0=rows["g2"][:], scalar1=1.0), r=["r_g2"], w=["r_g2"])
    for t in range(16):
        p = t % 2
        kb.dma("sp", xt[p][:], d_x1[:, t, :], w=[f"xt{p}"])
        kb.dma("sp", y4[p][:], d_Y4[:, t, :, :], w=[f"y4{p}"])
        kb.op("dve", lambda: nc.vector.tensor_tensor(out=sa[p][:], in0=y4[p][:, 0, :], in1=y4[p][:, 1, :], op=ALU.add), r=[f"y4{p}"], w=[f"sa{p}a"])
        kb.op("pool", lambda: nc.gpsimd.tensor_tensor(out=sb_[p][:], in0=y4[p][:, 2, :], in1=y4[p][:, 3, :], op=ALU.add), r=[f"y4{p}"], w=[f"sb{p}"])
        kb.op("pool", lambda: nc.gpsimd.tensor_tensor(out=sa[p][:], in0=sa[p][:], in1=sb_[p][:], op=ALU.add), r=[f"sa{p}a", f"sb{p}"], w=[f"sa{p}b"])
        kb.op("pool", lambda: nc.gpsimd.tensor_tensor(out=sa[p][:], in0=sa[p][:], in1=rows["g2"][:], op=ALU.mult), r=[f"sa{p}b", "r_g2"], w=[f"sa{p}"])
        kb.op("dve", lambda: nc.vector.scalar_tensor_tensor(out=u[p][:], in0=xt[p][:], scalar=ALPHA, in1=sa[p][:], op0=ALU.mult, op1=ALU.add),
              r=[f"xt{p}", f"sa{p}"], w=[f"u{p}"])
        emit_ln(kb, nc, u[p], f"u{p}", xn[p], f"xn{p}", scr[p], f"ln{p}")
        kb.op("dve", lambda: nc.vector.tensor_tensor(out=xn[p][:], in0=xn[p][:], in1=rows["lng"][:], op=ALU.mult), r=[f"xn{p}", "r_lng"], w=[f"xn{p}b"])
        kb.op("pool", lambda: nc.gpsimd.tensor_tensor(out=xn[p][:], in0=xn[p][:], in1=rows["lnb"][:], op=ALU.add), r=[f"xn{p}b", "r_lnb"], w=[f"xn{p}c"])
        kb.dma("sp", d_x2[:, t, :], xn[p][:], r=[f"xn{p}c"], is_output=True)
    return kb.finish()


def host_E(I, l, x1, Y4, mods):
    maps = []
    for r in range(8):
        b, j = r // 4, r % 4
        sl = slice(j * 2048, (j + 1) * 2048)
        y = Y4[b * 8192 + j * 2048: b * 8192 + (j + 1) * 2048]
        maps.append({"x1": core_tok_tiles(x1[b, sl]),
                     "Y4": np.ascontiguousarray(y.reshape(16, 128, 4, 1024).transpose(1, 0, 2, 3)),
                     "g2": rep128(mods[l, b, 5120:6144]), "lng": rep128(I['ln2_g'][l]), "lnb": rep128(I['ln2_b'][l])})
    return maps


def post_E(res):
    return np.stack([np.concatenate([from_core_tok_tiles(res.results[b * 4 + j]["x2"]) for j in range(4)], axis=0) for b in range(2)])


_PROGS = {}


def _prog(name, fn):
    if name not in _PROGS:
        _PROGS[name] = fn()
    return _PROGS[name]


def kernel(**I):
    I = {k: np.asarray(v) for k, v in I.items()}
    x = np.ascontiguousarray(I['x'], dtype=np.float32)
    mods = post_M(run(_prog("M", build_M), host_M(I)))
    pairs_all = [(b, h) for b in range(2) for h in range(6)]
    for l in range(4):
        z = post_A(run(_prog("A", build_A), host_A(I, l, x, mods)))
        mix = np.zeros((2, 8192, 1024), np.float32)
        res = run(_prog("B", build_B), host_B(z, pairs_all[0:8]))
        for i, (b, h) in enumerate(pairs_all[0:8]):
            mix[b, :, h * 64:(h + 1) * 64] = res.results[i]["oT"].T
        res = run(_prog("B2", lambda: build_B(half=True)), host_B2(z, pairs_all[8:12]))
        for r in range(8):
            (b, h) = pairs_all[8 + r // 2]; par = r % 2
            o = res.results[r]["oT"].T
            for i in range(8):
                qb = 2 * i + par
                mix[b, qb * 512:(qb + 1) * 512, h * 64:(h + 1) * 64] = o[i * 512:(i + 1) * 512]
        res = run(_prog("G", build_G), host_G(I, l, z))
        for r in range(8):
            b, h = r // 4, r % 4
            mix[b, :, 384 + h * 96:384 + (h + 1) * 96] = res.results[r]["og"].transpose(1, 0, 2).reshape(8192, 96)
            mix[b, :, 768 + h * 64:768 + (h + 1) * 64] = res.results[r]["opT"].T
        del z
        x1, h2, G = post_C(run(_prog("C", build_C), host_C(I, l, x, mix, mods)))
        Gf = G.reshape(-1, 32); h2f = h2.reshape(-1, 1024)
        idxs = route_host(Gf)
        nmax = max(len(i) for i in idxs)
        cap = pick_cap(nmax)
        Y4 = None; fill = None; off = 0
        while True:
            res = run(_prog(f"D{cap}", lambda: build_D(cap)), host_D(I, l, h2f, Gf, idxs, off, CAP=cap))
            Y4, fill = post_D(res, idxs, Y4, fill, off, CAP=cap)
            off += cap
            if off >= nmax:
                break
        x = post_E(run(_prog("E", build_E), host_E(I, l, x1, Y4, mods)))
    return np.ascontiguousarray(x, dtype=np.float32)
```
